# Optimizing a Trainium2 kernel written in Bass

```python
import jax, jax.numpy as jnp
from jax import lax
import numpy as np

D_MODEL = 2048
BATCH = 4
SEQ = 4096
DEPTH = 2

N_META = 16
POOL_WINDOWS = (2, 4, 8, 16)
POOL_WIDTH = D_MODEL // 2
POOL_GROUP = POOL_WIDTH // len(POOL_WINDOWS)
N_HEADS = 16
N_KV_HEADS = 4
HEAD_DIM = 64
Q_PER_KV = N_HEADS // N_KV_HEADS
WINDOW = 128
BLOCK = 128
NEG = -1e30
LRU_WIDTH = D_MODEL // 2
LRU_BLOCKS = 4
LRU_BLOCK = LRU_WIDTH // LRU_BLOCKS
CONV_WIDTH = 4
LRU_C = 8.0
N_GROUPS = 4
EXPERTS_PER_GROUP = 8
N_EXPERTS = N_GROUPS * EXPERTS_PER_GROUP
TOP_K = 2
D_EXPERT = D_MODEL // 4
MOE_BLOCK = 128
LN_EPS = 1e-5
IN_SIZES = (POOL_WIDTH, N_HEADS * HEAD_DIM, N_KV_HEADS * HEAD_DIM, N_KV_HEADS * HEAD_DIM,
            LRU_WIDTH, LRU_WIDTH, 3 * D_MODEL)
IN_COLS = int(sum(IN_SIZES))
IN_OFFSETS = tuple(int(o) for o in np.cumsum(IN_SIZES)[:-1])

kernel_name = 'hybrid_pool_swa_rglru_hmoe_deepnorm'


def layer_norm(x, g, b):
    xf = x.astype(jnp.float32)
    mu = xf.mean(-1, keepdims=True)
    var = jnp.square(xf - mu).mean(-1, keepdims=True)
    y = (xf - mu) * lax.rsqrt(var + LN_EPS) * g.astype(jnp.float32) + b.astype(jnp.float32)
    return y.astype(x.dtype)


def pool_mixer(u, pool_w, pool_scale):
    B, L, _ = u.shape
    uf = u.astype(jnp.float32).reshape(B, L, len(POOL_WINDOWS), POOL_GROUP)
    maxw = max(POOL_WINDOWS)
    cs = jnp.pad(jnp.cumsum(uf, axis=1), ((0, 0), (maxw, 0), (0, 0), (0, 0)))
    t = jnp.arange(L)
    outs = []
    for gi, w in enumerate(POOL_WINDOWS):
        win_sum = cs[:, maxw:, gi] - cs[:, maxw - w:maxw - w + L, gi]
        cnt = jnp.minimum(t + 1, w).astype(jnp.float32)
        outs.append(win_sum / cnt[None, :, None])
    delta = (jnp.stack(outs, axis=2) - uf).astype(u.dtype)
    mixed = jnp.einsum('blgc,gce->blge', delta, pool_w).reshape(B, L, POOL_WIDTH)
    return mixed * pool_scale


def swa_attention(q, k, v, sink):
    B, L = q.shape[:2]
    pad = BLOCK - N_META
    Lp = L + pad
    nb = Lp // BLOCK
    f32 = jnp.float32
    qb = jnp.pad(q.astype(f32), ((0, 0), (pad, 0), (0, 0), (0, 0))).reshape(
        B, nb, BLOCK, N_KV_HEADS, Q_PER_KV, HEAD_DIM)
    kb = jnp.pad(k.astype(f32), ((0, 0), (pad + BLOCK, 0), (0, 0), (0, 0))).reshape(
        B, nb + 1, BLOCK, N_KV_HEADS, HEAD_DIM)
    vb = jnp.pad(v.astype(f32), ((0, 0), (pad + BLOCK, 0), (0, 0), (0, 0))).reshape(
        B, nb + 1, BLOCK, N_KV_HEADS, HEAD_DIM)
    kw = jnp.concatenate([kb[:, :-1], kb[:, 1:]], axis=2)
    vw = jnp.concatenate([vb[:, :-1], vb[:, 1:]], axis=2)
    dist = BLOCK + jnp.arange(BLOCK)[:, None] - jnp.arange(2 * BLOCK)[None, :]
    in_window = (dist >= 0) & (dist < WINDOW)
    k_pos = (jnp.arange(nb)[:, None] - 1) * BLOCK + jnp.arange(2 * BLOCK)[None, :]
    mask = in_window[None] & (k_pos >= pad)[:, None, :]
    slopes = 2.0 ** (-8.0 * jnp.arange(1, N_HEADS + 1, dtype=f32) / N_HEADS)
    alibi = -slopes.reshape(N_KV_HEADS, Q_PER_KV, 1, 1) * dist.astype(f32)
    s = jnp.einsum('bnqkgd,bnskd->bnkgqs', qb, kw) * (HEAD_DIM ** -0.5) + alibi
    s = jnp.where(mask[None, :, None, None], s, NEG)
    sk = sink.astype(f32).reshape(N_KV_HEADS, Q_PER_KV, 1)
    m = jnp.maximum(s.max(-1), sk)
    p = jnp.exp(s - m[..., None])
    den = p.sum(-1) + jnp.exp(sk - m)
    o = jnp.einsum('bnkgqs,bnskd->bnkgqd', p, vw) / den[..., None]
    o = o.transpose(0, 1, 4, 2, 3, 5).reshape(B, Lp, N_HEADS * HEAD_DIM)[:, pad:]
    return o.astype(q.dtype)


def rglru_branch(xr, yr, conv_w, conv_b, wa, ba, wx, bx, lam):
    B, L, C = xr.shape
    xc = lax.conv_general_dilated(xr, conv_w[:, None, :], window_strides=(1,),
                                  padding=[(CONV_WIDTH - 1, 0)],
                                  dimension_numbers=('NWC', 'WIO', 'NWC'),
                                  feature_group_count=C) + conv_b
    xblk = xc.reshape(B, L, LRU_BLOCKS, LRU_BLOCK)
    gate_a = jax.nn.sigmoid(jnp.einsum('blhc,hce->blhe', xblk, wa).reshape(B, L, C) + ba)
    gate_x = jax.nn.sigmoid(jnp.einsum('blhc,hce->blhe', xblk, wx).reshape(B, L, C) + bx)
    log_a = -LRU_C * gate_a.astype(jnp.float32) * jax.nn.softplus(-lam.astype(jnp.float32))
    a = jnp.exp(log_a)
    b_in = jnp.sqrt(-jnp.expm1(2.0 * log_a)) * gate_x.astype(jnp.float32) * xc.astype(jnp.float32)

    def combine(left, right):
        return (left[0] * right[0], right[0] * left[1] + right[1])

    _, h = lax.associative_scan(combine, (a, b_in), axis=1)
    return (h * jax.nn.gelu(yr.astype(jnp.float32))).astype(xr.dtype)


def hier_moe(u, wg, bg, we, be, w_gate, w_up, w_down):
    B, L, D = u.shape
    N = B * L
    f32 = jnp.float32
    xf = u.reshape(N, D)
    grp_logits = (xf @ wg).astype(f32) + bg.astype(f32)
    p_grp = jax.nn.softmax(grp_logits, axis=-1)
    g = jnp.argmax(grp_logits, axis=-1)
    p_g = jnp.take_along_axis(p_grp, g[:, None], axis=1)
    exp_logits = ((xf @ we).astype(f32) + be.astype(f32)).reshape(N, N_GROUPS, EXPERTS_PER_GROUP)
    sel = jnp.take_along_axis(exp_logits, g[:, None, None], axis=1)[:, 0]
    top_p, top_i = lax.top_k(jax.nn.softmax(sel, axis=-1), TOP_K)
    wts = p_g * top_p / top_p.sum(-1, keepdims=True)
    eid = g[:, None] * EXPERTS_PER_GROUP + top_i
    A = N * TOP_K
    flat_e = eid.reshape(A).astype(jnp.int32)
    flat_w = wts.reshape(A)
    order = jnp.argsort(flat_e)
    se = flat_e[order]
    counts = jnp.bincount(flat_e, length=N_EXPERTS)
    start = jnp.cumsum(counts) - counts
    padded = (counts + MOE_BLOCK - 1) // MOE_BLOCK * MOE_BLOCK
    pad_end = jnp.cumsum(padded)
    pad_start = pad_end - padded
    dest = pad_start[se] + jnp.arange(A) - start[se]
    n_blocks = -(-A // MOE_BLOCK) + N_EXPERTS
    cap = n_blocks * MOE_BLOCK
    slot_tok = jnp.full((cap,), N, jnp.int32).at[dest].set((order // TOP_K).astype(jnp.int32))
    slot_w = jnp.zeros((cap,), f32).at[dest].set(flat_w[order])
    block_e = jnp.minimum(jnp.searchsorted(pad_end, jnp.arange(n_blocks) * MOE_BLOCK, side='right'),
                          N_EXPERTS - 1)
    x_pad = jnp.concatenate([xf, jnp.zeros((1, D), xf.dtype)], axis=0)

    def run_block(args):
        tok, wt, e = args
        xb = x_pad[tok]
        hdn = jax.nn.silu(xb @ w_gate[e]) * (xb @ w_up[e])
        return (hdn @ w_down[e]) * wt[:, None].astype(xb.dtype)

    yb = lax.map(run_block, (slot_tok.reshape(n_blocks, MOE_BLOCK),
                             slot_w.reshape(n_blocks, MOE_BLOCK), block_e))
    y = jnp.zeros((N + 1, D), u.dtype).at[slot_tok].add(yb.reshape(cap, D).astype(u.dtype))
    return y[:N].reshape(B, L, D)


def setup_inputs(seed: int = 0) -> dict:
    key = jax.random.key(seed)
    ks = iter(jax.random.split(key, 40))
    f32 = jnp.float32
    D = D_MODEL
    beta = (8.0 * DEPTH) ** -0.25

    def nrm(shape, scale):
        return jax.random.normal(next(ks), shape, f32) * scale

    u = jax.random.uniform(next(ks), (DEPTH, LRU_WIDTH), f32, 0.9, 0.999)
    a0 = u ** (1.0 / LRU_C)
    lru_lambda = jnp.log(a0) - jnp.log1p(-a0)
    return {
        'x': nrm((BATCH, SEQ, D), 1.0),
        'meta': nrm((N_META, D), 1.0),
        'ln_emb_g': 1.0 + nrm((D,), 0.02),
        'ln_emb_b': nrm((D,), 0.02),
        'w_in': nrm((DEPTH, D, IN_COLS), D ** -0.5),
        'pool_w': nrm((DEPTH, len(POOL_WINDOWS), POOL_GROUP, POOL_GROUP), POOL_GROUP ** -0.5),
        'pool_scale': 1.0 + nrm((DEPTH, POOL_WIDTH), 0.1),
        'attn_sink': nrm((DEPTH, N_HEADS), 1.0),
        'conv_w': nrm((DEPTH, CONV_WIDTH, LRU_WIDTH), CONV_WIDTH ** -0.5),
        'conv_b': nrm((DEPTH, LRU_WIDTH), 0.02),
        'lru_wa': nrm((DEPTH, LRU_BLOCKS, LRU_BLOCK, LRU_BLOCK), LRU_BLOCK ** -0.5),
        'lru_ba': nrm((DEPTH, LRU_WIDTH), 0.02),
        'lru_wx': nrm((DEPTH, LRU_BLOCKS, LRU_BLOCK, LRU_BLOCK), LRU_BLOCK ** -0.5),
        'lru_bx': nrm((DEPTH, LRU_WIDTH), 0.02),
        'lru_lambda': lru_lambda,
        'proj_pool': nrm((DEPTH, POOL_WIDTH, D), beta * POOL_WIDTH ** -0.5),
        'proj_attn': nrm((DEPTH, N_HEADS * HEAD_DIM, D), beta * (N_HEADS * HEAD_DIM) ** -0.5),
        'proj_lru': nrm((DEPTH, LRU_WIDTH, D), beta * LRU_WIDTH ** -0.5),
        'w_out': nrm((DEPTH, D, D), beta * D ** -0.5),
        'ln1_g': 1.0 + nrm((DEPTH, D), 0.02),
        'ln1_b': nrm((DEPTH, D), 0.02),
        'router_grp_w': nrm((DEPTH, D, N_GROUPS), D ** -0.5),
        'router_grp_b': nrm((DEPTH, N_GROUPS), 0.01),
        'router_exp_w': nrm((DEPTH, D, N_EXPERTS), D ** -0.5),
        'router_exp_b': nrm((DEPTH, N_EXPERTS), 0.01),
        'exp_w_gate': nrm((DEPTH, N_EXPERTS, D, D_EXPERT), D ** -0.5),
        'exp_w_up': nrm((DEPTH, N_EXPERTS, D, D_EXPERT), beta * D ** -0.5),
        'exp_w_down': nrm((DEPTH, N_EXPERTS, D_EXPERT, D), beta * D_EXPERT ** -0.5),
        'ln2_g': 1.0 + nrm((DEPTH, D), 0.02),
        'ln2_b': nrm((DEPTH, D), 0.02),
    }


def reference(x, meta, ln_emb_g, ln_emb_b, w_in, pool_w, pool_scale, attn_sink, conv_w, conv_b,
              lru_wa, lru_ba, lru_wx, lru_bx, lru_lambda, proj_pool, proj_attn, proj_lru, w_out,
              ln1_g, ln1_b, router_grp_w, router_grp_b, router_exp_w, router_exp_b,
              exp_w_gate, exp_w_up, exp_w_down, ln2_g, ln2_b):
    B = x.shape[0]
    alpha = (2.0 * DEPTH) ** 0.25
    h = jnp.concatenate([jnp.broadcast_to(meta[None].astype(x.dtype), (B, N_META, D_MODEL)), x], axis=1)
    h = layer_norm(h, ln_emb_g, ln_emb_b)
    L = h.shape[1]
    for l in range(DEPTH):
        cols = h @ w_in[l]
        pool_in, q, k, v, lru_x, lru_y, gate_cols = jnp.split(cols, IN_OFFSETS, axis=-1)
        pool_o = pool_mixer(pool_in, pool_w[l], pool_scale[l])
        attn_o = swa_attention(q.reshape(B, L, N_HEADS, HEAD_DIM),
                               k.reshape(B, L, N_KV_HEADS, HEAD_DIM),
                               v.reshape(B, L, N_KV_HEADS, HEAD_DIM), attn_sink[l])
        lru_o = rglru_branch(lru_x, lru_y, conv_w[l], conv_b[l], lru_wa[l], lru_ba[l],
                             lru_wx[l], lru_bx[l], lru_lambda[l])
        gts = jax.nn.sigmoid(gate_cols).reshape(B, L, 3, D_MODEL)
        merged = (gts[:, :, 0] * (pool_o @ proj_pool[l])
                  + gts[:, :, 1] * (attn_o @ proj_attn[l])
                  + gts[:, :, 2] * (lru_o @ proj_lru[l]))
        h = layer_norm(alpha * h + merged @ w_out[l], ln1_g[l], ln1_b[l])
        ffn = hier_moe(h, router_grp_w[l], router_grp_b[l], router_exp_w[l], router_exp_b[l],
                       exp_w_gate[l], exp_w_up[l], exp_w_down[l])
        h = layer_norm(alpha * h + ffn, ln2_g[l], ln2_b[l])
    return h[:, N_META:]
```

```python
import contextlib
import numpy as np
import ml_dtypes
import concourse.bass as bass
import concourse.mybir as mybir
from concourse.bass_utils import run_bass_kernel_spmd

F32 = mybir.dt.float32
BF16 = mybir.dt.bfloat16
AF = mybir.ActivationFunctionType
ALU = mybir.AluOpType
AX = mybir.AxisListType

COMPUTE = ('scalar', 'vector', 'tensor', 'gpsimd')
QUEUES = ('sync', 'scalar', 'vector', 'tensor', 'gpsimd')
DT_SIZE = {F32: 4, BF16: 2, mybir.dt.int32: 4}

D = 2048
DC = 16
SEQ = 4096
NMETA = 16
PAD = 112
T = 4224
NT = 384
NTILES = 11
NBLK = 33
DEPTH = 2
NCH_COLS = 84
C_POOL, C_Q, C_K, C_V, C_LX, C_LY, C_G = 0, 8, 16, 18, 20, 28, 36
NEXP = 32
ALPHA = (2.0 * DEPTH) ** 0.25
EPS = 1e-5
NEG = -1e30
NSB = 97
CAP = NSB * 128
BIGI = 1048576.0
I32 = mybir.dt.int32
SP_LN1G, SP_LN1B, SP_LN2G, SP_LN2B = 0, 16, 32, 48
SP_PSC, SP_CW, SP_CB, SP_BA, SP_BX, SP_LAM, SP_SINK = 64, 72, 104, 112, 120, 128, 136
SP_N = 152


class Buf:
    __slots__ = ('name', 'w', 'r', 'sem')

    def __init__(self, name):
        self.name = name
        self.w = None
        self.r = []
        self.sem = None


class Op:
    __slots__ = ('q', 'fn', 'deps', 'is_dma', 'sem', 'val', 'needed')

    def __init__(self, q, fn, is_dma):
        self.q = q
        self.fn = fn
        self.deps = []
        self.is_dma = is_dma
        self.sem = None
        self.val = 0
        self.needed = False


class Tile:
    __slots__ = ('t', 'buf')

    def __init__(self, t, name):
        self.t = t
        self.buf = Buf(name)

    def __getitem__(self, idx):
        return self.t[idx]


def _b(x):
    return getattr(x, 'buf', x)


_BREG = {}


def _breg(e, val):
    k = (id(e), int(val))
    if k not in _BREG:
        _BREG[k] = e.to_reg(int(val))
    return _BREG[k]


class Prog:
    SB_LO = 20480
    SB_HI = 222 * 1024

    def __init__(self, nc, n_dma_sems=56):
        self.nc = nc
        self.ops = {q: [] for q in QUEUES}
        self.esem = {}
        self.dma_pool = []
        self.n_dma_sems = n_dma_sems
        self.pool_idx = 0
        self.sb_off = self.SB_LO
        self.sb_base = self.SB_LO
        self.sb_max = 0
        self.uid = 0
        self.live = []

    def setup(self, stack):
        for e in COMPUTE:
            self.esem[e] = stack.enter_context(self.nc.semaphore("es_" + e))
        for i in range(self.n_dma_sems):
            self.dma_pool.append([stack.enter_context(self.nc.semaphore("ds%d" % i)), 0])

    def sbuf(self, name, shape, dtype):
        nbytes = int(np.prod(shape[1:])) * DT_SIZE[dtype]
        off = (self.sb_off + 63) // 64 * 64
        self.uid += 1
        t = self.nc.alloc_sbuf_tensor_at("%s_%d" % (name, self.uid), list(shape), dtype, offset=off)
        self.sb_off = off + nbytes
        self.sb_max = max(self.sb_max, self.sb_off)
        assert self.sb_off <= self.SB_HI, ("SBUF overflow", name, self.sb_off)
        return Tile(t, name)

    def phase_begin(self):
        self.sb_off = self.sb_base

    def persist_mark(self):
        self.sb_base = self.sb_off

    def _hazards(self, op, reads, writes):
        deps = []
        strong = set()
        for b in reads:
            b = _b(b)
            if b.w is not None:
                deps.append(b.w)
                strong.add(id(b.w))
        for b in writes:
            b = _b(b)
            if b.w is not None:
                deps.append(b.w)
                strong.add(id(b.w))
            deps.extend(b.r)
        for b in reads:
            _b(b).r.append(op)
        for b in writes:
            b = _b(b)
            b.w = op
            b.r = []
        seen = set()
        for d in deps:
            if d is op or id(d) in seen:
                continue
            seen.add(id(d))
            if (not d.is_dma) and (not op.is_dma) and d.q == op.q:
                if d.q == 'tensor' or id(d) not in strong:
                    continue
            if not d.is_dma:
                d.needed = True
            op.deps.append(d)

    def op(self, eng, fn, reads=(), writes=()):
        o = Op(eng, fn, False)
        self._hazards(o, reads, writes)
        self.ops[eng].append(o)
        return o

    def dma(self, q, out, in_, reads=(), writes=(), key=None):
        o = Op(q, (lambda e, out=out, in_=in_: e.dma_start(out=out, in_=in_)), True)
        b = _b(key)
        if b.sem is None:
            assert self.pool_idx < len(self.dma_pool), "out of dma sems"
            b.sem = self.dma_pool[self.pool_idx]
            self.pool_idx += 1
            self.live.append(b)
        b.sem[1] += 16
        o.sem = b.sem[0]
        o.val = b.sem[1]
        self._hazards(o, reads, writes)
        self.ops[q].append(o)
        return o

    def dma_fn(self, q, fn, reads=(), writes=(), key=None):
        o = Op(q, fn, True)
        b = _b(key)
        if b.sem is None:
            assert self.pool_idx < len(self.dma_pool), "out of dma sems"
            b.sem = self.dma_pool[self.pool_idx]
            self.pool_idx += 1
            self.live.append(b)
        b.sem[1] += 16
        o.sem = b.sem[0]
        o.val = b.sem[1]
        self._hazards(o, reads, writes)
        self.ops[q].append(o)
        return o

    def barrier(self):
        last = []
        for e in COMPUTE:
            for o in reversed(self.ops[e]):
                if o.fn is not None and not o.is_dma:
                    o.needed = True
                    last.append(o)
                    break
        dmas = []
        for i in range(self.pool_idx):
            s, c = self.dma_pool[i]
            if c > 0:
                d = Op('sync', None, True)
                d.sem = s
                d.val = c
                dmas.append(d)
        for q in QUEUES:
            o = Op(q, None, False)
            o.deps = [d for d in last if d.q != q] + dmas
            self.ops[q].append(o)
        for b in self.live:
            b.sem = None
        self.live = []
        self.pool_idx = 0

    def emit(self):
        nc = self.nc
        for e in COMPUTE:
            c = 0
            for o in self.ops[e]:
                if o.needed and not o.is_dma:
                    c += 1
                    o.sem = self.esem[e]
                    o.val = c
        with nc.Block() as block:
            for q in QUEUES:
                lst = self.ops[q]
                if not lst:
                    continue

                def body(eng, lst=lst):
                    waited = {}
                    for o in lst:
                        for d in o.deps:
                            k = id(d.sem)
                            if waited.get(k, 0) >= d.val:
                                continue
                            waited[k] = d.val
                            eng.wait_ge(d.sem, d.val)
                        if o.fn is None:
                            continue
                        ins = o.fn(eng)
                        if o.is_dma:
                            ins.then_inc(o.sem, 16)
                        elif o.needed:
                            ins.then_inc(o.sem, 1)

                getattr(block, q)(body)

    def V(self, name, reads=(), writes=(), **kw):
        return self.op('vector', lambda e: getattr(e, name)(**kw), reads, writes)

    def A(self, name, reads=(), writes=(), **kw):
        return self.op('scalar', lambda e: getattr(e, name)(**kw), reads, writes)

    def G(self, name, reads=(), writes=(), **kw):
        return self.op('gpsimd', lambda e: getattr(e, name)(**kw), reads, writes)

    def MM(self, reads=(), writes=(), **kw):
        return self.op('tensor', lambda e: e.matmul(**kw), reads, writes)

    def TR(self, reads=(), writes=(), **kw):
        return self.op('tensor', lambda e: e.transpose(**kw), reads, writes)


def build_nc(depth=DEPTH, debug=False, stop_after=None):
    _BREG.clear()
    nc = bass.Bass("TRN2", target_bir_lowering=False)
    dbg_set = set(debug) if debug else set()

    def scr(name, shape, dt):
        return nc.dram_tensor(name, list(shape), dt, kind=("ExternalOutput" if name in dbg_set else "Internal")).ap()

    def din(name, shape, dt=F32):
        return nc.dram_tensor(name, list(shape), dt, kind="ExternalInput").ap()

    xin = din("xin", [T, D])
    w_in = din("w_in", [DEPTH, D, 10752])
    pool_w = din("pool_w", [DEPTH, 4, 256, 256])
    lru_wa = din("lru_wa", [DEPTH, 4, 256, 256])
    lru_wx = din("lru_wx", [DEPTH, 4, 256, 256])
    proj = [din("proj_pool", [DEPTH, 1024, D]), din("proj_attn", [DEPTH, 1024, D]), din("proj_lru", [DEPTH, 1024, D])]
    w_out = din("w_out", [DEPTH, D, D])
    rw_tab = din("rw_tab", [DEPTH, 128, DC * 36])
    rbias = din("rbias", [DEPTH, 1, 36])
    ewg = din("exp_w_gate", [DEPTH, NEXP, D, 512])
    ewu = din("exp_w_up", [DEPTH, NEXP, D, 512])
    ewd = din("exp_w_down", [DEPTH, NEXP, 512, D])
    smallp = din("smallp", [DEPTH, 128, SP_N])
    embp = din("embp", [128, 32])
    c_ab = din("c_ab", [128, 16, 256])
    c_pm = din("c_pm", [128, 256])
    c_invcnt = din("c_invcnt", [4, 128, T])
    c_identf = din("c_identf", [128, 128])
    c_identb = din("c_identb", [128, 128], BF16)
    c_tri = din("c_tri", [128, 128], BF16)
    c_thr = din("c_thr", [128, NSB])
    c_piota = din("c_piota", [128, 1])
    ln2g_bc = din("ln2g_bc", [DEPTH, 128, D])
    ln2b_bc = din("ln2b_bc", [DEPTH, 128, D])

    out = nc.dram_tensor("out", [SEQ, D], F32, kind="ExternalOutput").ap()
    hres = scr("hres", [NTILES, 128, DC, NT], F32)
    hbf = scr("hbf", [NTILES, 128, DC, NT], BF16)
    cols = scr("cols", [NTILES, 128, NCH_COLS, NT], F32)
    mix = scr("mix", [NTILES, 128, 24, NT], BF16)
    merged = scr("merged", [NTILES, 128, DC, NT], BF16)
    h1tm_f = scr("h1tm_f", [T, D], F32)
    h1tm_b = scr("h1tm_b", [T, D], BF16)
    xs = scr("xs", [CAP, D], BF16)
    yb = scr("yb", [CAP, D], F32)

    def rowap(x, c, p0=0, p1=128):
        return x.rearrange("t p c n -> p t c n")[p0:p1, :, c, :]

    def rowview(ap2d):
        return ap2d.rearrange("p (t n) -> p t n", n=NT)

    with contextlib.ExitStack() as st:
        P = Prog(nc)
        P.setup(st)
        psf = [Tile(st.enter_context(nc.psum_tensor("psf%d" % i, [128, 512], F32)), "psf%d" % i) for i in range(6)]
        psb = [Tile(st.enter_context(nc.psum_tensor("psb%d" % i, [128, 1024], BF16)), "psb%d" % i) for i in range(2)]
        psi = [0]

        def next_ps():
            psi[0] += 1
            return psf[psi[0] % 6]

        identf = P.sbuf("identf", [128, 128], F32)
        identb = P.sbuf("identb", [128, 128], BF16)
        onesf = P.sbuf("onesf", [128, 128], F32)
        embt = P.sbuf("embt", [128, 32], F32)
        sp = P.sbuf("sp", [128, SP_N], F32)
        lamc = P.sbuf("lamc", [128, 16], F32)
        Lall = P.sbuf("Lall", [128, NBLK, 36], F32)
        dest_i = P.sbuf("dest_i", [128, NBLK, 2], I32)
        cw = P.sbuf("cw", [128, NBLK, 2], F32)
        widx = P.sbuf("widx", [128, NSB, 16], I32)
        didx = P.sbuf("didx", [128, NSB, 4], I32)
        P.persist_mark()
        P.dma('sync', identf[:], c_identf, writes=[identf], key=identf)
        P.dma('sync', identb[:], c_identb, writes=[identb], key=identb)
        P.dma('sync', embt[:], embp, writes=[embt], key=embt)
        P.V('memset', writes=[onesf], ap=onesf[:], constant=1.0)

        evac_i = [0]

        def evac(out_ap, in_ap, reads, writes):
            evac_i[0] += 1
            if evac_i[0] % 2 == 0:
                P.A('copy', reads=reads, writes=writes, out=out_ap, in_=in_ap)
            else:
                P.V('tensor_copy', reads=reads, writes=writes, out=out_ap, in_=in_ap)

        def emit_ln(hz, n, gcol, bcol, tmp, zero_cols=0, hb=None):
            ps1 = next_ps()
            ps2 = next_ps()
            for mc in range(DC):
                P.MM(reads=[onesf, hz], writes=[ps1], out=ps1[:, 0:n], lhsT=onesf[:], rhs=hz[:, mc, 0:n],
                     start=(mc == 0), stop=(mc == DC - 1))
            for mc in range(DC):
                zs = tmp['zsq'][mc % 2]
                P.A('activation', reads=[hz], writes=[zs], out=zs[:, 0:n], in_=hz[:, mc, 0:n], func=AF.Square)
                P.MM(reads=[onesf, zs], writes=[ps2], out=ps2[:, 0:n], lhsT=onesf[:], rhs=zs[:, 0:n],
                     start=(mc == 0), stop=(mc == DC - 1))
            mean, rstd = tmp['mean'], tmp['rstd']
            P.A('mul', reads=[ps1], writes=[mean], out=mean[:, 0:n], in_=ps1[:, 0:n], mul=1.0 / D)
            P.V('tensor_tensor', reads=[mean], writes=[rstd], out=rstd[:, 0:n], in0=mean[:, 0:n], in1=mean[:, 0:n], op=ALU.mult)
            P.V('scalar_tensor_tensor', reads=[ps2, rstd], writes=[rstd], out=rstd[:, 0:n], in0=ps2[:, 0:n], scalar=1.0 / D,
                in1=rstd[:, 0:n], op0=ALU.mult, op1=ALU.subtract)
            P.V('tensor_scalar', reads=[rstd], writes=[rstd], out=rstd[:, 0:n], in0=rstd[:, 0:n], scalar1=0.0, scalar2=EPS,
                op0=ALU.max, op1=ALU.add)
            P.A('activation', reads=[rstd], writes=[rstd], out=rstd[:, 0:n], in_=rstd[:, 0:n], func=AF.Sqrt)
            P.V('reciprocal', reads=[rstd], writes=[rstd], out=rstd[:, 0:n], in_=rstd[:, 0:n])
            for mc in range(DC):
                P.V('tensor_tensor', reads=[hz, mean], writes=[hz], out=hz[:, mc, 0:n], in0=hz[:, mc, 0:n], in1=mean[:, 0:n], op=ALU.subtract)
                P.V('tensor_tensor', reads=[hz, rstd], writes=[hz], out=hz[:, mc, 0:n], in0=hz[:, mc, 0:n], in1=rstd[:, 0:n], op=ALU.mult)
                P.A('activation', reads=[hz, sp, embt], writes=[hz], out=hz[:, mc, 0:n], in_=hz[:, mc, 0:n], func=AF.Identity,
                    scale=gcol(mc), bias=bcol(mc))
            if zero_cols:
                P.V('memset', writes=[hz], ap=hz[:, :, 0:zero_cols], constant=0.0)
            if hb is not None:
                P.G('tensor_copy', reads=[hz], writes=[hb], out=hb[:, :, 0:n], in_=hz[:, :, 0:n])

        def phase_embed():
            P.phase_begin()
            xt = [P.sbuf("xt", [128, D], F32) for _ in range(2)]
            xn = [P.sbuf("xn", [128, D], F32) for _ in range(2)]
            stt = P.sbuf("stt", [128, 4, 6], F32)
            mv = P.sbuf("mv", [128, 2], F32)
            rs = P.sbuf("rs", [128, 1], F32)
            hs = [P.sbuf("hs", [128, DC, 128], F32) for _ in range(2)]
            hsb = [P.sbuf("hsb", [128, DC, 128], BF16) for _ in range(2)]
            for blk in range(NBLK):
                x_ = xt[blk % 2]
                n_ = xn[blk % 2]
                h_ = hs[blk % 2]
                hb_ = hsb[blk % 2]
                P.dma('sync', x_[:], xin[blk * 128:(blk + 1) * 128, :], writes=[x_], key=x_)
                for j in range(4):
                    P.V('bn_stats', reads=[x_], writes=[stt], out=stt[:, j, :], in_=x_[:, j * 512:(j + 1) * 512])
                P.V('bn_aggr', reads=[stt], writes=[mv], out=mv[:], in_=stt[:].rearrange("p a b -> p (a b)"))
                P.V('tensor_scalar', reads=[mv], writes=[rs], out=rs[:], in0=mv[:, 1:2], scalar1=EPS, scalar2=None, op0=ALU.add)
                P.A('activation', reads=[rs], writes=[rs], out=rs[:], in_=rs[:], func=AF.Sqrt)
                P.V('reciprocal', reads=[rs], writes=[rs], out=rs[:], in_=rs[:])
                P.V('tensor_scalar', reads=[x_, mv, rs], writes=[n_], out=n_[:], in0=x_[:], scalar1=mv[:, 0:1], scalar2=rs[:, 0:1],
                    op0=ALU.subtract, op1=ALU.mult)
                import os
                CUT = int(os.environ.get('EMBED_CUT', '9'))
                for c4 in range(4):
                    ps = next_ps()
                    for cc in range(4):
                        c = c4 * 4 + cc
                        if CUT >= 1:
                            P.TR(reads=[n_, identf], writes=[ps], out=ps[:, cc * 128:(cc + 1) * 128], in_=n_[:, c * 128:(c + 1) * 128],
                                 identity=identf[:])
                    for cc in range(4):
                        c = c4 * 4 + cc
                        if CUT >= 2:
                            P.A('activation', reads=[ps, embt], writes=[h_], out=h_[:, c, :], in_=ps[:, cc * 128:(cc + 1) * 128],
                                func=AF.Identity, scale=embt[:, c:c + 1], bias=embt[:, 16 + c:17 + c])
                        else:
                            P.V('tensor_copy', reads=[n_], writes=[h_], out=h_[:, c, :], in_=n_[:, c * 128:(c + 1) * 128])
                if blk == 0:
                    P.V('memset', writes=[h_], ap=h_[:, :, 0:PAD], constant=0.0)
                P.V('tensor_copy', reads=[h_], writes=[hb_], out=hb_[:], in_=h_[:])
                tl, off = blk // 3, (blk % 3) * 128
                P.dma('sync', hres[tl, :, :, off:off + 128], h_[:], reads=[h_], key=h_)
                P.dma('sync', hbf[tl, :, :, off:off + 128], hb_[:], reads=[hb_], key=hb_)
            P.barrier()

        def phase_win(l):
            P.phase_begin()
            wb = [P.sbuf("wb", [128, DC, 512], BF16) for _ in range(2)]
            hball = [P.sbuf("hball", [128, DC, NT], BF16) for _ in range(NTILES)]
            ot = [P.sbuf("ot", [128, 4, NT], F32) for _ in range(2)]
            wsrc = w_in[l].rearrange("(kc p) m -> p kc m", p=128)
            it = 0
            def load_w(mg):
                w_ = wb[mg % 2]
                for half in range(2):
                    P.dma('gpsimd', w_[:, half * 8:(half + 1) * 8, :], wsrc[:, half * 8:(half + 1) * 8, mg * 512:(mg + 1) * 512],
                          writes=[w_], key=w_)
            load_w(0)
            for tl in range(NTILES):
                P.dma('sync', hball[tl][:], hbf[tl], writes=[hball[tl]], key=hball[tl])
            for mg in range(21):
                w_ = wb[mg % 2]
                if mg + 1 < 21:
                    load_w(mg + 1)
                for tl in range(NTILES):
                    o_ = ot[it % 2]
                    it += 1
                    for mc in range(4):
                        ps = next_ps()
                        for kc in range(DC):
                            P.MM(reads=[w_, hball[tl]], writes=[ps], out=ps[:, 0:NT], lhsT=w_[:, kc, mc * 128:(mc + 1) * 128], rhs=hball[tl][:, kc, :],
                                 start=(kc == 0), stop=(kc == DC - 1))
                        evac(o_[:, mc, :], ps[:, 0:NT], [ps], [o_])
                    P.dma('gpsimd', cols[tl, :, mg * 4:(mg + 1) * 4, :], o_[:], reads=[o_], key=o_)
            P.barrier()

        def phase_pool(l):
            P.phase_begin()
            U = P.sbuf("U", [128, 16 + T], F32)
            Wk = [P.sbuf("Wk", [128, 16 + T], F32) for _ in range(2)]
            icn = P.sbuf("icn", [128, T], F32)
            dlt = [P.sbuf("dlt", [128, T], BF16) for _ in range(2)]
            po = P.sbuf("po", [128, T], BF16)
            pw = P.sbuf("pw", [128, 2, 256], BF16)
            P.V('memset', writes=[U], ap=U[:, 0:16], constant=0.0)
            for w_ in Wk:
                P.V('memset', writes=[w_], ap=w_[:, 0:16], constant=0.0)
            for g in range(4):
                P.dma('sync', icn[:], c_invcnt[g], writes=[icn], key=icn)
                P.dma('gpsimd', pw[:], pool_w[l, g].rearrange("(kc p) m -> p kc m", p=128), writes=[pw], key=pw)
                for half in range(2):
                    c = 2 * g + half
                    P.dma('sync', rowview(U[:, 16:16 + T]), rowap(cols, C_POOL + c), writes=[U], key=U)
                    src = U
                    for j in range(g + 1):
                        sh = 1 << j
                        dst = Wk[j % 2]
                        P.V('tensor_tensor', reads=[src], writes=[dst], out=dst[:, 16:16 + T], in0=src[:, 16:16 + T],
                            in1=src[:, 16 - sh:16 - sh + T], op=ALU.add)
                        src = dst
                    other = Wk[(g + 1) % 2]
                    P.V('tensor_tensor', reads=[src, icn], writes=[other], out=other[:, 16:16 + T], in0=src[:, 16:16 + T], in1=icn[:],
                        op=ALU.mult)
                    P.V('tensor_tensor', reads=[other, U], writes=[dlt[half]], out=dlt[half][:], in0=other[:, 16:16 + T],
                        in1=U[:, 16:16 + T], op=ALU.subtract)
                for mo in range(2):
                    co = 2 * g + mo
                    for tl in range(NTILES):
                        ps = next_ps()
                        for kc in range(2):
                            P.MM(reads=[pw, dlt[kc]], writes=[ps], out=ps[:, 0:NT], lhsT=pw[:, kc, mo * 128:(mo + 1) * 128],
                                 rhs=dlt[kc][:, tl * NT:(tl + 1) * NT], start=(kc == 0), stop=(kc == 1))
                        P.A('activation', reads=[ps, sp], writes=[po], out=po[:, tl * NT:(tl + 1) * NT], in_=ps[:, 0:NT], func=AF.Identity,
                            scale=sp[:, SP_PSC + co:SP_PSC + co + 1])
                    P.dma('gpsimd', rowap(mix, co), rowview(po[:]), reads=[po], key=po)
            P.barrier()

        def phase_lru(l):
            P.phase_begin()
            X = P.sbuf("X", [128, 3 + T], F32)
            XC = [P.sbuf("XC", [128, T], F32) for _ in range(2)]
            xcb = [P.sbuf("xcb", [128, T], BF16) for _ in range(2)]
            GA = P.sbuf("GA", [128, T], F32)
            GX = P.sbuf("GX", [128, T], F32)
            AR = P.sbuf("AR", [128, T], F32)
            TB = P.sbuf("TB", [128, T], F32)
            lo = P.sbuf("lo", [128, T], BF16)
            wa = P.sbuf("wa", [128, 2, 256], BF16)
            wx = P.sbuf("wx", [128, 2, 256], BF16)
            P.A('activation', reads=[sp], writes=[lamc], out=lamc[:, 0:8], in_=sp[:, SP_LAM:SP_LAM + 8], func=AF.Exp, scale=-1.0)
            P.A('activation', reads=[lamc], writes=[lamc], out=lamc[:, 0:8], in_=lamc[:, 0:8], func=AF.Ln, bias=1.0)
            P.A('mul', reads=[lamc], writes=[lamc], out=lamc[:, 8:16], in_=lamc[:, 0:8], mul=-16.0)
            P.A('mul', reads=[lamc], writes=[lamc], out=lamc[:, 0:8], in_=lamc[:, 0:8], mul=-8.0)
            P.V('memset', writes=[X], ap=X[:, 0:3], constant=0.0)
            for blk in range(4):
                P.dma('gpsimd', wa[:], lru_wa[l, blk].rearrange("(kc p) m -> p kc m", p=128), writes=[wa], key=wa)
                P.dma('gpsimd', wx[:], lru_wx[l, blk].rearrange("(kc p) m -> p kc m", p=128), writes=[wx], key=wx)
                for half in range(2):
                    c = 2 * blk + half
                    xc = XC[half]
                    P.dma('sync', rowview(X[:, 3:3 + T]), rowap(cols, C_LX + c), writes=[X], key=X)
                    P.V('tensor_scalar', reads=[X, sp], writes=[xc], out=xc[:], in0=X[:, 0:T], scalar1=sp[:, SP_CW + c:SP_CW + c + 1],
                        scalar2=sp[:, SP_CB + c:SP_CB + c + 1], op0=ALU.mult, op1=ALU.add)
                    for j in range(1, 4):
                        P.V('scalar_tensor_tensor', reads=[X, sp, xc], writes=[xc], out=xc[:], in0=X[:, j:j + T],
                            scalar=sp[:, SP_CW + j * 8 + c:SP_CW + j * 8 + c + 1], in1=xc[:], op0=ALU.mult, op1=ALU.add)
                    P.G('tensor_copy', reads=[xc], writes=[xcb[half]], out=xcb[half][:], in_=xc[:])
                for mo in range(2):
                    co = 2 * blk + mo
                    for tl in range(NTILES):
                        sl = slice(tl * NT, (tl + 1) * NT)
                        psa = next_ps()
                        for kc in range(2):
                            P.MM(reads=[wa, xcb[kc]], writes=[psa], out=psa[:, 0:NT], lhsT=wa[:, kc, mo * 128:(mo + 1) * 128],
                                 rhs=xcb[kc][:, sl], start=(kc == 0), stop=(kc == 1))
                        P.A('activation', reads=[psa, sp], writes=[GA], out=GA[:, sl], in_=psa[:, 0:NT], func=AF.Sigmoid,
                            bias=sp[:, SP_BA + co:SP_BA + co + 1])
                        psx = next_ps()
                        for kc in range(2):
                            P.MM(reads=[wx, xcb[kc]], writes=[psx], out=psx[:, 0:NT], lhsT=wx[:, kc, mo * 128:(mo + 1) * 128],
                                 rhs=xcb[kc][:, sl], start=(kc == 0), stop=(kc == 1))
                        P.A('activation', reads=[psx, sp], writes=[GX], out=GX[:, sl], in_=psx[:, 0:NT], func=AF.Sigmoid,
                            bias=sp[:, SP_BX + co:SP_BX + co + 1])
                    P.A('activation', reads=[GA, lamc], writes=[AR], out=AR[:], in_=GA[:], func=AF.Exp, scale=lamc[:, co:co + 1])
                    P.A('activation', reads=[GA, lamc], writes=[TB], out=TB[:], in_=GA[:], func=AF.Exp, scale=lamc[:, 8 + co:9 + co])
                    P.V('tensor_scalar', reads=[TB], writes=[TB], out=TB[:], in0=TB[:], scalar1=-1.0, scalar2=1.0, op0=ALU.mult, op1=ALU.add)
                    P.V('tensor_scalar', reads=[TB], writes=[TB], out=TB[:], in0=TB[:], scalar1=0.0, scalar2=None, op0=ALU.max)
                    P.A('activation', reads=[TB], writes=[TB], out=TB[:], in_=TB[:], func=AF.Sqrt)
                    P.V('tensor_tensor', reads=[TB, GX], writes=[TB], out=TB[:], in0=TB[:], in1=GX[:], op=ALU.mult)
                    P.V('tensor_tensor', reads=[TB, XC[mo]], writes=[TB], out=TB[:], in0=TB[:], in1=XC[mo][:], op=ALU.mult)
                    P.V('memset', writes=[TB], ap=TB[:, 0:PAD], constant=0.0)
                    P.V('tensor_tensor_scan', reads=[AR, TB], writes=[GA], out=GA[:], data0=AR[:], data1=TB[:], initial=0.0,
                        op0=ALU.mult, op1=ALU.add)
                    P.dma('sync', rowview(GX[:]), rowap(cols, C_LY + co), writes=[GX], key=GX)
                    P.A('activation', reads=[GX], writes=[GX], out=GX[:], in_=GX[:], func=AF.Gelu)
                    P.V('tensor_tensor', reads=[GA, GX], writes=[lo], out=lo[:], in0=GA[:], in1=GX[:], op=ALU.mult)
                    P.dma('gpsimd', rowap(mix, 16 + co), rowview(lo[:]), reads=[lo], key=lo)
            P.barrier()

        def phase_attn(l):
            P.phase_begin()
            AB = P.sbuf("AB", [128, 16, 256], F32)
            PM = P.sbuf("PM", [128, 256], F32)
            VT = P.sbuf("VT", [128, NBLK + 1, 256], BF16)
            kT = P.sbuf("kT", [64, 4, 128 + T], BF16)
            vrow = P.sbuf("vrow", [128, T], BF16)
            qT = [P.sbuf("qT", [64, T], BF16) for _ in range(2)]
            AO = [P.sbuf("AO", [128, T], BF16) for _ in range(2)]
            ssb = [P.sbuf("ssb", [128, 256], F32) for _ in range(4)]
            pn = [P.sbuf("pn", [128, 256], BF16) for _ in range(4)]
            pT = [P.sbuf("pT", [128, 256], BF16) for _ in range(4)]
            sm = [P.sbuf("sm", [128, 8], F32) for _ in range(4)]
            psS = [psf[0], psf[1]]
            psO = [psf[2], psf[3]]
            P.dma('sync', AB[:], c_ab, writes=[AB], key=AB)
            P.dma('sync', PM[:], c_pm, writes=[PM], key=PM)
            P.V('memset', writes=[VT], ap=VT[:, 0, :], constant=0.0)
            P.V('memset', writes=[kT], ap=kT[:, :, 0:128], constant=0.0)
            for j in range(4):
                P.dma('gpsimd', rowview(kT[0:64, j, 128:128 + T]), rowap(cols, C_K + j // 2, (j % 2) * 64, (j % 2) * 64 + 64),
                      writes=[kT], key=kT)
            for vc in range(2):
                P.dma('gpsimd', rowview(vrow[:]), rowap(cols, C_V + vc), writes=[vrow], key=vrow)
                for blk in range(NBLK):
                    pb = psb[blk % 2]
                    P.TR(reads=[vrow, identb], writes=[pb], out=pb[:, 0:128], in_=vrow[:, blk * 128:(blk + 1) * 128], identity=identb[:])
                    evac(VT[:, blk + 1, vc * 128:(vc + 1) * 128], pb[:, 0:128], [pb], [VT])
            P.barrier()

            class Reg:
                def __init__(self, ap):
                    self.ap = ap
                    self.buf = Buf("reg")

            DEP = 4
            rS = [Reg(psf[k // 2][:, (k % 2) * 256:(k % 2) * 256 + 256]) for k in range(4)]
            rO = [psf[2][:, k * 128:(k + 1) * 128] for k in range(4)]
            rOb = [Reg(None) for _ in range(4)]
            rB = [Reg(psb[k // 4][:, (k % 4) * 256:(k % 4) * 256 + 256]) for k in range(8)]
            iters = [(h, blk) for h in range(16) for blk in range(NBLK)]
            NI = len(iters)

            def stageA(i):
                h, blk = iters[i]
                j = h // 4
                q_ = qT[h % 2]
                p0 = (h % 2) * 64
                if blk == 0:
                    P.dma('gpsimd', rowview(q_[0:64, :]), rowap(cols, C_Q + h // 2, p0, p0 + 64), writes=[q_], key=q_)
                s_, sm_, pS = ssb[i % DEP], sm[i % DEP], rS[i % 4]
                P.MM(reads=[q_, kT], writes=[pS], out=pS.ap, lhsT=q_[0:64, blk * 128:(blk + 1) * 128],
                     rhs=kT[0:64, j, blk * 128:blk * 128 + 256], start=True, stop=True)
                P.V('scalar_tensor_tensor', reads=[pS, AB], writes=[s_], out=s_[:], in0=pS.ap, scalar=0.125, in1=AB[:, h, :],
                    op0=ALU.mult, op1=ALU.add)
                if blk == 0:
                    P.V('tensor_tensor', reads=[s_, PM], writes=[s_], out=s_[:], in0=s_[:], in1=PM[:], op=ALU.add)
                if blk == 1:
                    P.V('tensor_tensor', reads=[s_, PM], writes=[s_], out=s_[:, 0:PAD], in0=s_[:, 0:PAD], in1=PM[:, 0:PAD], op=ALU.add)
                P.V('reduce_max', reads=[s_], writes=[sm_], out=sm_[:, 0:1], in_=s_[:], axis=AX.X)
                P.V('tensor_scalar', reads=[sm_, sp], writes=[sm_], out=sm_[:, 1:2], in0=sm_[:, 0:1],
                    scalar1=sp[:, SP_SINK + h:SP_SINK + h + 1], scalar2=-1.0, op0=ALU.max, op1=ALU.mult)

            def stageB_act(i):
                h, blk = iters[i]
                s_, sm_ = ssb[i % DEP], sm[i % DEP]
                P.A('activation', reads=[s_, sm_], writes=[s_, sm_], out=s_[:], in_=s_[:], func=AF.Exp, bias=sm_[:, 1:2],
                    accum_out=sm_[:, 2:3])
                P.A('activation', reads=[sp, sm_], writes=[sm_], out=sm_[:, 3:4], in_=sp[:, SP_SINK + h:SP_SINK + h + 1], func=AF.Exp,
                    bias=sm_[:, 1:2])

            def stageB_dve(i):
                s_, sm_, pn_ = ssb[i % DEP], sm[i % DEP], pn[i % DEP]
                P.V('tensor_tensor', reads=[sm_], writes=[sm_], out=sm_[:, 4:5], in0=sm_[:, 2:3], in1=sm_[:, 3:4], op=ALU.add)
                P.V('reciprocal', reads=[sm_], writes=[sm_], out=sm_[:, 5:6], in_=sm_[:, 4:5])
                P.V('tensor_scalar', reads=[s_, sm_], writes=[pn_], out=pn_[:], in0=s_[:], scalar1=sm_[:, 5:6], scalar2=None, op0=ALU.mult)

            def stageC1(i):
                pn_, pT_, pb = pn[i % DEP], pT[i % DEP], rB[i % 8]
                for kb in range(2):
                    P.TR(reads=[pn_, identb], writes=[pb], out=pb.ap[:, kb * 128:(kb + 1) * 128], in_=pn_[:, kb * 128:(kb + 1) * 128],
                         identity=identb[:])
                P.A('copy', reads=[pb], writes=[pT_], out=pT_[:], in_=pb.ap)

            def stageC2(i):
                h, blk = iters[i]
                j = h // 4
                p0 = (h % 2) * 64
                ao = AO[(h // 2) % 2]
                pT_, pO, pOb = pT[i % DEP], rO[i % 4], rOb[i % 4]
                for kb in range(2):
                    P.MM(reads=[VT, pT_], writes=[pOb], out=pO[p0:p0 + 64, :], lhsT=VT[:, blk + kb, j * 64:(j + 1) * 64],
                         rhs=pT_[:, kb * 128:(kb + 1) * 128], start=(kb == 0), stop=(kb == 1))
                P.A('copy', reads=[pOb], writes=[ao], out=ao[p0:p0 + 64, blk * 128:(blk + 1) * 128], in_=pO[p0:p0 + 64, :])
                if h % 2 == 1 and blk == NBLK - 1:
                    P.dma('gpsimd', rowap(mix, 8 + h // 2), rowview(ao[:]), reads=[ao], key=ao)

            for step in range(NI + 3):
                if step - 1 >= 0 and step - 1 < NI:
                    stageB_act(step - 1)
                if step - 3 >= 0 and step - 3 < NI:
                    stageC2(step - 3)
                if step < NI:
                    stageA(step)
                if step - 2 >= 0 and step - 2 < NI:
                    stageC1(step - 2)
                if step - 1 >= 0 and step - 1 < NI:
                    stageB_dve(step - 1)
            P.barrier()

        def phase_merge(l):
            P.phase_begin()
            pw = [[P.sbuf("pjw", [128, 8, 512], BF16) for _ in range(3)] for _ in range(2)]
            mx = [P.sbuf("mx", [128, 24, NT], BF16) for _ in range(2)]
            gt = [P.sbuf("gt", [128, 3, 4, NT], F32) for _ in range(2)]
            acc = [P.sbuf("acc", [128, NT], F32) for _ in range(2)]
            tm = [P.sbuf("tm", [128, NT], F32) for _ in range(2)]
            mt = [P.sbuf("mt", [128, 4, NT], BF16) for _ in range(2)]
            it = 0
            def load_pw(mg):
                for i in range(3):
                    P.dma('gpsimd', pw[mg % 2][i][:], proj[i][l].rearrange("(kc p) m -> p kc m", p=128)[:, :, mg * 512:(mg + 1) * 512],
                          writes=[pw[mg % 2][i]], key=pw[mg % 2][i])
            load_pw(0)
            for mg in range(4):
                pws = pw[mg % 2]
                if mg + 1 < 4:
                    load_pw(mg + 1)
                for tl in range(NTILES):
                    mx_, gt_, mt_ = mx[it % 2], gt[it % 2], mt[it % 2]
                    it += 1
                    P.dma('sync', mx_[:], mix[tl], writes=[mx_], key=mx_)
                    for i in range(3):
                        c0 = C_G + i * 16 + mg * 4
                        P.dma('sync', gt_[:, i, :, :], cols[tl, :, c0:c0 + 4, :], writes=[gt_], key=gt_)
                    P.A('activation', reads=[gt_], writes=[gt_], out=gt_[:].rearrange("p a b n -> p (a b n)"), in_=gt_[:].rearrange("p a b n -> p (a b n)"), func=AF.Sigmoid)
                    for mc in range(4):
                        ac, t_ = acc[mc % 2], tm[mc % 2]
                        for i in range(3):
                            ps = next_ps()
                            for kc in range(8):
                                P.MM(reads=[pws[i], mx_], writes=[ps], out=ps[:, 0:NT], lhsT=pws[i][:, kc, mc * 128:(mc + 1) * 128],
                                     rhs=mx_[:, i * 8 + kc, :], start=(kc == 0), stop=(kc == 7))
                            if i == 0:
                                P.V('tensor_tensor', reads=[ps, gt_], writes=[ac], out=ac[:], in0=ps[:, 0:NT], in1=gt_[:, i, mc, :], op=ALU.mult)
                            else:
                                P.V('tensor_tensor', reads=[ps, gt_], writes=[t_], out=t_[:], in0=ps[:, 0:NT], in1=gt_[:, i, mc, :], op=ALU.mult)
                                if i == 1:
                                    P.V('tensor_tensor', reads=[ac, t_], writes=[ac], out=ac[:], in0=ac[:], in1=t_[:], op=ALU.add)
                                else:
                                    P.V('tensor_tensor', reads=[ac, t_], writes=[mt_], out=mt_[:, mc, :], in0=ac[:], in1=t_[:], op=ALU.add)
                    P.dma('gpsimd', merged[tl, :, mg * 4:(mg + 1) * 4, :], mt_[:], reads=[mt_], key=mt_)
            P.barrier()

        def phase_wout(l):
            P.phase_begin()
            wo = P.sbuf("wo", [128, DC, D], BF16)
            mg_ = [P.sbuf("mgd", [128, DC, NT], BF16) for _ in range(2)]
            hz = [P.sbuf("hz", [128, DC, NT], F32) for _ in range(2)]
            tmp = {'zsq': [P.sbuf("zsq", [128, NT], F32) for _ in range(2)], 'mean': P.sbuf("mean", [128, NT], F32),
                   'rstd': P.sbuf("rstd", [128, NT], F32)}
            tmf = [P.sbuf("tmf", [128, D], F32) for _ in range(2)]
            tmb = [P.sbuf("tmb", [128, D], BF16) for _ in range(2)]
            wr = P.sbuf("wr", [128, DC, 36], F32)
            rb = P.sbuf("rb", [1, 36], F32)
            P.dma('sync', wr[:].rearrange("p a b -> p (a b)"), rw_tab[l], writes=[wr], key=wr)
            P.dma('sync', rb[:], rbias[l], writes=[rb], key=rb)
            wsrc = w_out[l].rearrange("(kc p) m -> p kc m", p=128)
            for q4 in range(4):
                for half in range(2):
                    P.dma('gpsimd', wo[:, half * 8:(half + 1) * 8, q4 * 512:(q4 + 1) * 512],
                          wsrc[:, half * 8:(half + 1) * 8, q4 * 512:(q4 + 1) * 512], writes=[wo], key=wo)
            for tl in range(NTILES):
                m_, z_ = mg_[tl % 2], hz[tl % 2]
                P.dma('sync', m_[:], merged[tl], writes=[m_], key=m_)
                P.dma('sync', z_[:], hres[tl], writes=[z_], key=z_)
                for mc in range(DC):
                    ps = next_ps()
                    for kc in range(DC):
                        P.MM(reads=[wo, m_], writes=[ps], out=ps[:, 0:NT], lhsT=wo[:, kc, mc * 128:(mc + 1) * 128], rhs=m_[:, kc, :],
                             start=(kc == 0), stop=(kc == DC - 1))
                    P.V('scalar_tensor_tensor', reads=[z_, ps], writes=[z_], out=z_[:, mc, :], in0=z_[:, mc, :], scalar=ALPHA, in1=ps[:, 0:NT],
                        op0=ALU.mult, op1=ALU.add)
                emit_ln(z_, NT, lambda mc: sp[:, SP_LN1G + mc:SP_LN1G + mc + 1], lambda mc: sp[:, SP_LN1B + mc:SP_LN1B + mc + 1], tmp,
                        zero_cols=(PAD if tl == 0 else 0), hb=None)
                for sb3 in range(3):
                    blk = tl * 3 + sb3
                    tf, tb = tmf[blk % 2], tmb[blk % 2]
                    pl = next_ps()
                    for kc in range(DC):
                        P.MM(reads=[z_, wr], writes=[pl], out=pl[:, 0:36], lhsT=z_[:, kc, sb3 * 128:(sb3 + 1) * 128], rhs=wr[:, kc, :],
                             start=(kc == 0), stop=False)
                    P.MM(reads=[onesf, rb], writes=[pl], out=pl[:, 0:36], lhsT=onesf[0:1, :], rhs=rb[0:1, :], start=False, stop=True)
                    P.V('tensor_copy', reads=[pl], writes=[Lall], out=Lall[:, blk, :], in_=pl[:, 0:36])
                    for c4 in range(4):
                        ps = next_ps()
                        for cc in range(4):
                            c = c4 * 4 + cc
                            P.TR(reads=[z_, identf], writes=[ps], out=ps[:, cc * 128:(cc + 1) * 128],
                                 in_=z_[:, c, sb3 * 128:(sb3 + 1) * 128], identity=identf[:])
                        evac(tf[:, c4 * 512:(c4 + 1) * 512], ps[:, 0:512], [ps], [tf])
                    P.G('tensor_copy', reads=[tf], writes=[tb], out=tb[:], in_=tf[:])
                    P.dma('gpsimd', h1tm_f[blk * 128:(blk + 1) * 128, :], tf[:], reads=[tf], key=tf)
                    P.dma('gpsimd', h1tm_b[blk * 128:(blk + 1) * 128, :], tb[:], reads=[tb], key=tb)
            P.barrier()

        def phase_router(l):
            P.phase_begin()
            ELm = [P.sbuf("ELm", [128, 32], F32) for _ in range(2)]
            sc = [P.sbuf("sc", [128, 32], F32) for _ in range(2)]
            M1a = P.sbuf("M1a", [128, NBLK, 32], F32)
            M2a = P.sbuf("M2a", [128, NBLK, 32], F32)
            M12b = P.sbuf("M12b", [128, NBLK, 32], BF16)
            onesb = P.sbuf("onesb", [128, 128], BF16)
            trib = P.sbuf("trib", [128, 128], BF16)
            thr = P.sbuf("thr", [128, NSB], F32)
            pio = P.sbuf("pio", [128, 1], F32)
            cnt = P.sbuf("cnt", [128, 32], F32)
            nbk = P.sbuf("nbk", [128, 32], F32)
            pend = P.sbuf("pend", [128, 32], F32)
            pstart = P.sbuf("pstart", [128, 32], F32)
            Dm = [P.sbuf("Dm", [128, 32], F32) for _ in range(2)]
            tt = [P.sbuf("tt", [128, 32], F32) for _ in range(2)]
            destf = P.sbuf("destf", [128, NBLK, 2], F32)
            be = P.sbuf("be", [128, NSB], F32)
            chg = P.sbuf("chg", [128, NSB], F32)
            gb = P.sbuf("gb", [128, NSB], F32)
            db = P.sbuf("db", [128, NSB], F32)
            widf = P.sbuf("widf", [128, NSB, 16], F32)
            didf = P.sbuf("didf", [128, NSB, 4], F32)
            padfix = P.sbuf("padfix", [128, 2], F32)
            P.dma('sync', trib[:], c_tri, writes=[trib], key=trib)
            P.dma('sync', thr[:], c_thr, writes=[thr], key=thr)
            P.dma('sync', pio[:], c_piota, writes=[pio], key=pio)
            P.V('memset', writes=[onesb], ap=onesb[:], constant=1.0)
            for blk in range(NBLK):
                E_, s_ = ELm[blk % 2], sc[blk % 2]
                L_ = Lall
                P.V('reduce_max', reads=[L_], writes=[s_], out=s_[:, 0:1], in_=Lall[:, blk, 0:4], axis=AX.X)
                P.V('tensor_scalar', reads=[s_], writes=[s_], out=s_[:, 1:2], in0=s_[:, 0:1], scalar1=-1.0, scalar2=None, op0=ALU.mult)
                P.A('activation', reads=[L_, s_], writes=[s_], out=s_[:, 24:28], in_=Lall[:, blk, 0:4], func=AF.Exp, bias=s_[:, 1:2],
                    accum_out=s_[:, 2:3])
                P.V('reciprocal', reads=[s_], writes=[s_], out=s_[:, 3:4], in_=s_[:, 2:3])
                P.V('tensor_scalar', reads=[L_, s_], writes=[s_], out=s_[:, 4:8], in0=Lall[:, blk, 0:4], scalar1=s_[:, 0:1], scalar2=None,
                    op0=ALU.is_equal)
                P.V('tensor_scalar', reads=[s_], writes=[s_], out=s_[:, 4:8], in0=s_[:, 4:8], scalar1=-1.0, scalar2=1e30, op0=ALU.add,
                    op1=ALU.mult)
                for g in range(4):
                    P.V('tensor_scalar', reads=[L_, s_], writes=[E_], out=E_[:, g * 8:(g + 1) * 8], in0=Lall[:, blk, 4 + g * 8:12 + g * 8],
                        scalar1=s_[:, 4 + g:5 + g], scalar2=None, op0=ALU.add)
                P.V('max', reads=[E_], writes=[s_], out=s_[:, 8:16], in_=E_[:])
                P.V('tensor_tensor', reads=[s_], writes=[s_], out=s_[:, 16:17], in0=s_[:, 8:9], in1=s_[:, 9:10], op=ALU.subtract)
                P.A('activation', reads=[s_], writes=[s_], out=s_[:, 17:18], in_=s_[:, 16:17], func=AF.Sigmoid)
                P.V('tensor_scalar', reads=[s_], writes=[s_], out=s_[:, 18:19], in0=s_[:, 17:18], scalar1=-1.0, scalar2=1.0, op0=ALU.mult,
                    op1=ALU.add)
                P.V('tensor_scalar', reads=[s_], writes=[cw], out=cw[:, blk, :], in0=s_[:, 17:19], scalar1=s_[:, 3:4], scalar2=None,
                    op0=ALU.mult)
                P.V('tensor_scalar', reads=[E_, s_], writes=[M1a], out=M1a[:, blk, :], in0=E_[:], scalar1=s_[:, 8:9], scalar2=None,
                    op0=ALU.is_equal)
                P.V('tensor_scalar', reads=[E_, s_], writes=[M2a], out=M2a[:, blk, :], in0=E_[:], scalar1=s_[:, 9:10], scalar2=None,
                    op0=ALU.is_equal)
                if blk == 0:
                    P.V('memset', writes=[M1a], ap=M1a[0:PAD, 0, :], constant=0.0)
                    P.V('memset', writes=[M2a], ap=M2a[0:PAD, 0, :], constant=0.0)
                P.V('tensor_tensor', reads=[M1a, M2a], writes=[M12b], out=M12b[:, blk, :], in0=M1a[:, blk, :], in1=M2a[:, blk, :], op=ALU.add)
            pc = next_ps()
            for blk in range(NBLK):
                P.MM(reads=[onesb, M12b], writes=[pc], out=pc[:, 0:32], lhsT=onesb[:], rhs=M12b[:, blk, :], start=(blk == 0),
                     stop=(blk == NBLK - 1))
            P.V('tensor_copy', reads=[pc], writes=[cnt], out=cnt[:], in_=pc[:, 0:32])
            P.V('memset', writes=[nbk], ap=nbk[:], constant=0.0)
            for k in range(34):
                P.V('scalar_tensor_tensor', reads=[cnt, nbk], writes=[nbk], out=nbk[:], in0=cnt[:], scalar=float(128 * k), in1=nbk[:],
                    op0=ALU.is_gt, op1=ALU.add)
            P.V('tensor_scalar', reads=[nbk], writes=[nbk], out=nbk[:], in0=nbk[:], scalar1=128.0, scalar2=None, op0=ALU.mult)
            P.V('tensor_tensor_scan', reads=[onesf, nbk], writes=[pend], out=pend[:], data0=onesf[:, 0:32], data1=nbk[:], initial=0.0,
                op0=ALU.mult, op1=ALU.add)
            P.V('tensor_tensor', reads=[pend, nbk], writes=[pstart], out=pstart[:], in0=pend[:], in1=nbk[:], op=ALU.subtract)
            for blk in range(NBLK):
                pC = next_ps()
                P.MM(reads=[trib, M12b], writes=[pC], out=pC[:, 0:32], lhsT=trib[:], rhs=M12b[:, blk, :], start=True, stop=(blk == 0))
                for j in range(blk):
                    P.MM(reads=[onesb, M12b], writes=[pC], out=pC[:, 0:32], lhsT=onesb[:], rhs=M12b[:, j, :], start=False, stop=(j == blk - 1))
                D_, t_ = Dm[blk % 2], tt[blk % 2]
                P.V('tensor_tensor', reads=[pC, pstart], writes=[D_], out=D_[:], in0=pC[:, 0:32], in1=pstart[:], op=ALU.add)
                for k, Ma in enumerate((M1a, M2a)):
                    P.V('tensor_tensor', reads=[Ma, D_], writes=[t_], out=t_[:], in0=Ma[:, blk, :], in1=D_[:], op=ALU.mult)
                    P.V('reduce_sum', reads=[t_], writes=[destf], out=destf[:, blk, k:k + 1], in_=t_[:], axis=AX.X)
            P.V('reduce_sum', reads=[M1a], writes=[padfix], out=padfix[:, 0:1], in_=M1a[:, 0, :], axis=AX.X)
            P.V('tensor_scalar', reads=[padfix], writes=[padfix], out=padfix[:, 1:2], in0=padfix[:, 0:1], scalar1=-BIGI, scalar2=BIGI,
                op0=ALU.mult, op1=ALU.add)
            for k in range(2):
                P.V('tensor_tensor', reads=[destf, padfix], writes=[destf], out=destf[:, 0, k:k + 1], in0=destf[:, 0, k:k + 1],
                    in1=padfix[:, 1:2], op=ALU.add)
            P.V('tensor_copy', reads=[destf], writes=[dest_i], out=dest_i[:], in_=destf[:])
            P.V('memset', writes=[be], ap=be[:], constant=0.0)
            for e in range(NEXP):
                P.V('scalar_tensor_tensor', reads=[thr, pend, be], writes=[be], out=be[:], in0=thr[:], scalar=pend[:, e:e + 1], in1=be[:],
                    op0=ALU.is_ge, op1=ALU.add)
            P.V('tensor_scalar', reads=[be], writes=[be], out=be[:], in0=be[:], scalar1=31.0, scalar2=None, op0=ALU.min)
            P.V('memset', writes=[chg], ap=chg[:, 0:1], constant=1.0)
            P.V('tensor_tensor', reads=[be], writes=[chg], out=chg[:, 1:NSB], in0=be[:, 1:NSB], in1=be[:, 0:NSB - 1], op=ALU.not_equal)
            for (dst, mul) in ((gb, 128.0), (db, 128.0)):
                P.V('tensor_scalar', reads=[be], writes=[dst], out=dst[:], in0=be[:], scalar1=mul, scalar2=float(l * NEXP) * mul - BIGI, op0=ALU.mult, op1=ALU.add)
                P.V('tensor_tensor', reads=[dst, chg], writes=[dst], out=dst[:], in0=dst[:], in1=chg[:], op=ALU.mult)
                P.V('tensor_scalar', reads=[dst], writes=[dst], out=dst[:], in0=dst[:], scalar1=BIGI, scalar2=None, op0=ALU.add)
                P.V('tensor_scalar', reads=[dst, pio], writes=[dst], out=dst[:], in0=dst[:], scalar1=pio[:, 0:1], scalar2=None, op0=ALU.add)
            for kc in range(16):
                P.V('tensor_scalar', reads=[gb], writes=[widf], out=widf[:, :, kc], in0=gb[:], scalar1=float(kc * 128), scalar2=None, op0=ALU.add)
            for kc in range(4):
                P.V('tensor_scalar', reads=[db], writes=[didf], out=didf[:, :, kc], in0=db[:], scalar1=float(kc * 128), scalar2=None, op0=ALU.add)
            P.V('tensor_copy', reads=[widf], writes=[widx], out=widx[:], in_=widf[:])
            P.V('tensor_copy', reads=[didf], writes=[didx], out=didx[:], in_=didf[:])
            P.barrier()

        def phase_moe(l, last):
            P.phase_begin()
            xsrc = [P.sbuf("xsrc", [128, D], BF16) for _ in range(2)]
            for blk in range(NBLK):
                x_ = xsrc[blk % 2]
                P.dma('sync', x_[:], h1tm_b[blk * 128:(blk + 1) * 128, :], writes=[x_], key=x_)
                for k in range(2):
                    P.dma_fn('gpsimd', lambda e, x_=x_, blk=blk, k=k: e.indirect_dma_start(
                        out=xs[:, :], out_offset=bass.IndirectOffsetOnAxis(ap=dest_i[:, blk, k:k + 1], axis=0), in_=x_[:, :], in_offset=None,
                        bounds_check=_breg(e, CAP - 1), oob_is_err=False), reads=[x_, dest_i], key=x_)
            P.barrier()
            P.phase_begin()
            xsb = [P.sbuf("xsb", [128, D], BF16) for _ in range(2)]
            xT = [P.sbuf("xT", [128, D], BF16) for _ in range(2)]
            wg = P.sbuf("wg", [128, DC, 512], BF16)
            wu = P.sbuf("wu", [128, DC, 512], BF16)
            wd = P.sbuf("wd", [128, 4, D], BF16)
            sgt = [P.sbuf("sgt", [128, 512], F32) for _ in range(2)]
            hdt = [P.sbuf("hdt", [128, 512], BF16) for _ in range(2)]
            hdT = [P.sbuf("hdT", [128, 512], BF16) for _ in range(2)]
            yblk = [P.sbuf("yblk", [128, D], F32) for _ in range(2)]
            wst = [P.sbuf("wst", [128, 8192], F32) for _ in range(3)]
            gsrc = ewg.rearrange("l e (p kc) m -> (l e p) (kc m)", kc=16)
            usrc = ewu.rearrange("l e (p kc) m -> (l e p) (kc m)", kc=16)
            dsrc = ewd.rearrange("l e (p kc) m -> (l e p) (kc m)", kc=4)
            wbound = (l + 1) * NEXP * 128 - 1
            for b in range(NSB):
                x_, xT_, sg_, hd_, hT_, y_ = xsb[b % 2], xT[b % 2], sgt[b % 2], hdt[b % 2], hdT[b % 2], yblk[b % 2]
                P.dma('sync', x_[:], xs[b * 128:(b + 1) * 128, :], writes=[x_], key=x_)
                prev = []
                for (stg, src) in zip(wst, (gsrc, usrc, dsrc)):
                    P.dma_fn('gpsimd', lambda e, stg=stg, src=src, b=b: e.indirect_dma_start(
                        out=stg[:, :], out_offset=None, in_=src[:, :],
                        in_offset=bass.IndirectOffsetOnAxis(ap=widx[:, b, 0:1], axis=0), bounds_check=_breg(e, wbound), oob_is_err=False),
                        reads=[widx] + prev, writes=[stg], key=stg)
                    prev = [stg]
                P.A('copy', reads=[wst[0]], writes=[wg], out=wg[:].rearrange("p a b -> p (a b)"), in_=wst[0][:])
                P.V('tensor_copy', reads=[wst[1]], writes=[wu], out=wu[:].rearrange("p a b -> p (a b)"), in_=wst[1][:])
                P.A('copy', reads=[wst[2]], writes=[wd], out=wd[:, 0:2, :].rearrange("p a b -> p (a b)"), in_=wst[2][:, 0:4096])
                P.V('tensor_copy', reads=[wst[2]], writes=[wd], out=wd[:, 2:4, :].rearrange("p a b -> p (a b)"), in_=wst[2][:, 4096:8192])
                for half in range(2):
                    pb = psb[half]
                    for c8 in range(8):
                        c = half * 8 + c8
                        P.TR(reads=[x_, identb], writes=[pb], out=pb[:, c8 * 128:(c8 + 1) * 128], in_=x_[:, c:D:16],
                             identity=identb[:])
                    evac(xT_[:, half * 1024:(half + 1) * 1024], pb[:, 0:1024], [pb], [xT_])
                pg = next_ps()
                for kc in range(DC):
                    P.MM(reads=[xT_, wg], writes=[pg], out=pg[:, 0:512], lhsT=xT_[:, kc * 128:(kc + 1) * 128], rhs=wg[:, kc, :],
                         start=(kc == 0), stop=(kc == DC - 1))
                pu = next_ps()
                for kc in range(DC):
                    P.MM(reads=[xT_, wu], writes=[pu], out=pu[:, 0:512], lhsT=xT_[:, kc * 128:(kc + 1) * 128], rhs=wu[:, kc, :],
                         start=(kc == 0), stop=(kc == DC - 1))
                P.A('activation', reads=[pg], writes=[sg_], out=sg_[:], in_=pg[:, 0:512], func=AF.Silu)
                P.V('tensor_tensor', reads=[sg_, pu], writes=[hd_], out=hd_[:], in0=sg_[:], in1=pu[:, 0:512], op=ALU.mult)
                pb = psb[b % 2]
                for kc in range(4):
                    P.TR(reads=[hd_, identb], writes=[pb], out=pb[:, kc * 128:(kc + 1) * 128], in_=hd_[:, kc:512:4],
                         identity=identb[:])
                evac(hT_[:], pb[:, 0:512], [pb], [hT_])
                for fg in range(4):
                    py = next_ps()
                    for kc in range(4):
                        P.MM(reads=[hT_, wd], writes=[py], out=py[:, 0:512], lhsT=hT_[:, kc * 128:(kc + 1) * 128],
                             rhs=wd[:, kc, fg * 512:(fg + 1) * 512], start=(kc == 0), stop=(kc == 3))
                    evac(y_[:, fg * 512:(fg + 1) * 512], py[:, 0:512], [py], [y_])
                P.dma('sync', yb[b * 128:(b + 1) * 128, :], y_[:], reads=[y_], key=y_)
            P.barrier()
            P.phase_begin()
            G1 = [P.sbuf("G1", [128, D], F32) for _ in range(2)]
            G2 = [P.sbuf("G2", [128, D], F32) for _ in range(2)]
            h1t = [P.sbuf("h1t", [128, D], F32) for _ in range(2)]
            stt = P.sbuf("stt", [128, 4, 6], F32)
            mv = P.sbuf("mv", [128, 2], F32)
            rs = P.sbuf("rs", [128, 1], F32)
            if last:
                gbc = P.sbuf("gbc", [128, D], F32)
                bbc = P.sbuf("bbc", [128, D], F32)
                P.dma('sync', gbc[:], ln2g_bc[l], writes=[gbc], key=gbc)
                P.dma('sync', bbc[:], ln2b_bc[l], writes=[bbc], key=bbc)
            else:
                hs = [P.sbuf("hs", [128, DC, 128], F32) for _ in range(2)]
                hsb = [P.sbuf("hsb", [128, DC, 128], BF16) for _ in range(2)]
            for g_ in G1 + G2:
                P.V('memset', writes=[g_], ap=g_[:], constant=0.0)
            for blk in range(NBLK):
                z_, g2_, h_ = G1[blk % 2], G2[blk % 2], h1t[blk % 2]
                for k, gt_ in enumerate((z_, g2_)):
                    P.dma_fn('gpsimd', lambda e, gt_=gt_, blk=blk, k=k: e.indirect_dma_start(
                        out=gt_[:, :], out_offset=None, in_=yb[:, :], in_offset=bass.IndirectOffsetOnAxis(ap=dest_i[:, blk, k:k + 1], axis=0),
                        bounds_check=_breg(e, CAP - 1), oob_is_err=False), reads=[dest_i], writes=[gt_], key=gt_)
                P.dma('sync', h_[:], h1tm_f[blk * 128:(blk + 1) * 128, :], writes=[h_], key=h_)
                P.V('tensor_scalar', reads=[z_, cw], writes=[z_], out=z_[:], in0=z_[:], scalar1=cw[:, blk, 0:1], scalar2=None, op0=ALU.mult)
                P.V('scalar_tensor_tensor', reads=[g2_, cw, z_], writes=[z_], out=z_[:], in0=g2_[:], scalar=cw[:, blk, 1:2], in1=z_[:],
                    op0=ALU.mult, op1=ALU.add)
                P.V('scalar_tensor_tensor', reads=[h_, z_], writes=[z_], out=z_[:], in0=h_[:], scalar=ALPHA, in1=z_[:], op0=ALU.mult,
                    op1=ALU.add)
                for j in range(4):
                    P.V('bn_stats', reads=[z_], writes=[stt], out=stt[:, j, :], in_=z_[:, j * 512:(j + 1) * 512])
                P.V('bn_aggr', reads=[stt], writes=[mv], out=mv[:], in_=stt[:].rearrange("p a b -> p (a b)"))
                P.V('tensor_scalar', reads=[mv], writes=[rs], out=rs[:], in0=mv[:, 1:2], scalar1=EPS, scalar2=None, op0=ALU.add)
                P.A('activation', reads=[rs], writes=[rs], out=rs[:], in_=rs[:], func=AF.Sqrt)
                P.V('reciprocal', reads=[rs], writes=[rs], out=rs[:], in_=rs[:])
                P.V('tensor_scalar', reads=[z_, mv, rs], writes=[z_], out=z_[:], in0=z_[:], scalar1=mv[:, 0:1], scalar2=rs[:, 0:1],
                    op0=ALU.subtract, op1=ALU.mult)
                if last:
                    if blk == 0:
                        continue
                    P.V('tensor_tensor', reads=[z_, gbc], writes=[z_], out=z_[:], in0=z_[:], in1=gbc[:], op=ALU.mult)
                    P.V('tensor_tensor', reads=[z_, bbc], writes=[z_], out=z_[:], in0=z_[:], in1=bbc[:], op=ALU.add)
                    P.dma('sync', out[(blk - 1) * 128:blk * 128, :], z_[:], reads=[z_], key=z_)
                else:
                    ho, hb_ = hs[blk % 2], hsb[blk % 2]
                    for c4 in range(4):
                        ps = next_ps()
                        for cc in range(4):
                            c = c4 * 4 + cc
                            P.TR(reads=[z_, identf], writes=[ps], out=ps[:, cc * 128:(cc + 1) * 128], in_=z_[:, c * 128:(c + 1) * 128],
                                 identity=identf[:])
                        for cc in range(4):
                            c = c4 * 4 + cc
                            P.A('activation', reads=[ps, sp], writes=[ho], out=ho[:, c, :], in_=ps[:, cc * 128:(cc + 1) * 128],
                                func=AF.Identity, scale=sp[:, SP_LN2G + c:SP_LN2G + c + 1], bias=sp[:, SP_LN2B + c:SP_LN2B + c + 1])
                    if blk == 0:
                        P.V('memset', writes=[ho], ap=ho[:, :, 0:PAD], constant=0.0)
                    P.V('tensor_copy', reads=[ho], writes=[hb_], out=hb_[:], in_=ho[:])
                    tl, off = blk // 3, (blk % 3) * 128
                    P.dma('sync', hres[tl, :, :, off:off + 128], ho[:], reads=[ho], key=ho)
                    P.dma('sync', hbf[tl, :, :, off:off + 128], hb_[:], reads=[hb_], key=hb_)
            P.barrier()

        class _View:
            def __init__(self, tile, off):
                self.tile = tile
                self.off = off
                self.buf = tile.buf

            def __getitem__(self, idx):
                p, c, n = idx
                assert isinstance(n, slice)
                n0 = (n.start or 0) + self.off
                n1 = (n.stop if n.stop is not None else NT) + self.off
                return self.tile.t[p, c, n0:n1]

        phases = []
        phase_embed()
        done = (stop_after == 'embed')
        for l in range(depth):
            if done:
                break
            P.dma('sync', sp[:], smallp[l], writes=[sp], key=sp)
            for name, fn in (('win', phase_win), ('pool', phase_pool), ('lru', phase_lru), ('attn', phase_attn), ('merge', phase_merge),
                             ('wout', phase_wout), ('router', phase_router)):
                fn(l)
                if stop_after == "%s%d" % (name, l):
                    done = True
                    break
            if done:
                break
            phase_moe(l, last=(l == depth - 1))
        P.emit()
    return nc


def _is_tile_like(x):
    return hasattr(x, 'buf')


def _tab(v, n):
    return np.ascontiguousarray(np.asarray(v, np.float32).reshape(n, 128).T)


def make_consts():
    q = np.arange(128)[:, None]
    s = np.arange(256)[None, :]
    dist = (128 + q - s).astype(np.float32)
    inwin = (dist >= 0) & (dist < 128)
    slopes = (2.0 ** (-8.0 * np.arange(1, 17, dtype=np.float32) / 16)).astype(np.float32)
    ab = np.where(inwin[:, None, :], -slopes[None, :, None] * dist[:, None, :], np.float32(NEG)).astype(np.float32)
    pm = np.where(s < 128 + PAD, np.float32(NEG), np.float32(0.0)).astype(np.float32) * np.ones((128, 1), np.float32)
    invc = np.ones((4, 128, T), np.float32)
    tt = np.arange(T) - PAD
    for g, w in enumerate((2, 4, 8, 16)):
        cnt = np.where(tt >= 0, np.minimum(tt + 1, w), 1).astype(np.float32)
        invc[g] = (1.0 / cnt)[None, :]
    tri = (np.arange(128)[:, None] < np.arange(128)[None, :]).astype(np.float32).astype(ml_dtypes.bfloat16)
    thr = np.ascontiguousarray(np.broadcast_to((128.0 * np.arange(NSB, dtype=np.float32))[None, :], (128, NSB)))
    piota = np.arange(128, dtype=np.float32)[:, None].copy()
    return {
        "c_tri": tri, "c_thr": thr, "c_piota": piota,
        "c_ab": np.ascontiguousarray(ab), "c_pm": np.ascontiguousarray(pm), "c_invcnt": invc,
        "c_identf": np.eye(128, dtype=np.float32), "c_identb": np.eye(128, dtype=np.float32).astype(ml_dtypes.bfloat16),
    }


def make_inputs(inp, b):
    f = lambda k: np.ascontiguousarray(np.asarray(inp[k], np.float32))
    xin = np.zeros((T, D), np.float32)
    xin[PAD:PAD + NMETA] = np.asarray(inp['meta'], np.float32)
    xin[PAD + NMETA:] = np.asarray(inp['x'][b], np.float32)
    smallp = np.zeros((DEPTH, 128, SP_N), np.float32)
    for l in range(DEPTH):
        smallp[l, :, SP_LN1G:SP_LN1G + 16] = _tab(inp['ln1_g'][l], 16)
        smallp[l, :, SP_LN1B:SP_LN1B + 16] = _tab(inp['ln1_b'][l], 16)
        smallp[l, :, SP_LN2G:SP_LN2G + 16] = _tab(inp['ln2_g'][l], 16)
        smallp[l, :, SP_LN2B:SP_LN2B + 16] = _tab(inp['ln2_b'][l], 16)
        smallp[l, :, SP_PSC:SP_PSC + 8] = _tab(inp['pool_scale'][l], 8)
        for j in range(4):
            smallp[l, :, SP_CW + j * 8:SP_CW + j * 8 + 8] = _tab(inp['conv_w'][l][j], 8)
        smallp[l, :, SP_CB:SP_CB + 8] = _tab(inp['conv_b'][l], 8)
        smallp[l, :, SP_BA:SP_BA + 8] = _tab(inp['lru_ba'][l], 8)
        smallp[l, :, SP_BX:SP_BX + 8] = _tab(inp['lru_bx'][l], 8)
        smallp[l, :, SP_LAM:SP_LAM + 8] = _tab(inp['lru_lambda'][l], 8)
        smallp[l, :, SP_SINK:SP_SINK + 16] = np.asarray(inp['attn_sink'][l], np.float32)[None, :]
    embp = np.concatenate([_tab(inp['ln_emb_g'], 16), _tab(inp['ln_emb_b'], 16)], axis=1)
    rbias = np.concatenate([np.asarray(inp['router_grp_b'], np.float32), np.asarray(inp['router_exp_b'], np.float32)], axis=1)[:, None, :]
    rcat = np.concatenate([np.asarray(inp['router_grp_w'], np.float32), np.asarray(inp['router_exp_w'], np.float32)], axis=2)
    rw_tab = np.ascontiguousarray(rcat.reshape(DEPTH, DC, 128, 36).transpose(0, 2, 1, 3).reshape(DEPTH, 128, DC * 36))
    m = {
        "xin": xin, "w_in": f('w_in'), "pool_w": f('pool_w'), "lru_wa": f('lru_wa'), "lru_wx": f('lru_wx'),
        "proj_pool": f('proj_pool'), "proj_attn": f('proj_attn'), "proj_lru": f('proj_lru'), "w_out": f('w_out'),
        "rw_tab": rw_tab, "rbias": np.ascontiguousarray(rbias),
        "exp_w_gate": f('exp_w_gate'), "exp_w_up": f('exp_w_up'), "exp_w_down": f('exp_w_down'),
        "smallp": smallp, "embp": np.ascontiguousarray(embp),
        "ln2g_bc": np.ascontiguousarray(np.broadcast_to(np.asarray(inp['ln2_g'], np.float32)[:, None, :], (DEPTH, 128, D))),
        "ln2b_bc": np.ascontiguousarray(np.broadcast_to(np.asarray(inp['ln2_b'], np.float32)[:, None, :], (DEPTH, 128, D))),
    }
    m.update(make_consts())
    return m


_NC_CACHE = {}


def kernel(**inputs):
    B = inputs['x'].shape[0]
    if 'nc' not in _NC_CACHE:
        _NC_CACHE['nc'] = build_nc()
    nc = _NC_CACHE['nc']
    shared = None
    in_maps = []
    for b in range(B):
        m = make_inputs(inputs, b) if shared is None else dict(shared, xin=None)
        if shared is None:
            shared = m
        else:
            xin = np.zeros((T, D), np.float32)
            xin[PAD:PAD + NMETA] = np.asarray(inputs['meta'], np.float32)
            xin[PAD + NMETA:] = np.asarray(inputs['x'][b], np.float32)
            m['xin'] = xin
        in_maps.append(m)
    res = run_bass_kernel_spmd(nc, in_maps, core_ids=list(range(B)))
    return np.stack([np.asarray(r["out"], np.float32) for r in res.results], axis=0)
```

```python
import contextlib
import numpy as np
import ml_dtypes
import concourse.bass as bass
import concourse.mybir as mybir
from concourse.bass_utils import run_bass_kernel_spmd

F32 = mybir.dt.float32
BF16 = mybir.dt.bfloat16
AF = mybir.ActivationFunctionType
ALU = mybir.AluOpType
AX = mybir.AxisListType

COMPUTE = ('scalar', 'vector', 'tensor', 'gpsimd')
QUEUES = ('sync', 'scalar', 'vector', 'tensor', 'gpsimd')
DT_SIZE = {F32: 4, BF16: 2, mybir.dt.int32: 4}

D = 2048
DC = 16
SEQ = 4096
NMETA = 16
PAD = 112
T = 4224
NT = 384
NTILES = 11
NBLK = 33
DEPTH = 2
NCH_COLS = 84
C_POOL, C_Q, C_K, C_V, C_LX, C_LY, C_G = 0, 8, 16, 18, 20, 28, 36
NEXP = 32
ALPHA = (2.0 * DEPTH) ** 0.25
EPS = 1e-5
NEG = -1e30
NSB = 97
CAP = NSB * 128
BIGI = 1048576.0
I32 = mybir.dt.int32
SP_LN1G, SP_LN1B, SP_LN2G, SP_LN2B = 0, 16, 32, 48
SP_PSC, SP_CW, SP_CB, SP_BA, SP_BX, SP_LAM, SP_SINK = 64, 72, 104, 112, 120, 128, 136
SP_N = 152


class Buf:
    __slots__ = ('name', 'w', 'r', 'sem')

    def __init__(self, name):
        self.name = name
        self.w = None
        self.r = []
        self.sem = None


class Op:
    __slots__ = ('q', 'fn', 'deps', 'is_dma', 'sem', 'val', 'needed')

    def __init__(self, q, fn, is_dma):
        self.q = q
        self.fn = fn
        self.deps = []
        self.is_dma = is_dma
        self.sem = None
        self.val = 0
        self.needed = False


class Tile:
    __slots__ = ('t', 'buf')

    def __init__(self, t, name):
        self.t = t
        self.buf = Buf(name)

    def __getitem__(self, idx):
        return self.t[idx]


def _b(x):
    return getattr(x, 'buf', x)


_BREG = {}


def _breg(e, val):
    k = (id(e), int(val))
    if k not in _BREG:
        _BREG[k] = e.to_reg(int(val))
    return _BREG[k]


class Prog:
    SB_LO = 20480
    SB_HI = 222 * 1024

    def __init__(self, nc, n_dma_sems=56):
        self.nc = nc
        self.ops = {q: [] for q in QUEUES}
        self.esem = {}
        self.dma_pool = []
        self.n_dma_sems = n_dma_sems
        self.pool_idx = 0
        self.sb_off = self.SB_LO
        self.sb_base = self.SB_LO
        self.sb_max = 0
        self.uid = 0
        self.live = []

    def setup(self, stack):
        for e in COMPUTE:
            self.esem[e] = stack.enter_context(self.nc.semaphore("es_" + e))
        for i in range(self.n_dma_sems):
            self.dma_pool.append([stack.enter_context(self.nc.semaphore("ds%d" % i)), 0])

    def sbuf(self, name, shape, dtype):
        nbytes = int(np.prod(shape[1:])) * DT_SIZE[dtype]
        off = (self.sb_off + 63) // 64 * 64
        self.uid += 1
        t = self.nc.alloc_sbuf_tensor_at("%s_%d" % (name, self.uid), list(shape), dtype, offset=off)
        self.sb_off = off + nbytes
        self.sb_max = max(self.sb_max, self.sb_off)
        assert self.sb_off <= self.SB_HI, ("SBUF overflow", name, self.sb_off)
        return Tile(t, name)

    def phase_begin(self):
        self.sb_off = self.sb_base

    def persist_mark(self):
        self.sb_base = self.sb_off

    def _hazards(self, op, reads, writes):
        deps = []
        strong = set()
        for b in reads:
            b = _b(b)
            if b.w is not None:
                deps.append(b.w)
                strong.add(id(b.w))
        for b in writes:
            b = _b(b)
            if b.w is not None:
                deps.append(b.w)
                strong.add(id(b.w))
            deps.extend(b.r)
        for b in reads:
            _b(b).r.append(op)
        for b in writes:
            b = _b(b)
            b.w = op
            b.r = []
        seen = set()
        for d in deps:
            if d is op or id(d) in seen:
                continue
            seen.add(id(d))
            if (not d.is_dma) and (not op.is_dma) and d.q == op.q:
                if d.q == 'tensor' or id(d) not in strong:
                    continue
            if not d.is_dma:
                d.needed = True
            op.deps.append(d)

    def op(self, eng, fn, reads=(), writes=()):
        o = Op(eng, fn, False)
        self._hazards(o, reads, writes)
        self.ops[eng].append(o)
        return o

    def dma(self, q, out, in_, reads=(), writes=(), key=None):
        o = Op(q, (lambda e, out=out, in_=in_: e.dma_start(out=out, in_=in_)), True)
        b = _b(key)
        if b.sem is None:
            assert self.pool_idx < len(self.dma_pool), "out of dma sems"
            b.sem = self.dma_pool[self.pool_idx]
            self.pool_idx += 1
            self.live.append(b)
        b.sem[1] += 16
        o.sem = b.sem[0]
        o.val = b.sem[1]
        self._hazards(o, reads, writes)
        self.ops[q].append(o)
        return o

    def dma_fn(self, q, fn, reads=(), writes=(), key=None):
        o = Op(q, fn, True)
        b = _b(key)
        if b.sem is None:
            assert self.pool_idx < len(self.dma_pool), "out of dma sems"
            b.sem = self.dma_pool[self.pool_idx]
            self.pool_idx += 1
            self.live.append(b)
        b.sem[1] += 16
        o.sem = b.sem[0]
        o.val = b.sem[1]
        self._hazards(o, reads, writes)
        self.ops[q].append(o)
        return o

    def barrier(self):
        last = []
        for e in COMPUTE:
            for o in reversed(self.ops[e]):
                if o.fn is not None and not o.is_dma:
                    o.needed = True
                    last.append(o)
                    break
        dmas = []
        for i in range(self.pool_idx):
            s, c = self.dma_pool[i]
            if c > 0:
                d = Op('sync', None, True)
                d.sem = s
                d.val = c
                dmas.append(d)
        for q in QUEUES:
            o = Op(q, None, False)
            o.deps = [d for d in last if d.q != q] + dmas
            self.ops[q].append(o)
        for b in self.live:
            b.sem = None
        self.live = []
        self.pool_idx = 0

    def emit(self):
        nc = self.nc
        for e in COMPUTE:
            c = 0
            for o in self.ops[e]:
                if o.needed and not o.is_dma:
                    c += 1
                    o.sem = self.esem[e]
                    o.val = c
        with nc.Block() as block:
            for q in QUEUES:
                lst = self.ops[q]
                if not lst:
                    continue

                def body(eng, lst=lst):
                    waited = {}
                    for o in lst:
                        for d in o.deps:
                            k = id(d.sem)
                            if waited.get(k, 0) >= d.val:
                                continue
                            waited[k] = d.val
                            eng.wait_ge(d.sem, d.val)
                        if o.fn is None:
                            continue
                        ins = o.fn(eng)
                        if o.is_dma:
                            ins.then_inc(o.sem, 16)
                        elif o.needed:
                            ins.then_inc(o.sem, 1)

                getattr(block, q)(body)

    def V(self, name, reads=(), writes=(), **kw):
        return self.op('vector', lambda e: getattr(e, name)(**kw), reads, writes)

    def A(self, name, reads=(), writes=(), **kw):
        return self.op('scalar', lambda e: getattr(e, name)(**kw), reads, writes)

    def G(self, name, reads=(), writes=(), **kw):
        return self.op('gpsimd', lambda e: getattr(e, name)(**kw), reads, writes)

    def MM(self, reads=(), writes=(), **kw):
        return self.op('tensor', lambda e: e.matmul(**kw), reads, writes)

    def TR(self, reads=(), writes=(), **kw):
        return self.op('tensor', lambda e: e.transpose(**kw), reads, writes)


def build_nc(depth=DEPTH, debug=False, stop_after=None):
    _BREG.clear()
    nc = bass.Bass("TRN2", target_bir_lowering=False)
    dbg_set = set(debug) if debug else set()

    def scr(name, shape, dt):
        return nc.dram_tensor(name, list(shape), dt, kind=("ExternalOutput" if name in dbg_set else "Internal")).ap()

    def din(name, shape, dt=F32):
        return nc.dram_tensor(name, list(shape), dt, kind="ExternalInput").ap()

    xin = din("xin", [T, D])
    w_in = din("w_in", [DEPTH, D, 10752])
    pool_w = din("pool_w", [DEPTH, 4, 256, 256])
    lru_wa = din("lru_wa", [DEPTH, 4, 256, 256])
    lru_wx = din("lru_wx", [DEPTH, 4, 256, 256])
    proj = [din("proj_pool", [DEPTH, 1024, D]), din("proj_attn", [DEPTH, 1024, D]), din("proj_lru", [DEPTH, 1024, D])]
    w_out = din("w_out", [DEPTH, D, D])
    rw_tab = din("rw_tab", [DEPTH, 128, DC * 36])
    rbias = din("rbias", [DEPTH, 1, 36])
    ewg = din("exp_w_gate", [DEPTH, NEXP, D, 512])
    ewu = din("exp_w_up", [DEPTH, NEXP, D, 512])
    ewd = din("exp_w_down", [DEPTH, NEXP, 512, D])
    smallp = din("smallp", [DEPTH, 128, SP_N])
    embp = din("embp", [128, 32])
    c_ab = din("c_ab", [128, 16, 256])
    c_pm = din("c_pm", [128, 256])
    c_invcnt = din("c_invcnt", [4, 128, T])
    c_identf = din("c_identf", [128, 128])
    c_identb = din("c_identb", [128, 128], BF16)
    c_tri = din("c_tri", [128, 128], BF16)
    c_thr = din("c_thr", [128, NSB])
    c_piota = din("c_piota", [128, 1])
    ln2g_bc = din("ln2g_bc", [DEPTH, 128, D])
    ln2b_bc = din("ln2b_bc", [DEPTH, 128, D])

    out = nc.dram_tensor("out", [SEQ, D], F32, kind="ExternalOutput").ap()
    hres = scr("hres", [NTILES, 128, DC, NT], F32)
    hbf = scr("hbf", [NTILES, 128, DC, NT], BF16)
    cols = scr("cols", [NTILES, 128, NCH_COLS, NT], F32)
    mix = scr("mix", [NTILES, 128, 24, NT], BF16)
    merged = scr("merged", [NTILES, 128, DC, NT], BF16)
    h1tm_f = scr("h1tm_f", [T, D], F32)
    h1tm_b = scr("h1tm_b", [T, D], BF16)
    xs = scr("xs", [CAP, D], BF16)
    yb = scr("yb", [CAP, D], F32)

    def rowap(x, c, p0=0, p1=128):
        return x.rearrange("t p c n -> p t c n")[p0:p1, :, c, :]

    def rowview(ap2d):
        return ap2d.rearrange("p (t n) -> p t n", n=NT)

    with contextlib.ExitStack() as st:
        P = Prog(nc)
        P.setup(st)
        psf = [Tile(st.enter_context(nc.psum_tensor("psf%d" % i, [128, 512], F32)), "psf%d" % i) for i in range(6)]
        psb = [Tile(st.enter_context(nc.psum_tensor("psb%d" % i, [128, 1024], BF16)), "psb%d" % i) for i in range(2)]
        psi = [0]

        def next_ps():
            psi[0] += 1
            return psf[psi[0] % 6]

        identf = P.sbuf("identf", [128, 128], F32)
        identb = P.sbuf("identb", [128, 128], BF16)
        onesf = P.sbuf("onesf", [128, 128], F32)
        embt = P.sbuf("embt", [128, 32], F32)
        sp = P.sbuf("sp", [128, SP_N], F32)
        lamc = P.sbuf("lamc", [128, 16], F32)
        Lall = P.sbuf("Lall", [128, NBLK, 36], F32)
        dest_i = P.sbuf("dest_i", [128, NBLK, 2], I32)
        cw = P.sbuf("cw", [128, NBLK, 2], F32)
        widx = P.sbuf("widx", [128, NSB, 16], I32)
        didx = P.sbuf("didx", [128, NSB, 4], I32)
        P.persist_mark()
        P.dma('sync', identf[:], c_identf, writes=[identf], key=identf)
        P.dma('sync', identb[:], c_identb, writes=[identb], key=identb)
        P.dma('sync', embt[:], embp, writes=[embt], key=embt)
        P.V('memset', writes=[onesf], ap=onesf[:], constant=1.0)

        evac_i = [0]

        def evac(out_ap, in_ap, reads, writes):
            evac_i[0] += 1
            if evac_i[0] % 2 == 0:
                P.A('copy', reads=reads, writes=writes, out=out_ap, in_=in_ap)
            else:
                P.V('tensor_copy', reads=reads, writes=writes, out=out_ap, in_=in_ap)

        def emit_ln(hz, n, gcol, bcol, tmp, zero_cols=0, hb=None):
            ps1 = next_ps()
            ps2 = next_ps()
            for mc in range(DC):
                P.MM(reads=[onesf, hz], writes=[ps1], out=ps1[:, 0:n], lhsT=onesf[:], rhs=hz[:, mc, 0:n],
                     start=(mc == 0), stop=(mc == DC - 1))
            for mc in range(DC):
                zs = tmp['zsq'][mc % 2]
                P.A('activation', reads=[hz], writes=[zs], out=zs[:, 0:n], in_=hz[:, mc, 0:n], func=AF.Square)
                P.MM(reads=[onesf, zs], writes=[ps2], out=ps2[:, 0:n], lhsT=onesf[:], rhs=zs[:, 0:n],
                     start=(mc == 0), stop=(mc == DC - 1))
            mean, rstd = tmp['mean'], tmp['rstd']
            P.A('mul', reads=[ps1], writes=[mean], out=mean[:, 0:n], in_=ps1[:, 0:n], mul=1.0 / D)
            P.V('tensor_tensor', reads=[mean], writes=[rstd], out=rstd[:, 0:n], in0=mean[:, 0:n], in1=mean[:, 0:n], op=ALU.mult)
            P.V('scalar_tensor_tensor', reads=[ps2, rstd], writes=[rstd], out=rstd[:, 0:n], in0=ps2[:, 0:n], scalar=1.0 / D,
                in1=rstd[:, 0:n], op0=ALU.mult, op1=ALU.subtract)
            P.V('tensor_scalar', reads=[rstd], writes=[rstd], out=rstd[:, 0:n], in0=rstd[:, 0:n], scalar1=0.0, scalar2=EPS,
                op0=ALU.max, op1=ALU.add)
            P.A('activation', reads=[rstd], writes=[rstd], out=rstd[:, 0:n], in_=rstd[:, 0:n], func=AF.Sqrt)
            P.V('reciprocal', reads=[rstd], writes=[rstd], out=rstd[:, 0:n], in_=rstd[:, 0:n])
            for mc in range(DC):
                P.V('tensor_tensor', reads=[hz, mean], writes=[hz], out=hz[:, mc, 0:n], in0=hz[:, mc, 0:n], in1=mean[:, 0:n], op=ALU.subtract)
                P.V('tensor_tensor', reads=[hz, rstd], writes=[hz], out=hz[:, mc, 0:n], in0=hz[:, mc, 0:n], in1=rstd[:, 0:n], op=ALU.mult)
                P.A('activation', reads=[hz, sp, embt], writes=[hz], out=hz[:, mc, 0:n], in_=hz[:, mc, 0:n], func=AF.Identity,
                    scale=gcol(mc), bias=bcol(mc))
            if zero_cols:
                P.V('memset', writes=[hz], ap=hz[:, :, 0:zero_cols], constant=0.0)
            if hb is not None:
                P.G('tensor_copy', reads=[hz], writes=[hb], out=hb[:, :, 0:n], in_=hz[:, :, 0:n])

        def phase_embed():
            P.phase_begin()
            xt = [P.sbuf("xt", [128, D], F32) for _ in range(2)]
            xn = [P.sbuf("xn", [128, D], F32) for _ in range(2)]
            stt = P.sbuf("stt", [128, 4, 6], F32)
            mv = P.sbuf("mv", [128, 2], F32)
            rs = P.sbuf("rs", [128, 1], F32)
            hs = [P.sbuf("hs", [128, DC, 128], F32) for _ in range(2)]
            hsb = [P.sbuf("hsb", [128, DC, 128], BF16) for _ in range(2)]
            for blk in range(NBLK):
                x_ = xt[blk % 2]
                n_ = xn[blk % 2]
                h_ = hs[blk % 2]
                hb_ = hsb[blk % 2]
                P.dma('sync', x_[:], xin[blk * 128:(blk + 1) * 128, :], writes=[x_], key=x_)
                for j in range(4):
                    P.V('bn_stats', reads=[x_], writes=[stt], out=stt[:, j, :], in_=x_[:, j * 512:(j + 1) * 512])
                P.V('bn_aggr', reads=[stt], writes=[mv], out=mv[:], in_=stt[:].rearrange("p a b -> p (a b)"))
                P.V('tensor_scalar', reads=[mv], writes=[rs], out=rs[:], in0=mv[:, 1:2], scalar1=EPS, scalar2=None, op0=ALU.add)
                P.A('activation', reads=[rs], writes=[rs], out=rs[:], in_=rs[:], func=AF.Sqrt)
                P.V('reciprocal', reads=[rs], writes=[rs], out=rs[:], in_=rs[:])
                P.V('tensor_scalar', reads=[x_, mv, rs], writes=[n_], out=n_[:], in0=x_[:], scalar1=mv[:, 0:1], scalar2=rs[:, 0:1],
                    op0=ALU.subtract, op1=ALU.mult)
                import os
                CUT = int(os.environ.get('EMBED_CUT', '9'))
                for c4 in range(4):
                    ps = next_ps()
                    for cc in range(4):
                        c = c4 * 4 + cc
                        if CUT >= 1:
                            P.TR(reads=[n_, identf], writes=[ps], out=ps[:, cc * 128:(cc + 1) * 128], in_=n_[:, c * 128:(c + 1) * 128],
                                 identity=identf[:])
                    for cc in range(4):
                        c = c4 * 4 + cc
                        if CUT >= 2:
                            P.A('activation', reads=[ps, embt], writes=[h_], out=h_[:, c, :], in_=ps[:, cc * 128:(cc + 1) * 128],
                                func=AF.Identity, scale=embt[:, c:c + 1], bias=embt[:, 16 + c:17 + c])
                        else:
                            P.V('tensor_copy', reads=[n_], writes=[h_], out=h_[:, c, :], in_=n_[:, c * 128:(c + 1) * 128])
                if blk == 0:
                    P.V('memset', writes=[h_], ap=h_[:, :, 0:PAD], constant=0.0)
                P.V('tensor_copy', reads=[h_], writes=[hb_], out=hb_[:], in_=h_[:])
                tl, off = blk // 3, (blk % 3) * 128
                P.dma('sync', hres[tl, :, :, off:off + 128], h_[:], reads=[h_], key=h_)
                P.dma('sync', hbf[tl, :, :, off:off + 128], hb_[:], reads=[hb_], key=hb_)
            P.barrier()

        def phase_win(l):
            P.phase_begin()
            wb = [P.sbuf("wb", [128, DC, 512], BF16) for _ in range(2)]
            hball = [P.sbuf("hball", [128, DC, NT], BF16) for _ in range(NTILES)]
            ot = [P.sbuf("ot", [128, 4, NT], F32) for _ in range(2)]
            wsrc = w_in[l].rearrange("(kc p) m -> p kc m", p=128)
            it = 0
            def load_w(mg):
                w_ = wb[mg % 2]
                for half in range(2):
                    P.dma('gpsimd', w_[:, half * 8:(half + 1) * 8, :], wsrc[:, half * 8:(half + 1) * 8, mg * 512:(mg + 1) * 512],
                          writes=[w_], key=w_)
            load_w(0)
            for tl in range(NTILES):
                P.dma('sync', hball[tl][:], hbf[tl], writes=[hball[tl]], key=hball[tl])
            for mg in range(21):
                w_ = wb[mg % 2]
                if mg + 1 < 21:
                    load_w(mg + 1)
                for tl in range(NTILES):
                    o_ = ot[it % 2]
                    it += 1
                    for mc in range(4):
                        ps = next_ps()
                        for kc in range(DC):
                            P.MM(reads=[w_, hball[tl]], writes=[ps], out=ps[:, 0:NT], lhsT=w_[:, kc, mc * 128:(mc + 1) * 128], rhs=hball[tl][:, kc, :],
                                 start=(kc == 0), stop=(kc == DC - 1))
                        evac(o_[:, mc, :], ps[:, 0:NT], [ps], [o_])
                    P.dma('gpsimd', cols[tl, :, mg * 4:(mg + 1) * 4, :], o_[:], reads=[o_], key=o_)
            P.barrier()

        def phase_pool(l):
            P.phase_begin()
            U = P.sbuf("U", [128, 16 + T], F32)
            Wk = [P.sbuf("Wk", [128, 16 + T], F32) for _ in range(2)]
            icn = P.sbuf("icn", [128, T], F32)
            dlt = [P.sbuf("dlt", [128, T], BF16) for _ in range(2)]
            po = P.sbuf("po", [128, T], BF16)
            pw = P.sbuf("pw", [128, 2, 256], BF16)
            P.V('memset', writes=[U], ap=U[:, 0:16], constant=0.0)
            for w_ in Wk:
                P.V('memset', writes=[w_], ap=w_[:, 0:16], constant=0.0)
            for g in range(4):
                P.dma('sync', icn[:], c_invcnt[g], writes=[icn], key=icn)
                P.dma('gpsimd', pw[:], pool_w[l, g].rearrange("(kc p) m -> p kc m", p=128), writes=[pw], key=pw)
                for half in range(2):
                    c = 2 * g + half
                    P.dma('sync', rowview(U[:, 16:16 + T]), rowap(cols, C_POOL + c), writes=[U], key=U)
                    src = U
                    for j in range(g + 1):
                        sh = 1 << j
                        dst = Wk[j % 2]
                        P.V('tensor_tensor', reads=[src], writes=[dst], out=dst[:, 16:16 + T], in0=src[:, 16:16 + T],
                            in1=src[:, 16 - sh:16 - sh + T], op=ALU.add)
                        src = dst
                    other = Wk[(g + 1) % 2]
                    P.V('tensor_tensor', reads=[src, icn], writes=[other], out=other[:, 16:16 + T], in0=src[:, 16:16 + T], in1=icn[:],
                        op=ALU.mult)
                    P.V('tensor_tensor', reads=[other, U], writes=[dlt[half]], out=dlt[half][:], in0=other[:, 16:16 + T],
                        in1=U[:, 16:16 + T], op=ALU.subtract)
                for mo in range(2):
                    co = 2 * g + mo
                    for tl in range(NTILES):
                        ps = next_ps()
                        for kc in range(2):
                            P.MM(reads=[pw, dlt[kc]], writes=[ps], out=ps[:, 0:NT], lhsT=pw[:, kc, mo * 128:(mo + 1) * 128],
                                 rhs=dlt[kc][:, tl * NT:(tl + 1) * NT], start=(kc == 0), stop=(kc == 1))
                        P.A('activation', reads=[ps, sp], writes=[po], out=po[:, tl * NT:(tl + 1) * NT], in_=ps[:, 0:NT], func=AF.Identity,
                            scale=sp[:, SP_PSC + co:SP_PSC + co + 1])
                    P.dma('gpsimd', rowap(mix, co), rowview(po[:]), reads=[po], key=po)
            P.barrier()

        def phase_lru(l):
            P.phase_begin()
            X = P.sbuf("X", [128, 3 + T], F32)
            XC = [P.sbuf("XC", [128, T], F32) for _ in range(2)]
            xcb = [P.sbuf("xcb", [128, T], BF16) for _ in range(2)]
            GA = P.sbuf("GA", [128, T], F32)
            GX = P.sbuf("GX", [128, T], F32)
            AR = P.sbuf("AR", [128, T], F32)
            TB = P.sbuf("TB", [128, T], F32)
            lo = P.sbuf("lo", [128, T], BF16)
            wa = P.sbuf("wa", [128, 2, 256], BF16)
            wx = P.sbuf("wx", [128, 2, 256], BF16)
            P.A('activation', reads=[sp], writes=[lamc], out=lamc[:, 0:8], in_=sp[:, SP_LAM:SP_LAM + 8], func=AF.Exp, scale=-1.0)
            P.A('activation', reads=[lamc], writes=[lamc], out=lamc[:, 0:8], in_=lamc[:, 0:8], func=AF.Ln, bias=1.0)
            P.A('mul', reads=[lamc], writes=[lamc], out=lamc[:, 8:16], in_=lamc[:, 0:8], mul=-16.0)
            P.A('mul', reads=[lamc], writes=[lamc], out=lamc[:, 0:8], in_=lamc[:, 0:8], mul=-8.0)
            P.V('memset', writes=[X], ap=X[:, 0:3], constant=0.0)
            for blk in range(4):
                P.dma('gpsimd', wa[:], lru_wa[l, blk].rearrange("(kc p) m -> p kc m", p=128), writes=[wa], key=wa)
                P.dma('gpsimd', wx[:], lru_wx[l, blk].rearrange("(kc p) m -> p kc m", p=128), writes=[wx], key=wx)
                for half in range(2):
                    c = 2 * blk + half
                    xc = XC[half]
                    P.dma('sync', rowview(X[:, 3:3 + T]), rowap(cols, C_LX + c), writes=[X], key=X)
                    P.V('tensor_scalar', reads=[X, sp], writes=[xc], out=xc[:], in0=X[:, 0:T], scalar1=sp[:, SP_CW + c:SP_CW + c + 1],
                        scalar2=sp[:, SP_CB + c:SP_CB + c + 1], op0=ALU.mult, op1=ALU.add)
                    for j in range(1, 4):
                        P.V('scalar_tensor_tensor', reads=[X, sp, xc], writes=[xc], out=xc[:], in0=X[:, j:j + T],
                            scalar=sp[:, SP_CW + j * 8 + c:SP_CW + j * 8 + c + 1], in1=xc[:], op0=ALU.mult, op1=ALU.add)
                    P.G('tensor_copy', reads=[xc], writes=[xcb[half]], out=xcb[half][:], in_=xc[:])
                for mo in range(2):
                    co = 2 * blk + mo
                    for tl in range(NTILES):
                        sl = slice(tl * NT, (tl + 1) * NT)
                        psa = next_ps()
                        for kc in range(2):
                            P.MM(reads=[wa, xcb[kc]], writes=[psa], out=psa[:, 0:NT], lhsT=wa[:, kc, mo * 128:(mo + 1) * 128],
                                 rhs=xcb[kc][:, sl], start=(kc == 0), stop=(kc == 1))
                        P.A('activation', reads=[psa, sp], writes=[GA], out=GA[:, sl], in_=psa[:, 0:NT], func=AF.Sigmoid,
                            bias=sp[:, SP_BA + co:SP_BA + co + 1])
                        psx = next_ps()
                        for kc in range(2):
                            P.MM(reads=[wx, xcb[kc]], writes=[psx], out=psx[:, 0:NT], lhsT=wx[:, kc, mo * 128:(mo + 1) * 128],
                                 rhs=xcb[kc][:, sl], start=(kc == 0), stop=(kc == 1))
                        P.A('activation', reads=[psx, sp], writes=[GX], out=GX[:, sl], in_=psx[:, 0:NT], func=AF.Sigmoid,
                            bias=sp[:, SP_BX + co:SP_BX + co + 1])
                    P.A('activation', reads=[GA, lamc], writes=[AR], out=AR[:], in_=GA[:], func=AF.Exp, scale=lamc[:, co:co + 1])
                    P.A('activation', reads=[GA, lamc], writes=[TB], out=TB[:], in_=GA[:], func=AF.Exp, scale=lamc[:, 8 + co:9 + co])
                    P.V('tensor_scalar', reads=[TB], writes=[TB], out=TB[:], in0=TB[:], scalar1=-1.0, scalar2=1.0, op0=ALU.mult, op1=ALU.add)
                    P.V('tensor_scalar', reads=[TB], writes=[TB], out=TB[:], in0=TB[:], scalar1=0.0, scalar2=None, op0=ALU.max)
                    P.A('activation', reads=[TB], writes=[TB], out=TB[:], in_=TB[:], func=AF.Sqrt)
                    P.V('tensor_tensor', reads=[TB, GX], writes=[TB], out=TB[:], in0=TB[:], in1=GX[:], op=ALU.mult)
                    P.V('tensor_tensor', reads=[TB, XC[mo]], writes=[TB], out=TB[:], in0=TB[:], in1=XC[mo][:], op=ALU.mult)
                    P.V('memset', writes=[TB], ap=TB[:, 0:PAD], constant=0.0)
                    P.V('tensor_tensor_scan', reads=[AR, TB], writes=[GA], out=GA[:], data0=AR[:], data1=TB[:], initial=0.0,
                        op0=ALU.mult, op1=ALU.add)
                    P.dma('sync', rowview(GX[:]), rowap(cols, C_LY + co), writes=[GX], key=GX)
                    P.A('activation', reads=[GX], writes=[GX], out=GX[:], in_=GX[:], func=AF.Gelu)
                    P.V('tensor_tensor', reads=[GA, GX], writes=[lo], out=lo[:], in0=GA[:], in1=GX[:], op=ALU.mult)
                    P.dma('gpsimd', rowap(mix, 16 + co), rowview(lo[:]), reads=[lo], key=lo)
            P.barrier()

        def phase_attn(l):
            P.phase_begin()
            AB = P.sbuf("AB", [128, 16, 256], F32)
            PM = P.sbuf("PM", [128, 256], F32)
            VT = P.sbuf("VT", [128, NBLK + 1, 256], BF16)
            kT = P.sbuf("kT", [64, 4, 128 + T], BF16)
            vrow = P.sbuf("vrow", [128, T], BF16)
            qT = [P.sbuf("qT", [64, T], BF16) for _ in range(2)]
            AO = [P.sbuf("AO", [128, T], BF16) for _ in range(2)]
            ssb = [P.sbuf("ssb", [128, 256], F32) for _ in range(4)]
            pn = [P.sbuf("pn", [128, 256], BF16) for _ in range(4)]
            pT = [P.sbuf("pT", [128, 256], BF16) for _ in range(4)]
            sm = [P.sbuf("sm", [128, 8], F32) for _ in range(4)]
            psS = [psf[0], psf[1]]
            psO = [psf[2], psf[3]]
            P.dma('sync', AB[:], c_ab, writes=[AB], key=AB)
            P.dma('sync', PM[:], c_pm, writes=[PM], key=PM)
            P.V('memset', writes=[VT], ap=VT[:, 0, :], constant=0.0)
            P.V('memset', writes=[kT], ap=kT[:, :, 0:128], constant=0.0)
            for j in range(4):
                P.dma('gpsimd', rowview(kT[0:64, j, 128:128 + T]), rowap(cols, C_K + j // 2, (j % 2) * 64, (j % 2) * 64 + 64),
                      writes=[kT], key=kT)
            for vc in range(2):
                P.dma('gpsimd', rowview(vrow[:]), rowap(cols, C_V + vc), writes=[vrow], key=vrow)
                for blk in range(NBLK):
                    pb = psb[blk % 2]
                    P.TR(reads=[vrow, identb], writes=[pb], out=pb[:, 0:128], in_=vrow[:, blk * 128:(blk + 1) * 128], identity=identb[:])
                    evac(VT[:, blk + 1, vc * 128:(vc + 1) * 128], pb[:, 0:128], [pb], [VT])
            P.barrier()

            class Reg:
                def __init__(self, ap):
                    self.ap = ap
                    self.buf = Buf("reg")

            DEP = 4
            rS = [Reg(psf[k // 2][:, (k % 2) * 256:(k % 2) * 256 + 256]) for k in range(4)]
            rO = [psf[2][:, k * 128:(k + 1) * 128] for k in range(4)]
            rOb = [Reg(None) for _ in range(4)]
            rB = [Reg(psb[k // 4][:, (k % 4) * 256:(k % 4) * 256 + 256]) for k in range(8)]
            iters = [(h, blk) for h in range(16) for blk in range(NBLK)]
            NI = len(iters)

            def stageA(i):
                h, blk = iters[i]
                j = h // 4
                q_ = qT[h % 2]
                p0 = (h % 2) * 64
                if blk == 0:
                    P.dma('gpsimd', rowview(q_[0:64, :]), rowap(cols, C_Q + h // 2, p0, p0 + 64), writes=[q_], key=q_)
                s_, sm_, pS = ssb[i % DEP], sm[i % DEP], rS[i % 4]
                P.MM(reads=[q_, kT], writes=[pS], out=pS.ap, lhsT=q_[0:64, blk * 128:(blk + 1) * 128],
                     rhs=kT[0:64, j, blk * 128:blk * 128 + 256], start=True, stop=True)
                P.V('scalar_tensor_tensor', reads=[pS, AB], writes=[s_], out=s_[:], in0=pS.ap, scalar=0.125, in1=AB[:, h, :],
                    op0=ALU.mult, op1=ALU.add)
                if blk == 0:
                    P.V('tensor_tensor', reads=[s_, PM], writes=[s_], out=s_[:], in0=s_[:], in1=PM[:], op=ALU.add)
                if blk == 1:
                    P.V('tensor_tensor', reads=[s_, PM], writes=[s_], out=s_[:, 0:PAD], in0=s_[:, 0:PAD], in1=PM[:, 0:PAD], op=ALU.add)
                P.V('reduce_max', reads=[s_], writes=[sm_], out=sm_[:, 0:1], in_=s_[:], axis=AX.X)
                P.V('tensor_scalar', reads=[sm_, sp], writes=[sm_], out=sm_[:, 1:2], in0=sm_[:, 0:1],
                    scalar1=sp[:, SP_SINK + h:SP_SINK + h + 1], scalar2=-1.0, op0=ALU.max, op1=ALU.mult)

            def stageB_act(i):
                h, blk = iters[i]
                s_, sm_ = ssb[i % DEP], sm[i % DEP]
                P.A('activation', reads=[s_, sm_], writes=[s_, sm_], out=s_[:], in_=s_[:], func=AF.Exp, bias=sm_[:, 1:2],
                    accum_out=sm_[:, 2:3])
                P.A('activation', reads=[sp, sm_], writes=[sm_], out=sm_[:, 3:4], in_=sp[:, SP_SINK + h:SP_SINK + h + 1], func=AF.Exp,
                    bias=sm_[:, 1:2])

            def stageB_dve(i):
                s_, sm_, pn_ = ssb[i % DEP], sm[i % DEP], pn[i % DEP]
                P.V('tensor_tensor', reads=[sm_], writes=[sm_], out=sm_[:, 4:5], in0=sm_[:, 2:3], in1=sm_[:, 3:4], op=ALU.add)
                P.V('reciprocal', reads=[sm_], writes=[sm_], out=sm_[:, 5:6], in_=sm_[:, 4:5])
                P.V('tensor_scalar', reads=[s_, sm_], writes=[pn_], out=pn_[:], in0=s_[:], scalar1=sm_[:, 5:6], scalar2=None, op0=ALU.mult)

            def stageC1(i):
                pn_, pT_, pb = pn[i % DEP], pT[i % DEP], rB[i % 8]
                for kb in range(2):
                    P.TR(reads=[pn_, identb], writes=[pb], out=pb.ap[:, kb * 128:(kb + 1) * 128], in_=pn_[:, kb * 128:(kb + 1) * 128],
                         identity=identb[:])
                P.A('copy', reads=[pb], writes=[pT_], out=pT_[:], in_=pb.ap)

            def stageC2(i):
                h, blk = iters[i]
                j = h // 4
                p0 = (h % 2) * 64
                ao = AO[(h // 2) % 2]
                pT_, pO, pOb = pT[i % DEP], rO[i % 4], rOb[i % 4]
                for kb in range(2):
                    P.MM(reads=[VT, pT_], writes=[pOb], out=pO[p0:p0 + 64, :], lhsT=VT[:, blk + kb, j * 64:(j + 1) * 64],
                         rhs=pT_[:, kb * 128:(kb + 1) * 128], start=(kb == 0), stop=(kb == 1))
                P.A('copy', reads=[pOb], writes=[ao], out=ao[p0:p0 + 64, blk * 128:(blk + 1) * 128], in_=pO[p0:p0 + 64, :])
                if h % 2 == 1 and blk == NBLK - 1:
                    P.dma('gpsimd', rowap(mix, 8 + h // 2), rowview(ao[:]), reads=[ao], key=ao)

            for step in range(NI + 3):
                if step - 1 >= 0 and step - 1 < NI:
                    stageB_act(step - 1)
                if step - 3 >= 0 and step - 3 < NI:
                    stageC2(step - 3)
                if step < NI:
                    stageA(step)
                if step - 2 >= 0 and step - 2 < NI:
                    stageC1(step - 2)
                if step - 1 >= 0 and step - 1 < NI:
                    stageB_dve(step - 1)
            P.barrier()

        def phase_merge(l):
            P.phase_begin()
            pw = [[P.sbuf("pjw", [128, 8, 1024], BF16) for _ in range(3)]]
            mx = [P.sbuf("mx", [128, 24, NT], BF16) for _ in range(2)]
            gt = [P.sbuf("gt", [128, 3, 8, NT], F32) for _ in range(2)]
            acc = [P.sbuf("acc", [128, NT], F32) for _ in range(2)]
            tm = [P.sbuf("tm", [128, NT], F32) for _ in range(2)]
            mt = [P.sbuf("mt", [128, 8, NT], BF16) for _ in range(2)]
            it = 0
            def load_pw(mg):
                for i in range(3):
                    P.dma('gpsimd', pw[0][i][:], proj[i][l].rearrange("(kc p) m -> p kc m", p=128)[:, :, mg * 1024:(mg + 1) * 1024],
                          writes=[pw[0][i]], key=pw[0][i])
            for mg in range(2):
                pws = pw[0]
                load_pw(mg)
                for tl in range(NTILES):
                    mx_, gt_, mt_ = mx[it % 2], gt[it % 2], mt[it % 2]
                    it += 1
                    P.dma('sync', mx_[:], mix[tl], writes=[mx_], key=mx_)
                    for i in range(3):
                        c0 = C_G + i * 16 + mg * 8
                        P.dma('sync', gt_[:, i, :, :], cols[tl, :, c0:c0 + 8, :], writes=[gt_], key=gt_)
                    P.A('activation', reads=[gt_], writes=[gt_], out=gt_[:].rearrange("p a b n -> p (a b n)"), in_=gt_[:].rearrange("p a b n -> p (a b n)"), func=AF.Sigmoid)
                    for mc in range(8):
                        ac, t_ = acc[mc % 2], tm[mc % 2]
                        for i in range(3):
                            ps = next_ps()
                            for kc in range(8):
                                P.MM(reads=[pws[i], mx_], writes=[ps], out=ps[:, 0:NT], lhsT=pws[i][:, kc, mc * 128:(mc + 1) * 128],
                                     rhs=mx_[:, i * 8 + kc, :], start=(kc == 0), stop=(kc == 7))
                            if i == 0:
                                P.V('tensor_tensor', reads=[ps, gt_], writes=[ac], out=ac[:], in0=ps[:, 0:NT], in1=gt_[:, i, mc, :], op=ALU.mult)
                            else:
                                P.V('tensor_tensor', reads=[ps, gt_], writes=[t_], out=t_[:], in0=ps[:, 0:NT], in1=gt_[:, i, mc, :], op=ALU.mult)
                                if i == 1:
                                    P.V('tensor_tensor', reads=[ac, t_], writes=[ac], out=ac[:], in0=ac[:], in1=t_[:], op=ALU.add)
                                else:
                                    P.V('tensor_tensor', reads=[ac, t_], writes=[mt_], out=mt_[:, mc, :], in0=ac[:], in1=t_[:], op=ALU.add)
                    P.dma('gpsimd', merged[tl, :, mg * 8:(mg + 1) * 8, :], mt_[:], reads=[mt_], key=mt_)
            P.barrier()

        def phase_wout(l):
            P.phase_begin()
            wo = P.sbuf("wo", [128, DC, D], BF16)
            mg_ = [P.sbuf("mgd", [128, DC, NT], BF16) for _ in range(2)]
            hz = [P.sbuf("hz", [128, DC, NT], F32) for _ in range(2)]
            tmp = {'zsq': [P.sbuf("zsq", [128, NT], F32) for _ in range(2)], 'mean': P.sbuf("mean", [128, NT], F32),
                   'rstd': P.sbuf("rstd", [128, NT], F32)}
            tmf = [P.sbuf("tmf", [128, D], F32) for _ in range(2)]
            tmb = [P.sbuf("tmb", [128, D], BF16) for _ in range(2)]
            wr = P.sbuf("wr", [128, DC, 36], F32)
            rb = P.sbuf("rb", [1, 36], F32)
            P.dma('sync', wr[:].rearrange("p a b -> p (a b)"), rw_tab[l], writes=[wr], key=wr)
            P.dma('sync', rb[:], rbias[l], writes=[rb], key=rb)
            wsrc = w_out[l].rearrange("(kc p) m -> p kc m", p=128)
            for q4 in range(4):
                for half in range(2):
                    P.dma('gpsimd', wo[:, half * 8:(half + 1) * 8, q4 * 512:(q4 + 1) * 512],
                          wsrc[:, half * 8:(half + 1) * 8, q4 * 512:(q4 + 1) * 512], writes=[wo], key=wo)
            for tl in range(NTILES):
                m_, z_ = mg_[tl % 2], hz[tl % 2]
                P.dma('sync', m_[:], merged[tl], writes=[m_], key=m_)
                P.dma('sync', z_[:], hres[tl], writes=[z_], key=z_)
                for mc in range(DC):
                    ps = next_ps()
                    for kc in range(DC):
                        P.MM(reads=[wo, m_], writes=[ps], out=ps[:, 0:NT], lhsT=wo[:, kc, mc * 128:(mc + 1) * 128], rhs=m_[:, kc, :],
                             start=(kc == 0), stop=(kc == DC - 1))
                    P.V('scalar_tensor_tensor', reads=[z_, ps], writes=[z_], out=z_[:, mc, :], in0=z_[:, mc, :], scalar=ALPHA, in1=ps[:, 0:NT],
                        op0=ALU.mult, op1=ALU.add)
                emit_ln(z_, NT, lambda mc: sp[:, SP_LN1G + mc:SP_LN1G + mc + 1], lambda mc: sp[:, SP_LN1B + mc:SP_LN1B + mc + 1], tmp,
                        zero_cols=(PAD if tl == 0 else 0), hb=None)
                for sb3 in range(3):
                    blk = tl * 3 + sb3
                    tf, tb = tmf[blk % 2], tmb[blk % 2]
                    pl = next_ps()
                    for kc in range(DC):
                        P.MM(reads=[z_, wr], writes=[pl], out=pl[:, 0:36], lhsT=z_[:, kc, sb3 * 128:(sb3 + 1) * 128], rhs=wr[:, kc, :],
                             start=(kc == 0), stop=False)
                    P.MM(reads=[onesf, rb], writes=[pl], out=pl[:, 0:36], lhsT=onesf[0:1, :], rhs=rb[0:1, :], start=False, stop=True)
                    P.V('tensor_copy', reads=[pl], writes=[Lall], out=Lall[:, blk, :], in_=pl[:, 0:36])
                    for c4 in range(4):
                        ps = next_ps()
                        for cc in range(4):
                            c = c4 * 4 + cc
                            P.TR(reads=[z_, identf], writes=[ps], out=ps[:, cc * 128:(cc + 1) * 128],
                                 in_=z_[:, c, sb3 * 128:(sb3 + 1) * 128], identity=identf[:])
                        evac(tf[:, c4 * 512:(c4 + 1) * 512], ps[:, 0:512], [ps], [tf])
                    P.G('tensor_copy', reads=[tf], writes=[tb], out=tb[:], in_=tf[:])
                    P.dma('gpsimd', h1tm_f[blk * 128:(blk + 1) * 128, :], tf[:], reads=[tf], key=tf)
                    P.dma('gpsimd', h1tm_b[blk * 128:(blk + 1) * 128, :], tb[:], reads=[tb], key=tb)
            P.barrier()

        def phase_router(l):
            P.phase_begin()
            ELm = [P.sbuf("ELm", [128, 32], F32) for _ in range(2)]
            sc = [P.sbuf("sc", [128, 32], F32) for _ in range(2)]
            M1a = P.sbuf("M1a", [128, NBLK, 32], F32)
            M2a = P.sbuf("M2a", [128, NBLK, 32], F32)
            M12b = P.sbuf("M12b", [128, NBLK, 32], BF16)
            onesb = P.sbuf("onesb", [128, 128], BF16)
            trib = P.sbuf("trib", [128, 128], BF16)
            thr = P.sbuf("thr", [128, NSB], F32)
            pio = P.sbuf("pio", [128, 1], F32)
            cnt = P.sbuf("cnt", [128, 32], F32)
            nbk = P.sbuf("nbk", [128, 32], F32)
            pend = P.sbuf("pend", [128, 32], F32)
            pstart = P.sbuf("pstart", [128, 32], F32)
            Dm = [P.sbuf("Dm", [128, 32], F32) for _ in range(2)]
            tt = [P.sbuf("tt", [128, 32], F32) for _ in range(2)]
            destf = P.sbuf("destf", [128, NBLK, 2], F32)
            be = P.sbuf("be", [128, NSB], F32)
            chg = P.sbuf("chg", [128, NSB], F32)
            gb = P.sbuf("gb", [128, NSB], F32)
            db = P.sbuf("db", [128, NSB], F32)
            widf = P.sbuf("widf", [128, NSB, 16], F32)
            didf = P.sbuf("didf", [128, NSB, 4], F32)
            padfix = P.sbuf("padfix", [128, 2], F32)
            P.dma('sync', trib[:], c_tri, writes=[trib], key=trib)
            P.dma('sync', thr[:], c_thr, writes=[thr], key=thr)
            P.dma('sync', pio[:], c_piota, writes=[pio], key=pio)
            P.V('memset', writes=[onesb], ap=onesb[:], constant=1.0)
            for blk in range(NBLK):
                E_, s_ = ELm[blk % 2], sc[blk % 2]
                L_ = Lall
                P.V('reduce_max', reads=[L_], writes=[s_], out=s_[:, 0:1], in_=Lall[:, blk, 0:4], axis=AX.X)
                P.V('tensor_scalar', reads=[s_], writes=[s_], out=s_[:, 1:2], in0=s_[:, 0:1], scalar1=-1.0, scalar2=None, op0=ALU.mult)
                P.A('activation', reads=[L_, s_], writes=[s_], out=s_[:, 24:28], in_=Lall[:, blk, 0:4], func=AF.Exp, bias=s_[:, 1:2],
                    accum_out=s_[:, 2:3])
                P.V('reciprocal', reads=[s_], writes=[s_], out=s_[:, 3:4], in_=s_[:, 2:3])
                P.V('tensor_scalar', reads=[L_, s_], writes=[s_], out=s_[:, 4:8], in0=Lall[:, blk, 0:4], scalar1=s_[:, 0:1], scalar2=None,
                    op0=ALU.is_equal)
                P.V('tensor_scalar', reads=[s_], writes=[s_], out=s_[:, 4:8], in0=s_[:, 4:8], scalar1=-1.0, scalar2=1e30, op0=ALU.add,
                    op1=ALU.mult)
                for g in range(4):
                    P.V('tensor_scalar', reads=[L_, s_], writes=[E_], out=E_[:, g * 8:(g + 1) * 8], in0=Lall[:, blk, 4 + g * 8:12 + g * 8],
                        scalar1=s_[:, 4 + g:5 + g], scalar2=None, op0=ALU.add)
                P.V('max', reads=[E_], writes=[s_], out=s_[:, 8:16], in_=E_[:])
                P.V('tensor_tensor', reads=[s_], writes=[s_], out=s_[:, 16:17], in0=s_[:, 8:9], in1=s_[:, 9:10], op=ALU.subtract)
                P.A('activation', reads=[s_], writes=[s_], out=s_[:, 17:18], in_=s_[:, 16:17], func=AF.Sigmoid)
                P.V('tensor_scalar', reads=[s_], writes=[s_], out=s_[:, 18:19], in0=s_[:, 17:18], scalar1=-1.0, scalar2=1.0, op0=ALU.mult,
                    op1=ALU.add)
                P.V('tensor_scalar', reads=[s_], writes=[cw], out=cw[:, blk, :], in0=s_[:, 17:19], scalar1=s_[:, 3:4], scalar2=None,
                    op0=ALU.mult)
                P.V('tensor_scalar', reads=[E_, s_], writes=[M1a], out=M1a[:, blk, :], in0=E_[:], scalar1=s_[:, 8:9], scalar2=None,
                    op0=ALU.is_equal)
                P.V('tensor_scalar', reads=[E_, s_], writes=[M2a], out=M2a[:, blk, :], in0=E_[:], scalar1=s_[:, 9:10], scalar2=None,
                    op0=ALU.is_equal)
                if blk == 0:
                    P.V('memset', writes=[M1a], ap=M1a[0:PAD, 0, :], constant=0.0)
                    P.V('memset', writes=[M2a], ap=M2a[0:PAD, 0, :], constant=0.0)
                P.V('tensor_tensor', reads=[M1a, M2a], writes=[M12b], out=M12b[:, blk, :], in0=M1a[:, blk, :], in1=M2a[:, blk, :], op=ALU.add)
            pc = next_ps()
            for blk in range(NBLK):
                P.MM(reads=[onesb, M12b], writes=[pc], out=pc[:, 0:32], lhsT=onesb[:], rhs=M12b[:, blk, :], start=(blk == 0),
                     stop=(blk == NBLK - 1))
            P.V('tensor_copy', reads=[pc], writes=[cnt], out=cnt[:], in_=pc[:, 0:32])
            P.V('memset', writes=[nbk], ap=nbk[:], constant=0.0)
            for k in range(34):
                P.V('scalar_tensor_tensor', reads=[cnt, nbk], writes=[nbk], out=nbk[:], in0=cnt[:], scalar=float(128 * k), in1=nbk[:],
                    op0=ALU.is_gt, op1=ALU.add)
            P.V('tensor_scalar', reads=[nbk], writes=[nbk], out=nbk[:], in0=nbk[:], scalar1=128.0, scalar2=None, op0=ALU.mult)
            P.V('tensor_tensor_scan', reads=[onesf, nbk], writes=[pend], out=pend[:], data0=onesf[:, 0:32], data1=nbk[:], initial=0.0,
                op0=ALU.mult, op1=ALU.add)
            P.V('tensor_tensor', reads=[pend, nbk], writes=[pstart], out=pstart[:], in0=pend[:], in1=nbk[:], op=ALU.subtract)
            for blk in range(NBLK):
                pC = next_ps()
                P.MM(reads=[trib, M12b], writes=[pC], out=pC[:, 0:32], lhsT=trib[:], rhs=M12b[:, blk, :], start=True, stop=(blk == 0))
                for j in range(blk):
                    P.MM(reads=[onesb, M12b], writes=[pC], out=pC[:, 0:32], lhsT=onesb[:], rhs=M12b[:, j, :], start=False, stop=(j == blk - 1))
                D_, t_ = Dm[blk % 2], tt[blk % 2]
                P.V('tensor_tensor', reads=[pC, pstart], writes=[D_], out=D_[:], in0=pC[:, 0:32], in1=pstart[:], op=ALU.add)
                for k, Ma in enumerate((M1a, M2a)):
                    P.V('tensor_tensor', reads=[Ma, D_], writes=[t_], out=t_[:], in0=Ma[:, blk, :], in1=D_[:], op=ALU.mult)
                    P.V('reduce_sum', reads=[t_], writes=[destf], out=destf[:, blk, k:k + 1], in_=t_[:], axis=AX.X)
            P.V('reduce_sum', reads=[M1a], writes=[padfix], out=padfix[:, 0:1], in_=M1a[:, 0, :], axis=AX.X)
            P.V('tensor_scalar', reads=[padfix], writes=[padfix], out=padfix[:, 1:2], in0=padfix[:, 0:1], scalar1=-BIGI, scalar2=BIGI,
                op0=ALU.mult, op1=ALU.add)
            for k in range(2):
                P.V('tensor_tensor', reads=[destf, padfix], writes=[destf], out=destf[:, 0, k:k + 1], in0=destf[:, 0, k:k + 1],
                    in1=padfix[:, 1:2], op=ALU.add)
            P.V('tensor_copy', reads=[destf], writes=[dest_i], out=dest_i[:], in_=destf[:])
            P.V('memset', writes=[be], ap=be[:], constant=0.0)
            for e in range(NEXP):
                P.V('scalar_tensor_tensor', reads=[thr, pend, be], writes=[be], out=be[:], in0=thr[:], scalar=pend[:, e:e + 1], in1=be[:],
                    op0=ALU.is_ge, op1=ALU.add)
            P.V('tensor_scalar', reads=[be], writes=[be], out=be[:], in0=be[:], scalar1=31.0, scalar2=None, op0=ALU.min)
            P.V('memset', writes=[chg], ap=chg[:, 0:1], constant=1.0)
            P.V('tensor_tensor', reads=[be], writes=[chg], out=chg[:, 1:NSB], in0=be[:, 1:NSB], in1=be[:, 0:NSB - 1], op=ALU.not_equal)
            for (dst, mul) in ((gb, 128.0), (db, 128.0)):
                P.V('tensor_scalar', reads=[be], writes=[dst], out=dst[:], in0=be[:], scalar1=mul, scalar2=float(l * NEXP) * mul - BIGI, op0=ALU.mult, op1=ALU.add)
                P.V('tensor_tensor', reads=[dst, chg], writes=[dst], out=dst[:], in0=dst[:], in1=chg[:], op=ALU.mult)
                P.V('tensor_scalar', reads=[dst], writes=[dst], out=dst[:], in0=dst[:], scalar1=BIGI, scalar2=None, op0=ALU.add)
                P.V('tensor_scalar', reads=[dst, pio], writes=[dst], out=dst[:], in0=dst[:], scalar1=pio[:, 0:1], scalar2=None, op0=ALU.add)
            for kc in range(16):
                P.V('tensor_scalar', reads=[gb], writes=[widf], out=widf[:, :, kc], in0=gb[:], scalar1=float(kc * 128), scalar2=None, op0=ALU.add)
            for kc in range(4):
                P.V('tensor_scalar', reads=[db], writes=[didf], out=didf[:, :, kc], in0=db[:], scalar1=float(kc * 128), scalar2=None, op0=ALU.add)
            P.V('tensor_copy', reads=[widf], writes=[widx], out=widx[:], in_=widf[:])
            P.V('tensor_copy', reads=[didf], writes=[didx], out=didx[:], in_=didf[:])
            P.barrier()

        def phase_moe(l, last):
            P.phase_begin()
            xsrc = [P.sbuf("xsrc", [128, D], BF16) for _ in range(2)]
            for blk in range(NBLK):
                x_ = xsrc[blk % 2]
                P.dma('sync', x_[:], h1tm_b[blk * 128:(blk + 1) * 128, :], writes=[x_], key=x_)
                for k in range(2):
                    P.dma_fn('gpsimd', lambda e, x_=x_, blk=blk, k=k: e.indirect_dma_start(
                        out=xs[:, :], out_offset=bass.IndirectOffsetOnAxis(ap=dest_i[:, blk, k:k + 1], axis=0), in_=x_[:, :], in_offset=None,
                        bounds_check=_breg(e, CAP - 1), oob_is_err=False), reads=[x_, dest_i], key=x_)
            P.barrier()
            P.phase_begin()
            xsb = [P.sbuf("xsb", [128, D], BF16) for _ in range(2)]
            xT = [P.sbuf("xT", [128, D], BF16) for _ in range(2)]
            wg = P.sbuf("wg", [128, DC, 512], BF16)
            wu = P.sbuf("wu", [128, DC, 512], BF16)
            wd = P.sbuf("wd", [128, 4, D], BF16)
            sgt = [P.sbuf("sgt", [128, 512], F32) for _ in range(2)]
            hdt = [P.sbuf("hdt", [128, 512], BF16) for _ in range(2)]
            hdT = [P.sbuf("hdT", [128, 512], BF16) for _ in range(2)]
            yblk = [P.sbuf("yblk", [128, D], F32) for _ in range(2)]
            wst = [P.sbuf("wst", [128, 8192], F32) for _ in range(3)]
            gsrc = ewg.rearrange("l e (p kc) m -> (l e p) (kc m)", kc=16)
            usrc = ewu.rearrange("l e (p kc) m -> (l e p) (kc m)", kc=16)
            dsrc = ewd.rearrange("l e (p kc) m -> (l e p) (kc m)", kc=4)
            wbound = (l + 1) * NEXP * 128 - 1
            for b in range(NSB):
                x_, xT_, sg_, hd_, hT_, y_ = xsb[b % 2], xT[b % 2], sgt[b % 2], hdt[b % 2], hdT[b % 2], yblk[b % 2]
                P.dma('sync', x_[:], xs[b * 128:(b + 1) * 128, :], writes=[x_], key=x_)
                for (stg, src) in zip(wst, (gsrc, usrc, dsrc)):
                    P.dma_fn('gpsimd', lambda e, stg=stg, src=src, b=b: e.indirect_dma_start(
                        out=stg[:, :], out_offset=None, in_=src[:, :],
                        in_offset=bass.IndirectOffsetOnAxis(ap=widx[:, b, 0:1], axis=0), bounds_check=_breg(e, wbound), oob_is_err=False),
                        reads=[widx], writes=[stg], key=stg)
                P.A('copy', reads=[wst[0]], writes=[wg], out=wg[:].rearrange("p a b -> p (a b)"), in_=wst[0][:])
                P.V('tensor_copy', reads=[wst[1]], writes=[wu], out=wu[:].rearrange("p a b -> p (a b)"), in_=wst[1][:])
                P.A('copy', reads=[wst[2]], writes=[wd], out=wd[:, 0:2, :].rearrange("p a b -> p (a b)"), in_=wst[2][:, 0:4096])
                P.V('tensor_copy', reads=[wst[2]], writes=[wd], out=wd[:, 2:4, :].rearrange("p a b -> p (a b)"), in_=wst[2][:, 4096:8192])
                for half in range(2):
                    pb = psb[half]
                    for c8 in range(8):
                        c = half * 8 + c8
                        P.TR(reads=[x_, identb], writes=[pb], out=pb[:, c8 * 128:(c8 + 1) * 128], in_=x_[:, c:D:16],
                             identity=identb[:])
                    evac(xT_[:, half * 1024:(half + 1) * 1024], pb[:, 0:1024], [pb], [xT_])
                pg = next_ps()
                for kc in range(DC):
                    P.MM(reads=[xT_, wg], writes=[pg], out=pg[:, 0:512], lhsT=xT_[:, kc * 128:(kc + 1) * 128], rhs=wg[:, kc, :],
                         start=(kc == 0), stop=(kc == DC - 1))
                pu = next_ps()
                for kc in range(DC):
                    P.MM(reads=[xT_, wu], writes=[pu], out=pu[:, 0:512], lhsT=xT_[:, kc * 128:(kc + 1) * 128], rhs=wu[:, kc, :],
                         start=(kc == 0), stop=(kc == DC - 1))
                P.A('activation', reads=[pg], writes=[sg_], out=sg_[:], in_=pg[:, 0:512], func=AF.Silu)
                P.V('tensor_tensor', reads=[sg_, pu], writes=[hd_], out=hd_[:], in0=sg_[:], in1=pu[:, 0:512], op=ALU.mult)
                pb = psb[b % 2]
                for kc in range(4):
                    P.TR(reads=[hd_, identb], writes=[pb], out=pb[:, kc * 128:(kc + 1) * 128], in_=hd_[:, kc:512:4],
                         identity=identb[:])
                evac(hT_[:], pb[:, 0:512], [pb], [hT_])
                for fg in range(4):
                    py = next_ps()
                    for kc in range(4):
                        P.MM(reads=[hT_, wd], writes=[py], out=py[:, 0:512], lhsT=hT_[:, kc * 128:(kc + 1) * 128],
                             rhs=wd[:, kc, fg * 512:(fg + 1) * 512], start=(kc == 0), stop=(kc == 3))
                    evac(y_[:, fg * 512:(fg + 1) * 512], py[:, 0:512], [py], [y_])
                P.dma('sync', yb[b * 128:(b + 1) * 128, :], y_[:], reads=[y_], key=y_)
            P.barrier()
            P.phase_begin()
            G1 = [P.sbuf("G1", [128, D], F32) for _ in range(2)]
            G2 = [P.sbuf("G2", [128, D], F32) for _ in range(2)]
            h1t = [P.sbuf("h1t", [128, D], F32) for _ in range(2)]
            stt = P.sbuf("stt", [128, 4, 6], F32)
            mv = P.sbuf("mv", [128, 2], F32)
            rs = P.sbuf("rs", [128, 1], F32)
            if last:
                gbc = P.sbuf("gbc", [128, D], F32)
                bbc = P.sbuf("bbc", [128, D], F32)
                P.dma('sync', gbc[:], ln2g_bc[l], writes=[gbc], key=gbc)
                P.dma('sync', bbc[:], ln2b_bc[l], writes=[bbc], key=bbc)
            else:
                hs = [P.sbuf("hs", [128, DC, 128], F32) for _ in range(2)]
                hsb = [P.sbuf("hsb", [128, DC, 128], BF16) for _ in range(2)]
            for g_ in G1 + G2:
                P.V('memset', writes=[g_], ap=g_[:], constant=0.0)
            for blk in range(NBLK):
                z_, g2_, h_ = G1[blk % 2], G2[blk % 2], h1t[blk % 2]
                for k, gt_ in enumerate((z_, g2_)):
                    P.dma_fn('gpsimd', lambda e, gt_=gt_, blk=blk, k=k: e.indirect_dma_start(
                        out=gt_[:, :], out_offset=None, in_=yb[:, :], in_offset=bass.IndirectOffsetOnAxis(ap=dest_i[:, blk, k:k + 1], axis=0),
                        bounds_check=_breg(e, CAP - 1), oob_is_err=False), reads=[dest_i], writes=[gt_], key=gt_)
                P.dma('sync', h_[:], h1tm_f[blk * 128:(blk + 1) * 128, :], writes=[h_], key=h_)
                P.V('tensor_scalar', reads=[z_, cw], writes=[z_], out=z_[:], in0=z_[:], scalar1=cw[:, blk, 0:1], scalar2=None, op0=ALU.mult)
                P.V('scalar_tensor_tensor', reads=[g2_, cw, z_], writes=[z_], out=z_[:], in0=g2_[:], scalar=cw[:, blk, 1:2], in1=z_[:],
                    op0=ALU.mult, op1=ALU.add)
                P.V('scalar_tensor_tensor', reads=[h_, z_], writes=[z_], out=z_[:], in0=h_[:], scalar=ALPHA, in1=z_[:], op0=ALU.mult,
                    op1=ALU.add)
                for j in range(4):
                    P.V('bn_stats', reads=[z_], writes=[stt], out=stt[:, j, :], in_=z_[:, j * 512:(j + 1) * 512])
                P.V('bn_aggr', reads=[stt], writes=[mv], out=mv[:], in_=stt[:].rearrange("p a b -> p (a b)"))
                P.V('tensor_scalar', reads=[mv], writes=[rs], out=rs[:], in0=mv[:, 1:2], scalar1=EPS, scalar2=None, op0=ALU.add)
                P.A('activation', reads=[rs], writes=[rs], out=rs[:], in_=rs[:], func=AF.Sqrt)
                P.V('reciprocal', reads=[rs], writes=[rs], out=rs[:], in_=rs[:])
                P.V('tensor_scalar', reads=[z_, mv, rs], writes=[z_], out=z_[:], in0=z_[:], scalar1=mv[:, 0:1], scalar2=rs[:, 0:1],
                    op0=ALU.subtract, op1=ALU.mult)
                if last:
                    if blk == 0:
                        continue
                    P.V('tensor_tensor', reads=[z_, gbc], writes=[z_], out=z_[:], in0=z_[:], in1=gbc[:], op=ALU.mult)
                    P.V('tensor_tensor', reads=[z_, bbc], writes=[z_], out=z_[:], in0=z_[:], in1=bbc[:], op=ALU.add)
                    P.dma('sync', out[(blk - 1) * 128:blk * 128, :], z_[:], reads=[z_], key=z_)
                else:
                    ho, hb_ = hs[blk % 2], hsb[blk % 2]
                    for c4 in range(4):
                        ps = next_ps()
                        for cc in range(4):
                            c = c4 * 4 + cc
                            P.TR(reads=[z_, identf], writes=[ps], out=ps[:, cc * 128:(cc + 1) * 128], in_=z_[:, c * 128:(c + 1) * 128],
                                 identity=identf[:])
                        for cc in range(4):
                            c = c4 * 4 + cc
                            P.A('activation', reads=[ps, sp], writes=[ho], out=ho[:, c, :], in_=ps[:, cc * 128:(cc + 1) * 128],
                                func=AF.Identity, scale=sp[:, SP_LN2G + c:SP_LN2G + c + 1], bias=sp[:, SP_LN2B + c:SP_LN2B + c + 1])
                    if blk == 0:
                        P.V('memset', writes=[ho], ap=ho[:, :, 0:PAD], constant=0.0)
                    P.V('tensor_copy', reads=[ho], writes=[hb_], out=hb_[:], in_=ho[:])
                    tl, off = blk // 3, (blk % 3) * 128
                    P.dma('sync', hres[tl, :, :, off:off + 128], ho[:], reads=[ho], key=ho)
                    P.dma('sync', hbf[tl, :, :, off:off + 128], hb_[:], reads=[hb_], key=hb_)
            P.barrier()

        class _View:
            def __init__(self, tile, off):
                self.tile = tile
                self.off = off
                self.buf = tile.buf

            def __getitem__(self, idx):
                p, c, n = idx
                assert isinstance(n, slice)
                n0 = (n.start or 0) + self.off
                n1 = (n.stop if n.stop is not None else NT) + self.off
                return self.tile.t[p, c, n0:n1]

        phases = []
        phase_embed()
        done = (stop_after == 'embed')
        for l in range(depth):
            if done:
                break
            P.dma('sync', sp[:], smallp[l], writes=[sp], key=sp)
            for name, fn in (('win', phase_win), ('pool', phase_pool), ('lru', phase_lru), ('attn', phase_attn), ('merge', phase_merge),
                             ('wout', phase_wout), ('router', phase_router)):
                fn(l)
                if stop_after == "%s%d" % (name, l):
                    done = True
                    break
            if done:
                break
            phase_moe(l, last=(l == depth - 1))
        P.emit()
    return nc


def _is_tile_like(x):
    return hasattr(x, 'buf')


def _tab(v, n):
    return np.ascontiguousarray(np.asarray(v, np.float32).reshape(n, 128).T)


def make_consts():
    q = np.arange(128)[:, None]
    s = np.arange(256)[None, :]
    dist = (128 + q - s).astype(np.float32)
    inwin = (dist >= 0) & (dist < 128)
    slopes = (2.0 ** (-8.0 * np.arange(1, 17, dtype=np.float32) / 16)).astype(np.float32)
    ab = np.where(inwin[:, None, :], -slopes[None, :, None] * dist[:, None, :], np.float32(NEG)).astype(np.float32)
    pm = np.where(s < 128 + PAD, np.float32(NEG), np.float32(0.0)).astype(np.float32) * np.ones((128, 1), np.float32)
    invc = np.ones((4, 128, T), np.float32)
    tt = np.arange(T) - PAD
    for g, w in enumerate((2, 4, 8, 16)):
        cnt = np.where(tt >= 0, np.minimum(tt + 1, w), 1).astype(np.float32)
        invc[g] = (1.0 / cnt)[None, :]
    tri = (np.arange(128)[:, None] < np.arange(128)[None, :]).astype(np.float32).astype(ml_dtypes.bfloat16)
    thr = np.ascontiguousarray(np.broadcast_to((128.0 * np.arange(NSB, dtype=np.float32))[None, :], (128, NSB)))
    piota = np.arange(128, dtype=np.float32)[:, None].copy()
    return {
        "c_tri": tri, "c_thr": thr, "c_piota": piota,
        "c_ab": np.ascontiguousarray(ab), "c_pm": np.ascontiguousarray(pm), "c_invcnt": invc,
        "c_identf": np.eye(128, dtype=np.float32), "c_identb": np.eye(128, dtype=np.float32).astype(ml_dtypes.bfloat16),
    }


def make_inputs(inp, b):
    f = lambda k: np.ascontiguousarray(np.asarray(inp[k], np.float32))
    xin = np.zeros((T, D), np.float32)
    xin[PAD:PAD + NMETA] = np.asarray(inp['meta'], np.float32)
    xin[PAD + NMETA:] = np.asarray(inp['x'][b], np.float32)
    smallp = np.zeros((DEPTH, 128, SP_N), np.float32)
    for l in range(DEPTH):
        smallp[l, :, SP_LN1G:SP_LN1G + 16] = _tab(inp['ln1_g'][l], 16)
        smallp[l, :, SP_LN1B:SP_LN1B + 16] = _tab(inp['ln1_b'][l], 16)
        smallp[l, :, SP_LN2G:SP_LN2G + 16] = _tab(inp['ln2_g'][l], 16)
        smallp[l, :, SP_LN2B:SP_LN2B + 16] = _tab(inp['ln2_b'][l], 16)
        smallp[l, :, SP_PSC:SP_PSC + 8] = _tab(inp['pool_scale'][l], 8)
        for j in range(4):
            smallp[l, :, SP_CW + j * 8:SP_CW + j * 8 + 8] = _tab(inp['conv_w'][l][j], 8)
        smallp[l, :, SP_CB:SP_CB + 8] = _tab(inp['conv_b'][l], 8)
        smallp[l, :, SP_BA:SP_BA + 8] = _tab(inp['lru_ba'][l], 8)
        smallp[l, :, SP_BX:SP_BX + 8] = _tab(inp['lru_bx'][l], 8)
        smallp[l, :, SP_LAM:SP_LAM + 8] = _tab(inp['lru_lambda'][l], 8)
        smallp[l, :, SP_SINK:SP_SINK + 16] = np.asarray(inp['attn_sink'][l], np.float32)[None, :]
    embp = np.concatenate([_tab(inp['ln_emb_g'], 16), _tab(inp['ln_emb_b'], 16)], axis=1)
    rbias = np.concatenate([np.asarray(inp['router_grp_b'], np.float32), np.asarray(inp['router_exp_b'], np.float32)], axis=1)[:, None, :]
    rcat = np.concatenate([np.asarray(inp['router_grp_w'], np.float32), np.asarray(inp['router_exp_w'], np.float32)], axis=2)
    rw_tab = np.ascontiguousarray(rcat.reshape(DEPTH, DC, 128, 36).transpose(0, 2, 1, 3).reshape(DEPTH, 128, DC * 36))
    m = {
        "xin": xin, "w_in": f('w_in'), "pool_w": f('pool_w'), "lru_wa": f('lru_wa'), "lru_wx": f('lru_wx'),
        "proj_pool": f('proj_pool'), "proj_attn": f('proj_attn'), "proj_lru": f('proj_lru'), "w_out": f('w_out'),
        "rw_tab": rw_tab, "rbias": np.ascontiguousarray(rbias),
        "exp_w_gate": f('exp_w_gate'), "exp_w_up": f('exp_w_up'), "exp_w_down": f('exp_w_down'),
        "smallp": smallp, "embp": np.ascontiguousarray(embp),
        "ln2g_bc": np.ascontiguousarray(np.broadcast_to(np.asarray(inp['ln2_g'], np.float32)[:, None, :], (DEPTH, 128, D))),
        "ln2b_bc": np.ascontiguousarray(np.broadcast_to(np.asarray(inp['ln2_b'], np.float32)[:, None, :], (DEPTH, 128, D))),
    }
    m.update(make_consts())
    return m


_NC_CACHE = {}


def kernel(**inputs):
    B = inputs['x'].shape[0]
    if 'nc' not in _NC_CACHE:
        _NC_CACHE['nc'] = build_nc()
    nc = _NC_CACHE['nc']
    shared = None
    in_maps = []
    for b in range(B):
        m = make_inputs(inputs, b) if shared is None else dict(shared, xin=None)
        if shared is None:
            shared = m
        else:
            xin = np.zeros((T, D), np.float32)
            xin[PAD:PAD + NMETA] = np.asarray(inputs['meta'], np.float32)
            xin[PAD + NMETA:] = np.asarray(inputs['x'][b], np.float32)
            m['xin'] = xin
        in_maps.append(m)
    res = run_bass_kernel_spmd(nc, in_maps, core_ids=list(range(B)))
    return np.stack([np.asarray(r["out"], np.float32) for r in res.results], axis=0)
```

```python
import contextlib
import numpy as np
import ml_dtypes
import concourse.bass as bass
import concourse.mybir as mybir
from concourse.bass_utils import run_bass_kernel_spmd

F32 = mybir.dt.float32
BF16 = mybir.dt.bfloat16
AF = mybir.ActivationFunctionType
ALU = mybir.AluOpType
AX = mybir.AxisListType

COMPUTE = ('scalar', 'vector', 'tensor', 'gpsimd')
QUEUES = ('sync', 'scalar', 'vector', 'tensor', 'gpsimd')
DT_SIZE = {F32: 4, BF16: 2, mybir.dt.int32: 4}

D = 2048
DC = 16
SEQ = 4096
NMETA = 16
PAD = 112
T = 4224
NT = 384
NTILES = 11
NBLK = 33
DEPTH = 2
NCH_COLS = 84
C_POOL, C_Q, C_K, C_V, C_LX, C_LY, C_G = 0, 8, 16, 18, 20, 28, 36
NEXP = 32
ALPHA = (2.0 * DEPTH) ** 0.25
EPS = 1e-5
NEG = -1e30
NSB = 97
CAP = NSB * 128
BIGI = 1048576.0
I32 = mybir.dt.int32
SP_LN1G, SP_LN1B, SP_LN2G, SP_LN2B = 0, 16, 32, 48
SP_PSC, SP_CW, SP_CB, SP_BA, SP_BX, SP_LAM, SP_SINK = 64, 72, 104, 112, 120, 128, 136
SP_N = 152


class Buf:
    __slots__ = ('name', 'w', 'r', 'sem')

    def __init__(self, name):
        self.name = name
        self.w = None
        self.r = []
        self.sem = None


class Op:
    __slots__ = ('q', 'fn', 'deps', 'is_dma', 'sem', 'val', 'needed')

    def __init__(self, q, fn, is_dma):
        self.q = q
        self.fn = fn
        self.deps = []
        self.is_dma = is_dma
        self.sem = None
        self.val = 0
        self.needed = False


class Tile:
    __slots__ = ('t', 'buf')

    def __init__(self, t, name):
        self.t = t
        self.buf = Buf(name)

    def __getitem__(self, idx):
        return self.t[idx]


def _b(x):
    return getattr(x, 'buf', x)


_BREG = {}


def _breg(e, val):
    k = (id(e), int(val))
    if k not in _BREG:
        _BREG[k] = e.to_reg(int(val))
    return _BREG[k]


class Prog:
    SB_LO = 20480
    SB_HI = 222 * 1024

    def __init__(self, nc, n_dma_sems=56):
        self.nc = nc
        self.ops = {q: [] for q in QUEUES}
        self.esem = {}
        self.dma_pool = []
        self.n_dma_sems = n_dma_sems
        self.pool_idx = 0
        self.sb_off = self.SB_LO
        self.sb_base = self.SB_LO
        self.sb_max = 0
        self.uid = 0
        self.live = []

    def setup(self, stack):
        for e in COMPUTE:
            self.esem[e] = stack.enter_context(self.nc.semaphore("es_" + e))
        for i in range(self.n_dma_sems):
            self.dma_pool.append([stack.enter_context(self.nc.semaphore("ds%d" % i)), 0])

    def sbuf(self, name, shape, dtype):
        nbytes = int(np.prod(shape[1:])) * DT_SIZE[dtype]
        off = (self.sb_off + 63) // 64 * 64
        self.uid += 1
        t = self.nc.alloc_sbuf_tensor_at("%s_%d" % (name, self.uid), list(shape), dtype, offset=off)
        self.sb_off = off + nbytes
        self.sb_max = max(self.sb_max, self.sb_off)
        assert self.sb_off <= self.SB_HI, ("SBUF overflow", name, self.sb_off)
        return Tile(t, name)

    def phase_begin(self):
        self.sb_off = self.sb_base

    def persist_mark(self):
        self.sb_base = self.sb_off

    def _hazards(self, op, reads, writes):
        deps = []
        strong = set()
        for b in reads:
            b = _b(b)
            if b.w is not None:
                deps.append(b.w)
                strong.add(id(b.w))
        for b in writes:
            b = _b(b)
            if b.w is not None:
                deps.append(b.w)
                strong.add(id(b.w))
            deps.extend(b.r)
        for b in reads:
            _b(b).r.append(op)
        for b in writes:
            b = _b(b)
            b.w = op
            b.r = []
        seen = set()
        for d in deps:
            if d is op or id(d) in seen:
                continue
            seen.add(id(d))
            if (not d.is_dma) and (not op.is_dma) and d.q == op.q:
                if d.q == 'tensor' or id(d) not in strong:
                    continue
            if not d.is_dma:
                d.needed = True
            op.deps.append(d)

    def op(self, eng, fn, reads=(), writes=()):
        o = Op(eng, fn, False)
        self._hazards(o, reads, writes)
        self.ops[eng].append(o)
        return o

    def dma(self, q, out, in_, reads=(), writes=(), key=None):
        o = Op(q, (lambda e, out=out, in_=in_: e.dma_start(out=out, in_=in_)), True)
        b = _b(key)
        if b.sem is None:
            assert self.pool_idx < len(self.dma_pool), "out of dma sems"
            b.sem = self.dma_pool[self.pool_idx]
            self.pool_idx += 1
            self.live.append(b)
        b.sem[1] += 16
        o.sem = b.sem[0]
        o.val = b.sem[1]
        self._hazards(o, reads, writes)
        self.ops[q].append(o)
        return o

    def dma_fn(self, q, fn, reads=(), writes=(), key=None):
        o = Op(q, fn, True)
        b = _b(key)
        if b.sem is None:
            assert self.pool_idx < len(self.dma_pool), "out of dma sems"
            b.sem = self.dma_pool[self.pool_idx]
            self.pool_idx += 1
            self.live.append(b)
        b.sem[1] += 16
        o.sem = b.sem[0]
        o.val = b.sem[1]
        self._hazards(o, reads, writes)
        self.ops[q].append(o)
        return o

    def barrier(self):
        last = []
        for e in COMPUTE:
            for o in reversed(self.ops[e]):
                if o.fn is not None and not o.is_dma:
                    o.needed = True
                    last.append(o)
                    break
        dmas = []
        for i in range(self.pool_idx):
            s, c = self.dma_pool[i]
            if c > 0:
                d = Op('sync', None, True)
                d.sem = s
                d.val = c
                dmas.append(d)
        for q in QUEUES:
            o = Op(q, None, False)
            o.deps = [d for d in last if d.q != q] + dmas
            self.ops[q].append(o)
        for b in self.live:
            b.sem = None
        self.live = []
        self.pool_idx = 0

    def emit(self):
        nc = self.nc
        for e in COMPUTE:
            c = 0
            for o in self.ops[e]:
                if o.needed and not o.is_dma:
                    c += 1
                    o.sem = self.esem[e]
                    o.val = c
        with nc.Block() as block:
            for q in QUEUES:
                lst = self.ops[q]
                if not lst:
                    continue

                def body(eng, lst=lst):
                    waited = {}
                    for o in lst:
                        for d in o.deps:
                            k = id(d.sem)
                            if waited.get(k, 0) >= d.val:
                                continue
                            waited[k] = d.val
                            eng.wait_ge(d.sem, d.val)
                        if o.fn is None:
                            continue
                        ins = o.fn(eng)
                        if o.is_dma:
                            ins.then_inc(o.sem, 16)
                        elif o.needed:
                            ins.then_inc(o.sem, 1)

                getattr(block, q)(body)

    def V(self, name, reads=(), writes=(), **kw):
        return self.op('vector', lambda e: getattr(e, name)(**kw), reads, writes)

    def A(self, name, reads=(), writes=(), **kw):
        return self.op('scalar', lambda e: getattr(e, name)(**kw), reads, writes)

    def G(self, name, reads=(), writes=(), **kw):
        return self.op('gpsimd', lambda e: getattr(e, name)(**kw), reads, writes)

    def MM(self, reads=(), writes=(), **kw):
        return self.op('tensor', lambda e: e.matmul(**kw), reads, writes)

    def TR(self, reads=(), writes=(), **kw):
        return self.op('tensor', lambda e: e.transpose(**kw), reads, writes)


def build_nc(depth=DEPTH, debug=False, stop_after=None):
    _BREG.clear()
    nc = bass.Bass("TRN2", target_bir_lowering=False)
    dbg_set = set(debug) if debug else set()

    def scr(name, shape, dt):
        return nc.dram_tensor(name, list(shape), dt, kind=("ExternalOutput" if name in dbg_set else "Internal")).ap()

    def din(name, shape, dt=F32):
        return nc.dram_tensor(name, list(shape), dt, kind="ExternalInput").ap()

    xin = din("xin", [T, D])
    w_in = din("w_in", [DEPTH, D, 10752])
    pool_w = din("pool_w", [DEPTH, 4, 256, 256])
    lru_wa = din("lru_wa", [DEPTH, 4, 256, 256])
    lru_wx = din("lru_wx", [DEPTH, 4, 256, 256])
    proj = [din("proj_pool", [DEPTH, 1024, D]), din("proj_attn", [DEPTH, 1024, D]), din("proj_lru", [DEPTH, 1024, D])]
    w_out = din("w_out", [DEPTH, D, D])
    rw_tab = din("rw_tab", [DEPTH, 128, DC * 36])
    rbias = din("rbias", [DEPTH, 1, 36])
    ewg = din("exp_w_gate", [DEPTH, NEXP, D, 512])
    ewu = din("exp_w_up", [DEPTH, NEXP, D, 512])
    ewd = din("exp_w_down", [DEPTH, NEXP, 512, D])
    smallp = din("smallp", [DEPTH, 128, SP_N])
    embp = din("embp", [128, 32])
    c_ab = din("c_ab", [128, 16, 256])
    c_pm = din("c_pm", [128, 256])
    c_invcnt = din("c_invcnt", [4, 128, T])
    c_identf = din("c_identf", [128, 128])
    c_identb = din("c_identb", [128, 128], BF16)
    c_tri = din("c_tri", [128, 128], BF16)
    c_thr = din("c_thr", [128, NSB])
    c_piota = din("c_piota", [128, 1])
    ln2g_bc = din("ln2g_bc", [DEPTH, 128, D])
    ln2b_bc = din("ln2b_bc", [DEPTH, 128, D])

    out = nc.dram_tensor("out", [SEQ, D], F32, kind="ExternalOutput").ap()
    hres = scr("hres", [NTILES, 128, DC, NT], F32)
    hbf = scr("hbf", [NTILES, 128, DC, NT], BF16)
    cols = scr("cols", [NTILES, 128, NCH_COLS, NT], F32)
    mix = scr("mix", [NTILES, 128, 24, NT], BF16)
    merged = scr("merged", [NTILES, 128, DC, NT], BF16)
    h1tm_f = scr("h1tm_f", [T, D], F32)
    h1tm_b = scr("h1tm_b", [T, D], BF16)
    xs = scr("xs", [CAP, D], BF16)
    yb = scr("yb", [CAP, D], F32)

    def rowap(x, c, p0=0, p1=128):
        return x.rearrange("t p c n -> p t c n")[p0:p1, :, c, :]

    def rowview(ap2d):
        return ap2d.rearrange("p (t n) -> p t n", n=NT)

    with contextlib.ExitStack() as st:
        P = Prog(nc)
        P.setup(st)
        psf = [Tile(st.enter_context(nc.psum_tensor("psf%d" % i, [128, 512], F32)), "psf%d" % i) for i in range(6)]
        psb = [Tile(st.enter_context(nc.psum_tensor("psb%d" % i, [128, 1024], BF16)), "psb%d" % i) for i in range(2)]
        psi = [0]

        def next_ps():
            psi[0] += 1
            return psf[psi[0] % 6]

        identf = P.sbuf("identf", [128, 128], F32)
        identb = P.sbuf("identb", [128, 128], BF16)
        onesf = P.sbuf("onesf", [128, 128], F32)
        embt = P.sbuf("embt", [128, 32], F32)
        sp = P.sbuf("sp", [128, SP_N], F32)
        lamc = P.sbuf("lamc", [128, 16], F32)
        Lall = P.sbuf("Lall", [128, NBLK, 36], F32)
        dest_i = P.sbuf("dest_i", [128, NBLK, 2], I32)
        cw = P.sbuf("cw", [128, NBLK, 2], F32)
        widx = P.sbuf("widx", [128, NSB, 16], I32)
        didx = P.sbuf("didx", [128, NSB, 4], I32)
        P.persist_mark()
        P.dma('sync', identf[:], c_identf, writes=[identf], key=identf)
        P.dma('sync', identb[:], c_identb, writes=[identb], key=identb)
        P.dma('sync', embt[:], embp, writes=[embt], key=embt)
        P.V('memset', writes=[onesf], ap=onesf[:], constant=1.0)

        evac_i = [0]

        def evac(out_ap, in_ap, reads, writes):
            evac_i[0] += 1
            if evac_i[0] % 2 == 0:
                P.A('copy', reads=reads, writes=writes, out=out_ap, in_=in_ap)
            else:
                P.V('tensor_copy', reads=reads, writes=writes, out=out_ap, in_=in_ap)

        def emit_ln(hz, n, gcol, bcol, tmp, zero_cols=0, hb=None):
            ps1 = next_ps()
            ps2 = next_ps()
            for mc in range(DC):
                P.MM(reads=[onesf, hz], writes=[ps1], out=ps1[:, 0:n], lhsT=onesf[:], rhs=hz[:, mc, 0:n],
                     start=(mc == 0), stop=(mc == DC - 1))
            for mc in range(DC):
                zs = tmp['zsq'][mc % 2]
                P.A('activation', reads=[hz], writes=[zs], out=zs[:, 0:n], in_=hz[:, mc, 0:n], func=AF.Square)
                P.MM(reads=[onesf, zs], writes=[ps2], out=ps2[:, 0:n], lhsT=onesf[:], rhs=zs[:, 0:n],
                     start=(mc == 0), stop=(mc == DC - 1))
            mean, rstd = tmp['mean'], tmp['rstd']
            P.A('mul', reads=[ps1], writes=[mean], out=mean[:, 0:n], in_=ps1[:, 0:n], mul=1.0 / D)
            P.V('tensor_tensor', reads=[mean], writes=[rstd], out=rstd[:, 0:n], in0=mean[:, 0:n], in1=mean[:, 0:n], op=ALU.mult)
            P.V('scalar_tensor_tensor', reads=[ps2, rstd], writes=[rstd], out=rstd[:, 0:n], in0=ps2[:, 0:n], scalar=1.0 / D,
                in1=rstd[:, 0:n], op0=ALU.mult, op1=ALU.subtract)
            P.V('tensor_scalar', reads=[rstd], writes=[rstd], out=rstd[:, 0:n], in0=rstd[:, 0:n], scalar1=0.0, scalar2=EPS,
                op0=ALU.max, op1=ALU.add)
            P.A('activation', reads=[rstd], writes=[rstd], out=rstd[:, 0:n], in_=rstd[:, 0:n], func=AF.Sqrt)
            P.V('reciprocal', reads=[rstd], writes=[rstd], out=rstd[:, 0:n], in_=rstd[:, 0:n])
            for mc in range(DC):
                P.V('tensor_tensor', reads=[hz, mean], writes=[hz], out=hz[:, mc, 0:n], in0=hz[:, mc, 0:n], in1=mean[:, 0:n], op=ALU.subtract)
                P.V('tensor_tensor', reads=[hz, rstd], writes=[hz], out=hz[:, mc, 0:n], in0=hz[:, mc, 0:n], in1=rstd[:, 0:n], op=ALU.mult)
                P.A('activation', reads=[hz, sp, embt], writes=[hz], out=hz[:, mc, 0:n], in_=hz[:, mc, 0:n], func=AF.Identity,
                    scale=gcol(mc), bias=bcol(mc))
            if zero_cols:
                P.V('memset', writes=[hz], ap=hz[:, :, 0:zero_cols], constant=0.0)
            if hb is not None:
                P.G('tensor_copy', reads=[hz], writes=[hb], out=hb[:, :, 0:n], in_=hz[:, :, 0:n])

        def phase_embed():
            P.phase_begin()
            xt = [P.sbuf("xt", [128, D], F32) for _ in range(2)]
            xn = [P.sbuf("xn", [128, D], F32) for _ in range(2)]
            stt = P.sbuf("stt", [128, 4, 6], F32)
            mv = P.sbuf("mv", [128, 2], F32)
            rs = P.sbuf("rs", [128, 1], F32)
            hs = [P.sbuf("hs", [128, DC, 128], F32) for _ in range(2)]
            hsb = [P.sbuf("hsb", [128, DC, 128], BF16) for _ in range(2)]
            for blk in range(NBLK):
                x_ = xt[blk % 2]
                n_ = xn[blk % 2]
                h_ = hs[blk % 2]
                hb_ = hsb[blk % 2]
                P.dma('sync', x_[:], xin[blk * 128:(blk + 1) * 128, :], writes=[x_], key=x_)
                for j in range(4):
                    P.V('bn_stats', reads=[x_], writes=[stt], out=stt[:, j, :], in_=x_[:, j * 512:(j + 1) * 512])
                P.V('bn_aggr', reads=[stt], writes=[mv], out=mv[:], in_=stt[:].rearrange("p a b -> p (a b)"))
                P.V('tensor_scalar', reads=[mv], writes=[rs], out=rs[:], in0=mv[:, 1:2], scalar1=EPS, scalar2=None, op0=ALU.add)
                P.A('activation', reads=[rs], writes=[rs], out=rs[:], in_=rs[:], func=AF.Sqrt)
                P.V('reciprocal', reads=[rs], writes=[rs], out=rs[:], in_=rs[:])
                P.V('tensor_scalar', reads=[x_, mv, rs], writes=[n_], out=n_[:], in0=x_[:], scalar1=mv[:, 0:1], scalar2=rs[:, 0:1],
                    op0=ALU.subtract, op1=ALU.mult)
                import os
                CUT = int(os.environ.get('EMBED_CUT', '9'))
                for c4 in range(4):
                    ps = next_ps()
                    for cc in range(4):
                        c = c4 * 4 + cc
                        if CUT >= 1:
                            P.TR(reads=[n_, identf], writes=[ps], out=ps[:, cc * 128:(cc + 1) * 128], in_=n_[:, c * 128:(c + 1) * 128],
                                 identity=identf[:])
                    for cc in range(4):
                        c = c4 * 4 + cc
                        if CUT >= 2:
                            P.A('activation', reads=[ps, embt], writes=[h_], out=h_[:, c, :], in_=ps[:, cc * 128:(cc + 1) * 128],
                                func=AF.Identity, scale=embt[:, c:c + 1], bias=embt[:, 16 + c:17 + c])
                        else:
                            P.V('tensor_copy', reads=[n_], writes=[h_], out=h_[:, c, :], in_=n_[:, c * 128:(c + 1) * 128])
                if blk == 0:
                    P.V('memset', writes=[h_], ap=h_[:, :, 0:PAD], constant=0.0)
                P.V('tensor_copy', reads=[h_], writes=[hb_], out=hb_[:], in_=h_[:])
                tl, off = blk // 3, (blk % 3) * 128
                P.dma('sync', hres[tl, :, :, off:off + 128], h_[:], reads=[h_], key=h_)
                P.dma('sync', hbf[tl, :, :, off:off + 128], hb_[:], reads=[hb_], key=hb_)
            P.barrier()

        def phase_win(l):
            P.phase_begin()
            wb = [P.sbuf("wb", [128, DC, 512], BF16) for _ in range(2)]
            hb = [P.sbuf("hb", [128, DC, NT], BF16) for _ in range(3)]
            ot = [P.sbuf("ot", [128, 4, NT], F32) for _ in range(2)]
            colsdep = [Buf("colsdep%d" % mg) for mg in range(21)]
            wsrc = w_in[l].rearrange("(kc p) m -> p kc m", p=128)

            U = [P.sbuf("U", [128, 16 + T], F32) for _ in range(2)]
            Wk = [P.sbuf("Wk", [128, 16 + T], F32) for _ in range(2)]
            icns = P.sbuf("icns", [128, 4, 16], F32)
            dlt = [P.sbuf("dlt", [128, T], BF16) for _ in range(2)]
            po = [P.sbuf("po", [128, T], BF16) for _ in range(2)]
            pw = P.sbuf("pw", [128, 2, 256], BF16)

            def pool_gen():
                for u_ in U + Wk:
                    P.V('memset', writes=[u_], ap=u_[:, 0:16], constant=0.0)
                for g in range(4):
                    P.dma('sync', icns[:, g, :], c_invcnt[g][:, PAD:PAD + 16], writes=[icns], key=icns)
                yield
                for g in range(4):
                    wwin = float(2 << g)
                    P.dma('gpsimd', pw[:], pool_w[l, g].rearrange("(kc p) m -> p kc m", p=128), writes=[pw], key=pw)
                    for half in range(2):
                        c = 2 * g + half
                        u_ = U[c % 2]
                        P.dma('sync', rowview(u_[:, 16:16 + T]), rowap(cols, C_POOL + c), writes=[u_, colsdep[c // 4]], key=u_)
                        yield
                        src = u_
                        for j in range(g + 1):
                            sh = 1 << j
                            dst = Wk[j % 2]
                            P.V('tensor_tensor', reads=[src], writes=[dst], out=dst[:, 16:16 + T], in0=src[:, 16:16 + T],
                                in1=src[:, 16 - sh:16 - sh + T], op=ALU.add)
                            src = dst
                            yield
                        other = Wk[(g + 1) % 2]
                        P.V('tensor_scalar', reads=[src], writes=[other], out=other[:, 16:16 + T], in0=src[:, 16:16 + T], scalar1=1.0 / wwin,
                            scalar2=None, op0=ALU.mult)
                        P.V('tensor_tensor', reads=[src, icns], writes=[other], out=other[:, 16 + PAD:16 + PAD + 16],
                            in0=src[:, 16 + PAD:16 + PAD + 16], in1=icns[:, g, :], op=ALU.mult)
                        yield
                        P.V('tensor_tensor', reads=[other, u_], writes=[dlt[half]], out=dlt[half][:], in0=other[:, 16:16 + T],
                            in1=u_[:, 16:16 + T], op=ALU.subtract)
                        yield
                    for mo in range(2):
                        co = 2 * g + mo
                        po_ = po[co % 2]
                        for tl in range(NTILES):
                            ps = next_ps()
                            for kc in range(2):
                                P.MM(reads=[pw, dlt[kc]], writes=[ps], out=ps[:, 0:NT], lhsT=pw[:, kc, mo * 128:(mo + 1) * 128],
                                     rhs=dlt[kc][:, tl * NT:(tl + 1) * NT], start=(kc == 0), stop=(kc == 1))
                            P.A('activation', reads=[ps, sp], writes=[po_], out=po_[:, tl * NT:(tl + 1) * NT], in_=ps[:, 0:NT],
                                func=AF.Identity, scale=sp[:, SP_PSC + co:SP_PSC + co + 1])
                            yield
                        P.dma('gpsimd', rowap(mix, co), rowview(po_[:]), reads=[po_], key=po_)
                        yield

            gen = pool_gen()
            it = 0

            def load_w(mg):
                w_ = wb[mg % 2]
                for half in range(2):
                    P.dma('gpsimd', w_[:, half * 8:(half + 1) * 8, :], wsrc[:, half * 8:(half + 1) * 8, mg * 512:(mg + 1) * 512],
                          writes=[w_], key=w_)
            load_w(0)
            for mg in range(21):
                w_ = wb[mg % 2]
                if mg + 1 < 21:
                    load_w(mg + 1)
                for tl in range(NTILES):
                    h_ = hb[it % 3]
                    o_ = ot[it % 2]
                    it += 1
                    P.dma('sync', h_[:], hbf[tl], writes=[h_], key=h_)
                    for mc in range(4):
                        ps = next_ps()
                        for kc in range(DC):
                            P.MM(reads=[w_, h_], writes=[ps], out=ps[:, 0:NT], lhsT=w_[:, kc, mc * 128:(mc + 1) * 128], rhs=h_[:, kc, :],
                                 start=(kc == 0), stop=(kc == DC - 1))
                        evac(o_[:, mc, :], ps[:, 0:NT], [ps], [o_])
                    P.dma('gpsimd', cols[tl, :, mg * 4:(mg + 1) * 4, :], o_[:], reads=[o_, colsdep[mg]], key=o_)
                    if mg >= 2:
                        next(gen, None)
            for _ in gen:
                pass
            P.barrier()

        def phase_pool(l):
            return

        def phase_lru(l):
            P.phase_begin()
            X = P.sbuf("X", [128, 3 + T], F32)
            XC = [P.sbuf("XC", [128, T], F32) for _ in range(2)]
            xcb = [P.sbuf("xcb", [128, T], BF16) for _ in range(2)]
            GA = P.sbuf("GA", [128, T], F32)
            GX = P.sbuf("GX", [128, T], F32)
            AR = P.sbuf("AR", [128, T], F32)
            TB = P.sbuf("TB", [128, T], F32)
            lo = P.sbuf("lo", [128, T], BF16)
            wa = P.sbuf("wa", [128, 2, 256], BF16)
            wx = P.sbuf("wx", [128, 2, 256], BF16)
            P.A('activation', reads=[sp], writes=[lamc], out=lamc[:, 0:8], in_=sp[:, SP_LAM:SP_LAM + 8], func=AF.Exp, scale=-1.0)
            P.A('activation', reads=[lamc], writes=[lamc], out=lamc[:, 0:8], in_=lamc[:, 0:8], func=AF.Ln, bias=1.0)
            P.A('mul', reads=[lamc], writes=[lamc], out=lamc[:, 8:16], in_=lamc[:, 0:8], mul=-16.0)
            P.A('mul', reads=[lamc], writes=[lamc], out=lamc[:, 0:8], in_=lamc[:, 0:8], mul=-8.0)
            P.V('memset', writes=[X], ap=X[:, 0:3], constant=0.0)
            for blk in range(4):
                P.dma('gpsimd', wa[:], lru_wa[l, blk].rearrange("(kc p) m -> p kc m", p=128), writes=[wa], key=wa)
                P.dma('gpsimd', wx[:], lru_wx[l, blk].rearrange("(kc p) m -> p kc m", p=128), writes=[wx], key=wx)
                for half in range(2):
                    c = 2 * blk + half
                    xc = XC[half]
                    P.dma('sync', rowview(X[:, 3:3 + T]), rowap(cols, C_LX + c), writes=[X], key=X)
                    P.V('tensor_scalar', reads=[X, sp], writes=[xc], out=xc[:], in0=X[:, 0:T], scalar1=sp[:, SP_CW + c:SP_CW + c + 1],
                        scalar2=sp[:, SP_CB + c:SP_CB + c + 1], op0=ALU.mult, op1=ALU.add)
                    for j in range(1, 4):
                        P.V('scalar_tensor_tensor', reads=[X, sp, xc], writes=[xc], out=xc[:], in0=X[:, j:j + T],
                            scalar=sp[:, SP_CW + j * 8 + c:SP_CW + j * 8 + c + 1], in1=xc[:], op0=ALU.mult, op1=ALU.add)
                    P.G('tensor_copy', reads=[xc], writes=[xcb[half]], out=xcb[half][:], in_=xc[:])
                for mo in range(2):
                    co = 2 * blk + mo
                    for tl in range(NTILES):
                        sl = slice(tl * NT, (tl + 1) * NT)
                        psa = next_ps()
                        for kc in range(2):
                            P.MM(reads=[wa, xcb[kc]], writes=[psa], out=psa[:, 0:NT], lhsT=wa[:, kc, mo * 128:(mo + 1) * 128],
                                 rhs=xcb[kc][:, sl], start=(kc == 0), stop=(kc == 1))
                        P.A('activation', reads=[psa, sp], writes=[GA], out=GA[:, sl], in_=psa[:, 0:NT], func=AF.Sigmoid,
                            bias=sp[:, SP_BA + co:SP_BA + co + 1])
                        psx = next_ps()
                        for kc in range(2):
                            P.MM(reads=[wx, xcb[kc]], writes=[psx], out=psx[:, 0:NT], lhsT=wx[:, kc, mo * 128:(mo + 1) * 128],
                                 rhs=xcb[kc][:, sl], start=(kc == 0), stop=(kc == 1))
                        P.A('activation', reads=[psx, sp], writes=[GX], out=GX[:, sl], in_=psx[:, 0:NT], func=AF.Sigmoid,
                            bias=sp[:, SP_BX + co:SP_BX + co + 1])
                    P.A('activation', reads=[GA, lamc], writes=[AR], out=AR[:], in_=GA[:], func=AF.Exp, scale=lamc[:, co:co + 1])
                    P.A('activation', reads=[GA, lamc], writes=[TB], out=TB[:], in_=GA[:], func=AF.Exp, scale=lamc[:, 8 + co:9 + co])
                    P.V('tensor_scalar', reads=[TB], writes=[TB], out=TB[:], in0=TB[:], scalar1=-1.0, scalar2=1.0, op0=ALU.mult, op1=ALU.add)
                    P.V('tensor_scalar', reads=[TB], writes=[TB], out=TB[:], in0=TB[:], scalar1=0.0, scalar2=None, op0=ALU.max)
                    P.A('activation', reads=[TB], writes=[TB], out=TB[:], in_=TB[:], func=AF.Sqrt)
                    P.V('tensor_tensor', reads=[TB, GX], writes=[TB], out=TB[:], in0=TB[:], in1=GX[:], op=ALU.mult)
                    P.V('tensor_tensor', reads=[TB, XC[mo]], writes=[TB], out=TB[:], in0=TB[:], in1=XC[mo][:], op=ALU.mult)
                    P.V('memset', writes=[TB], ap=TB[:, 0:PAD], constant=0.0)
                    P.V('tensor_tensor_scan', reads=[AR, TB], writes=[GA], out=GA[:], data0=AR[:], data1=TB[:], initial=0.0,
                        op0=ALU.mult, op1=ALU.add)
                    P.dma('sync', rowview(GX[:]), rowap(cols, C_LY + co), writes=[GX], key=GX)
                    P.A('activation', reads=[GX], writes=[GX], out=GX[:], in_=GX[:], func=AF.Gelu)
                    P.V('tensor_tensor', reads=[GA, GX], writes=[lo], out=lo[:], in0=GA[:], in1=GX[:], op=ALU.mult)
                    P.dma('gpsimd', rowap(mix, 16 + co), rowview(lo[:]), reads=[lo], key=lo)
            P.barrier()

        def phase_attn(l):
            P.phase_begin()
            AB = P.sbuf("AB", [128, 16, 256], F32)
            PM = P.sbuf("PM", [128, 256], F32)
            VT = P.sbuf("VT", [128, NBLK + 1, 256], BF16)
            kT = P.sbuf("kT", [64, 4, 128 + T], BF16)
            vrow = P.sbuf("vrow", [128, T], BF16)
            qT = [P.sbuf("qT", [64, T], BF16) for _ in range(2)]
            AO = [P.sbuf("AO", [128, T], BF16) for _ in range(2)]
            ssb = [P.sbuf("ssb", [128, 256], F32) for _ in range(4)]
            pn = [P.sbuf("pn", [128, 256], BF16) for _ in range(4)]
            pT = [P.sbuf("pT", [128, 256], BF16) for _ in range(4)]
            sm = [P.sbuf("sm", [128, 8], F32) for _ in range(4)]
            psS = [psf[0], psf[1]]
            psO = [psf[2], psf[3]]
            P.dma('sync', AB[:], c_ab, writes=[AB], key=AB)
            P.dma('sync', PM[:], c_pm, writes=[PM], key=PM)
            P.V('memset', writes=[VT], ap=VT[:, 0, :], constant=0.0)
            P.V('memset', writes=[kT], ap=kT[:, :, 0:128], constant=0.0)
            for j in range(4):
                P.dma('gpsimd', rowview(kT[0:64, j, 128:128 + T]), rowap(cols, C_K + j // 2, (j % 2) * 64, (j % 2) * 64 + 64),
                      writes=[kT], key=kT)
            for vc in range(2):
                P.dma('gpsimd', rowview(vrow[:]), rowap(cols, C_V + vc), writes=[vrow], key=vrow)
                for blk in range(NBLK):
                    pb = psb[blk % 2]
                    P.TR(reads=[vrow, identb], writes=[pb], out=pb[:, 0:128], in_=vrow[:, blk * 128:(blk + 1) * 128], identity=identb[:])
                    evac(VT[:, blk + 1, vc * 128:(vc + 1) * 128], pb[:, 0:128], [pb], [VT])
            P.barrier()

            class Reg:
                def __init__(self, ap):
                    self.ap = ap
                    self.buf = Buf("reg")

            DEP = 4
            rS = [Reg(psf[k // 2][:, (k % 2) * 256:(k % 2) * 256 + 256]) for k in range(4)]
            rO = [psf[2][:, k * 128:(k + 1) * 128] for k in range(4)]
            rOb = [Reg(None) for _ in range(4)]
            rB = [Reg(psb[k // 4][:, (k % 4) * 256:(k % 4) * 256 + 256]) for k in range(8)]
            iters = [(h, blk) for h in range(16) for blk in range(NBLK)]
            NI = len(iters)

            def stageA(i):
                h, blk = iters[i]
                j = h // 4
                q_ = qT[h % 2]
                p0 = (h % 2) * 64
                if blk == 0:
                    P.dma('gpsimd', rowview(q_[0:64, :]), rowap(cols, C_Q + h // 2, p0, p0 + 64), writes=[q_], key=q_)
                s_, sm_, pS = ssb[i % DEP], sm[i % DEP], rS[i % 4]
                P.MM(reads=[q_, kT], writes=[pS], out=pS.ap, lhsT=q_[0:64, blk * 128:(blk + 1) * 128],
                     rhs=kT[0:64, j, blk * 128:blk * 128 + 256], start=True, stop=True)
                P.V('scalar_tensor_tensor', reads=[pS, AB], writes=[s_], out=s_[:], in0=pS.ap, scalar=0.125, in1=AB[:, h, :],
                    op0=ALU.mult, op1=ALU.add)
                if blk == 0:
                    P.V('tensor_tensor', reads=[s_, PM], writes=[s_], out=s_[:], in0=s_[:], in1=PM[:], op=ALU.add)
                if blk == 1:
                    P.V('tensor_tensor', reads=[s_, PM], writes=[s_], out=s_[:, 0:PAD], in0=s_[:, 0:PAD], in1=PM[:, 0:PAD], op=ALU.add)
                P.V('reduce_max', reads=[s_], writes=[sm_], out=sm_[:, 0:1], in_=s_[:], axis=AX.X)
                P.V('tensor_scalar', reads=[sm_, sp], writes=[sm_], out=sm_[:, 1:2], in0=sm_[:, 0:1],
                    scalar1=sp[:, SP_SINK + h:SP_SINK + h + 1], scalar2=-1.0, op0=ALU.max, op1=ALU.mult)

            def stageB_act(i):
                h, blk = iters[i]
                s_, sm_ = ssb[i % DEP], sm[i % DEP]
                P.A('activation', reads=[s_, sm_], writes=[s_, sm_], out=s_[:], in_=s_[:], func=AF.Exp, bias=sm_[:, 1:2],
                    accum_out=sm_[:, 2:3])
                P.A('activation', reads=[sp, sm_], writes=[sm_], out=sm_[:, 3:4], in_=sp[:, SP_SINK + h:SP_SINK + h + 1], func=AF.Exp,
                    bias=sm_[:, 1:2])

            def stageB_dve(i):
                s_, sm_, pn_ = ssb[i % DEP], sm[i % DEP], pn[i % DEP]
                P.V('tensor_tensor', reads=[sm_], writes=[sm_], out=sm_[:, 4:5], in0=sm_[:, 2:3], in1=sm_[:, 3:4], op=ALU.add)
                P.V('reciprocal', reads=[sm_], writes=[sm_], out=sm_[:, 5:6], in_=sm_[:, 4:5])
                P.V('tensor_scalar', reads=[s_, sm_], writes=[pn_], out=pn_[:], in0=s_[:], scalar1=sm_[:, 5:6], scalar2=None, op0=ALU.mult)

            def stageC1(i):
                pn_, pT_, pb = pn[i % DEP], pT[i % DEP], rB[i % 8]
                for kb in range(2):
                    P.TR(reads=[pn_, identb], writes=[pb], out=pb.ap[:, kb * 128:(kb + 1) * 128], in_=pn_[:, kb * 128:(kb + 1) * 128],
                         identity=identb[:])
                P.A('copy', reads=[pb], writes=[pT_], out=pT_[:], in_=pb.ap)

            def stageC2(i):
                h, blk = iters[i]
                j = h // 4
                p0 = (h % 2) * 64
                ao = AO[(h // 2) % 2]
                pT_, pO, pOb = pT[i % DEP], rO[i % 4], rOb[i % 4]
                for kb in range(2):
                    P.MM(reads=[VT, pT_], writes=[pOb], out=pO[p0:p0 + 64, :], lhsT=VT[:, blk + kb, j * 64:(j + 1) * 64],
                         rhs=pT_[:, kb * 128:(kb + 1) * 128], start=(kb == 0), stop=(kb == 1))
                P.A('copy', reads=[pOb], writes=[ao], out=ao[p0:p0 + 64, blk * 128:(blk + 1) * 128], in_=pO[p0:p0 + 64, :])
                if h % 2 == 1 and blk == NBLK - 1:
                    P.dma('gpsimd', rowap(mix, 8 + h // 2), rowview(ao[:]), reads=[ao], key=ao)

            for step in range(NI + 3):
                if step - 1 >= 0 and step - 1 < NI:
                    stageB_act(step - 1)
                if step - 3 >= 0 and step - 3 < NI:
                    stageC2(step - 3)
                if step < NI:
                    stageA(step)
                if step - 2 >= 0 and step - 2 < NI:
                    stageC1(step - 2)
                if step - 1 >= 0 and step - 1 < NI:
                    stageB_dve(step - 1)
            P.barrier()

        def phase_merge(l):
            P.phase_begin()
            pw = [[P.sbuf("pjw", [128, 8, 1024], BF16) for _ in range(3)]]
            mx = [P.sbuf("mx", [128, 24, NT], BF16) for _ in range(2)]
            gt = [P.sbuf("gt", [128, 3, 8, NT], F32) for _ in range(2)]
            acc = [P.sbuf("acc", [128, NT], F32) for _ in range(2)]
            tm = [P.sbuf("tm", [128, NT], F32) for _ in range(2)]
            mt = [P.sbuf("mt", [128, 8, NT], BF16) for _ in range(2)]
            it = 0
            def load_pw(mg):
                for i in range(3):
                    P.dma('gpsimd', pw[0][i][:], proj[i][l].rearrange("(kc p) m -> p kc m", p=128)[:, :, mg * 1024:(mg + 1) * 1024],
                          writes=[pw[0][i]], key=pw[0][i])
            for mg in range(2):
                pws = pw[0]
                load_pw(mg)
                for tl in range(NTILES):
                    mx_, gt_, mt_ = mx[it % 2], gt[it % 2], mt[it % 2]
                    it += 1
                    P.dma('sync', mx_[:], mix[tl], writes=[mx_], key=mx_)
                    for i in range(3):
                        c0 = C_G + i * 16 + mg * 8
                        P.dma('sync', gt_[:, i, :, :], cols[tl, :, c0:c0 + 8, :], writes=[gt_], key=gt_)
                    P.A('activation', reads=[gt_], writes=[gt_], out=gt_[:].rearrange("p a b n -> p (a b n)"), in_=gt_[:].rearrange("p a b n -> p (a b n)"), func=AF.Sigmoid)
                    for mc in range(8):
                        ac, t_ = acc[mc % 2], tm[mc % 2]
                        for i in range(3):
                            ps = next_ps()
                            for kc in range(8):
                                P.MM(reads=[pws[i], mx_], writes=[ps], out=ps[:, 0:NT], lhsT=pws[i][:, kc, mc * 128:(mc + 1) * 128],
                                     rhs=mx_[:, i * 8 + kc, :], start=(kc == 0), stop=(kc == 7))
                            if i == 0:
                                P.V('tensor_tensor', reads=[ps, gt_], writes=[ac], out=ac[:], in0=ps[:, 0:NT], in1=gt_[:, i, mc, :], op=ALU.mult)
                            else:
                                P.V('tensor_tensor', reads=[ps, gt_], writes=[t_], out=t_[:], in0=ps[:, 0:NT], in1=gt_[:, i, mc, :], op=ALU.mult)
                                if i == 1:
                                    P.V('tensor_tensor', reads=[ac, t_], writes=[ac], out=ac[:], in0=ac[:], in1=t_[:], op=ALU.add)
                                else:
                                    P.V('tensor_tensor', reads=[ac, t_], writes=[mt_], out=mt_[:, mc, :], in0=ac[:], in1=t_[:], op=ALU.add)
                    P.dma('gpsimd', merged[tl, :, mg * 8:(mg + 1) * 8, :], mt_[:], reads=[mt_], key=mt_)
            P.barrier()

        def phase_wout(l):
            P.phase_begin()
            wo = P.sbuf("wo", [128, DC, D], BF16)
            mg_ = [P.sbuf("mgd", [128, DC, NT], BF16) for _ in range(2)]
            hz = [P.sbuf("hz", [128, DC, NT], F32) for _ in range(2)]
            tmp = {'zsq': [P.sbuf("zsq", [128, NT], F32) for _ in range(2)], 'mean': P.sbuf("mean", [128, NT], F32),
                   'rstd': P.sbuf("rstd", [128, NT], F32)}
            tmf = [P.sbuf("tmf", [128, D], F32) for _ in range(2)]
            tmb = [P.sbuf("tmb", [128, D], BF16) for _ in range(2)]
            wr = P.sbuf("wr", [128, DC, 36], F32)
            rb = P.sbuf("rb", [1, 36], F32)
            P.dma('sync', wr[:].rearrange("p a b -> p (a b)"), rw_tab[l], writes=[wr], key=wr)
            P.dma('sync', rb[:], rbias[l], writes=[rb], key=rb)
            wsrc = w_out[l].rearrange("(kc p) m -> p kc m", p=128)
            for q4 in range(4):
                for half in range(2):
                    P.dma('gpsimd', wo[:, half * 8:(half + 1) * 8, q4 * 512:(q4 + 1) * 512],
                          wsrc[:, half * 8:(half + 1) * 8, q4 * 512:(q4 + 1) * 512], writes=[wo], key=wo)
            for tl in range(NTILES):
                m_, z_ = mg_[tl % 2], hz[tl % 2]
                P.dma('sync', m_[:], merged[tl], writes=[m_], key=m_)
                P.dma('sync', z_[:], hres[tl], writes=[z_], key=z_)
                for mc in range(DC):
                    ps = next_ps()
                    for kc in range(DC):
                        P.MM(reads=[wo, m_], writes=[ps], out=ps[:, 0:NT], lhsT=wo[:, kc, mc * 128:(mc + 1) * 128], rhs=m_[:, kc, :],
                             start=(kc == 0), stop=(kc == DC - 1))
                    P.V('scalar_tensor_tensor', reads=[z_, ps], writes=[z_], out=z_[:, mc, :], in0=z_[:, mc, :], scalar=ALPHA, in1=ps[:, 0:NT],
                        op0=ALU.mult, op1=ALU.add)
                emit_ln(z_, NT, lambda mc: sp[:, SP_LN1G + mc:SP_LN1G + mc + 1], lambda mc: sp[:, SP_LN1B + mc:SP_LN1B + mc + 1], tmp,
                        zero_cols=(PAD if tl == 0 else 0), hb=None)
                for sb3 in range(3):
                    blk = tl * 3 + sb3
                    tf, tb = tmf[blk % 2], tmb[blk % 2]
                    pl = next_ps()
                    for kc in range(DC):
                        P.MM(reads=[z_, wr], writes=[pl], out=pl[:, 0:36], lhsT=z_[:, kc, sb3 * 128:(sb3 + 1) * 128], rhs=wr[:, kc, :],
                             start=(kc == 0), stop=False)
                    P.MM(reads=[onesf, rb], writes=[pl], out=pl[:, 0:36], lhsT=onesf[0:1, :], rhs=rb[0:1, :], start=False, stop=True)
                    P.V('tensor_copy', reads=[pl], writes=[Lall], out=Lall[:, blk, :], in_=pl[:, 0:36])
                    for c4 in range(4):
                        ps = next_ps()
                        for cc in range(4):
                            c = c4 * 4 + cc
                            P.TR(reads=[z_, identf], writes=[ps], out=ps[:, cc * 128:(cc + 1) * 128],
                                 in_=z_[:, c, sb3 * 128:(sb3 + 1) * 128], identity=identf[:])
                        evac(tf[:, c4 * 512:(c4 + 1) * 512], ps[:, 0:512], [ps], [tf])
                    P.G('tensor_copy', reads=[tf], writes=[tb], out=tb[:], in_=tf[:])
                    P.dma('gpsimd', h1tm_f[blk * 128:(blk + 1) * 128, :], tf[:], reads=[tf], key=tf)
                    P.dma('gpsimd', h1tm_b[blk * 128:(blk + 1) * 128, :], tb[:], reads=[tb], key=tb)
            P.barrier()

        def phase_router(l):
            P.phase_begin()
            ELm = [P.sbuf("ELm", [128, 32], F32) for _ in range(2)]
            sc = [P.sbuf("sc", [128, 32], F32) for _ in range(2)]
            M1a = P.sbuf("M1a", [128, NBLK, 32], F32)
            M2a = P.sbuf("M2a", [128, NBLK, 32], F32)
            M12b = P.sbuf("M12b", [128, NBLK, 32], BF16)
            onesb = P.sbuf("onesb", [128, 128], BF16)
            trib = P.sbuf("trib", [128, 128], BF16)
            thr = P.sbuf("thr", [128, NSB], F32)
            pio = P.sbuf("pio", [128, 1], F32)
            cnt = P.sbuf("cnt", [128, 32], F32)
            nbk = P.sbuf("nbk", [128, 32], F32)
            pend = P.sbuf("pend", [128, 32], F32)
            pstart = P.sbuf("pstart", [128, 32], F32)
            Dm = [P.sbuf("Dm", [128, 32], F32) for _ in range(2)]
            tt = [P.sbuf("tt", [128, 32], F32) for _ in range(2)]
            destf = P.sbuf("destf", [128, NBLK, 2], F32)
            be = P.sbuf("be", [128, NSB], F32)
            chg = P.sbuf("chg", [128, NSB], F32)
            gb = P.sbuf("gb", [128, NSB], F32)
            db = P.sbuf("db", [128, NSB], F32)
            widf = P.sbuf("widf", [128, NSB, 16], F32)
            didf = P.sbuf("didf", [128, NSB, 4], F32)
            padfix = P.sbuf("padfix", [128, 2], F32)
            P.dma('sync', trib[:], c_tri, writes=[trib], key=trib)
            P.dma('sync', thr[:], c_thr, writes=[thr], key=thr)
            P.dma('sync', pio[:], c_piota, writes=[pio], key=pio)
            P.V('memset', writes=[onesb], ap=onesb[:], constant=1.0)
            for blk in range(NBLK):
                E_, s_ = ELm[blk % 2], sc[blk % 2]
                L_ = Lall
                P.V('reduce_max', reads=[L_], writes=[s_], out=s_[:, 0:1], in_=Lall[:, blk, 0:4], axis=AX.X)
                P.V('tensor_scalar', reads=[s_], writes=[s_], out=s_[:, 1:2], in0=s_[:, 0:1], scalar1=-1.0, scalar2=None, op0=ALU.mult)
                P.A('activation', reads=[L_, s_], writes=[s_], out=s_[:, 24:28], in_=Lall[:, blk, 0:4], func=AF.Exp, bias=s_[:, 1:2],
                    accum_out=s_[:, 2:3])
                P.V('reciprocal', reads=[s_], writes=[s_], out=s_[:, 3:4], in_=s_[:, 2:3])
                P.V('tensor_scalar', reads=[L_, s_], writes=[s_], out=s_[:, 4:8], in0=Lall[:, blk, 0:4], scalar1=s_[:, 0:1], scalar2=None,
                    op0=ALU.is_equal)
                P.V('tensor_scalar', reads=[s_], writes=[s_], out=s_[:, 4:8], in0=s_[:, 4:8], scalar1=-1.0, scalar2=1e30, op0=ALU.add,
                    op1=ALU.mult)
                for g in range(4):
                    P.V('tensor_scalar', reads=[L_, s_], writes=[E_], out=E_[:, g * 8:(g + 1) * 8], in0=Lall[:, blk, 4 + g * 8:12 + g * 8],
                        scalar1=s_[:, 4 + g:5 + g], scalar2=None, op0=ALU.add)
                P.V('max', reads=[E_], writes=[s_], out=s_[:, 8:16], in_=E_[:])
                P.V('tensor_tensor', reads=[s_], writes=[s_], out=s_[:, 16:17], in0=s_[:, 8:9], in1=s_[:, 9:10], op=ALU.subtract)
                P.A('activation', reads=[s_], writes=[s_], out=s_[:, 17:18], in_=s_[:, 16:17], func=AF.Sigmoid)
                P.V('tensor_scalar', reads=[s_], writes=[s_], out=s_[:, 18:19], in0=s_[:, 17:18], scalar1=-1.0, scalar2=1.0, op0=ALU.mult,
                    op1=ALU.add)
                P.V('tensor_scalar', reads=[s_], writes=[cw], out=cw[:, blk, :], in0=s_[:, 17:19], scalar1=s_[:, 3:4], scalar2=None,
                    op0=ALU.mult)
                P.V('tensor_scalar', reads=[E_, s_], writes=[M1a], out=M1a[:, blk, :], in0=E_[:], scalar1=s_[:, 8:9], scalar2=None,
                    op0=ALU.is_equal)
                P.V('tensor_scalar', reads=[E_, s_], writes=[M2a], out=M2a[:, blk, :], in0=E_[:], scalar1=s_[:, 9:10], scalar2=None,
                    op0=ALU.is_equal)
                if blk == 0:
                    P.V('memset', writes=[M1a], ap=M1a[0:PAD, 0, :], constant=0.0)
                    P.V('memset', writes=[M2a], ap=M2a[0:PAD, 0, :], constant=0.0)
                P.V('tensor_tensor', reads=[M1a, M2a], writes=[M12b], out=M12b[:, blk, :], in0=M1a[:, blk, :], in1=M2a[:, blk, :], op=ALU.add)
            pc = next_ps()
            for blk in range(NBLK):
                P.MM(reads=[onesb, M12b], writes=[pc], out=pc[:, 0:32], lhsT=onesb[:], rhs=M12b[:, blk, :], start=(blk == 0),
                     stop=(blk == NBLK - 1))
            P.V('tensor_copy', reads=[pc], writes=[cnt], out=cnt[:], in_=pc[:, 0:32])
            P.V('memset', writes=[nbk], ap=nbk[:], constant=0.0)
            for k in range(34):
                P.V('scalar_tensor_tensor', reads=[cnt, nbk], writes=[nbk], out=nbk[:], in0=cnt[:], scalar=float(128 * k), in1=nbk[:],
                    op0=ALU.is_gt, op1=ALU.add)
            P.V('tensor_scalar', reads=[nbk], writes=[nbk], out=nbk[:], in0=nbk[:], scalar1=128.0, scalar2=None, op0=ALU.mult)
            P.V('tensor_tensor_scan', reads=[onesf, nbk], writes=[pend], out=pend[:], data0=onesf[:, 0:32], data1=nbk[:], initial=0.0,
                op0=ALU.mult, op1=ALU.add)
            P.V('tensor_tensor', reads=[pend, nbk], writes=[pstart], out=pstart[:], in0=pend[:], in1=nbk[:], op=ALU.subtract)
            for blk in range(NBLK):
                pC = next_ps()
                P.MM(reads=[trib, M12b], writes=[pC], out=pC[:, 0:32], lhsT=trib[:], rhs=M12b[:, blk, :], start=True, stop=(blk == 0))
                for j in range(blk):
                    P.MM(reads=[onesb, M12b], writes=[pC], out=pC[:, 0:32], lhsT=onesb[:], rhs=M12b[:, j, :], start=False, stop=(j == blk - 1))
                D_, t_ = Dm[blk % 2], tt[blk % 2]
                P.V('tensor_tensor', reads=[pC, pstart], writes=[D_], out=D_[:], in0=pC[:, 0:32], in1=pstart[:], op=ALU.add)
                for k, Ma in enumerate((M1a, M2a)):
                    P.V('tensor_tensor', reads=[Ma, D_], writes=[t_], out=t_[:], in0=Ma[:, blk, :], in1=D_[:], op=ALU.mult)
                    P.V('reduce_sum', reads=[t_], writes=[destf], out=destf[:, blk, k:k + 1], in_=t_[:], axis=AX.X)
            P.V('reduce_sum', reads=[M1a], writes=[padfix], out=padfix[:, 0:1], in_=M1a[:, 0, :], axis=AX.X)
            P.V('tensor_scalar', reads=[padfix], writes=[padfix], out=padfix[:, 1:2], in0=padfix[:, 0:1], scalar1=-BIGI, scalar2=BIGI,
                op0=ALU.mult, op1=ALU.add)
            for k in range(2):
                P.V('tensor_tensor', reads=[destf, padfix], writes=[destf], out=destf[:, 0, k:k + 1], in0=destf[:, 0, k:k + 1],
                    in1=padfix[:, 1:2], op=ALU.add)
            P.V('tensor_copy', reads=[destf], writes=[dest_i], out=dest_i[:], in_=destf[:])
            P.V('memset', writes=[be], ap=be[:], constant=0.0)
            for e in range(NEXP):
                P.V('scalar_tensor_tensor', reads=[thr, pend, be], writes=[be], out=be[:], in0=thr[:], scalar=pend[:, e:e + 1], in1=be[:],
                    op0=ALU.is_ge, op1=ALU.add)
            P.V('tensor_scalar', reads=[be], writes=[be], out=be[:], in0=be[:], scalar1=31.0, scalar2=None, op0=ALU.min)
            P.V('memset', writes=[chg], ap=chg[:, 0:1], constant=1.0)
            P.V('tensor_tensor', reads=[be], writes=[chg], out=chg[:, 1:NSB], in0=be[:, 1:NSB], in1=be[:, 0:NSB - 1], op=ALU.not_equal)
            for (dst, mul) in ((gb, 128.0), (db, 128.0)):
                P.V('tensor_scalar', reads=[be], writes=[dst], out=dst[:], in0=be[:], scalar1=mul, scalar2=float(l * NEXP) * mul - BIGI, op0=ALU.mult, op1=ALU.add)
                P.V('tensor_tensor', reads=[dst, chg], writes=[dst], out=dst[:], in0=dst[:], in1=chg[:], op=ALU.mult)
                P.V('tensor_scalar', reads=[dst], writes=[dst], out=dst[:], in0=dst[:], scalar1=BIGI, scalar2=None, op0=ALU.add)
                P.V('tensor_scalar', reads=[dst, pio], writes=[dst], out=dst[:], in0=dst[:], scalar1=pio[:, 0:1], scalar2=None, op0=ALU.add)
            for kc in range(16):
                P.V('tensor_scalar', reads=[gb], writes=[widf], out=widf[:, :, kc], in0=gb[:], scalar1=float(kc * 128), scalar2=None, op0=ALU.add)
            for kc in range(4):
                P.V('tensor_scalar', reads=[db], writes=[didf], out=didf[:, :, kc], in0=db[:], scalar1=float(kc * 128), scalar2=None, op0=ALU.add)
            P.V('tensor_copy', reads=[widf], writes=[widx], out=widx[:], in_=widf[:])
            P.V('tensor_copy', reads=[didf], writes=[didx], out=didx[:], in_=didf[:])
            P.barrier()

        def phase_moe(l, last):
            P.phase_begin()
            xsrc = [P.sbuf("xsrc", [128, D], BF16) for _ in range(2)]
            for blk in range(NBLK):
                x_ = xsrc[blk % 2]
                P.dma('sync', x_[:], h1tm_b[blk * 128:(blk + 1) * 128, :], writes=[x_], key=x_)
                for k in range(2):
                    P.dma_fn('gpsimd', lambda e, x_=x_, blk=blk, k=k: e.indirect_dma_start(
                        out=xs[:, :], out_offset=bass.IndirectOffsetOnAxis(ap=dest_i[:, blk, k:k + 1], axis=0), in_=x_[:, :], in_offset=None,
                        bounds_check=_breg(e, CAP - 1), oob_is_err=False), reads=[x_, dest_i], key=x_)
            P.barrier()
            P.phase_begin()
            xsb = [P.sbuf("xsb", [128, D], BF16) for _ in range(2)]
            xT = [P.sbuf("xT", [128, D], BF16) for _ in range(2)]
            wg = P.sbuf("wg", [128, DC, 512], BF16)
            wu = P.sbuf("wu", [128, DC, 512], BF16)
            wd = P.sbuf("wd", [128, 4, D], BF16)
            sgt = [P.sbuf("sgt", [128, 512], F32) for _ in range(2)]
            hdt = [P.sbuf("hdt", [128, 512], BF16) for _ in range(2)]
            hdT = [P.sbuf("hdT", [128, 512], BF16) for _ in range(2)]
            yblk = [P.sbuf("yblk", [128, D], F32) for _ in range(2)]
            wst = [P.sbuf("wst", [128, 8192], F32) for _ in range(3)]
            gsrc = ewg.rearrange("l e (p kc) m -> (l e p) (kc m)", kc=16)
            usrc = ewu.rearrange("l e (p kc) m -> (l e p) (kc m)", kc=16)
            dsrc = ewd.rearrange("l e (p kc) m -> (l e p) (kc m)", kc=4)
            wbound = (l + 1) * NEXP * 128 - 1
            for b in range(NSB):
                x_, xT_, sg_, hd_, hT_, y_ = xsb[b % 2], xT[b % 2], sgt[b % 2], hdt[b % 2], hdT[b % 2], yblk[b % 2]
                P.dma('sync', x_[:], xs[b * 128:(b + 1) * 128, :], writes=[x_], key=x_)
                for (stg, src) in zip(wst, (gsrc, usrc, dsrc)):
                    P.dma_fn('gpsimd', lambda e, stg=stg, src=src, b=b: e.indirect_dma_start(
                        out=stg[:, :], out_offset=None, in_=src[:, :],
                        in_offset=bass.IndirectOffsetOnAxis(ap=widx[:, b, 0:1], axis=0), bounds_check=_breg(e, wbound), oob_is_err=False),
                        reads=[widx], writes=[stg], key=stg)
                P.A('copy', reads=[wst[0]], writes=[wg], out=wg[:].rearrange("p a b -> p (a b)"), in_=wst[0][:])
                P.V('tensor_copy', reads=[wst[1]], writes=[wu], out=wu[:].rearrange("p a b -> p (a b)"), in_=wst[1][:])
                P.A('copy', reads=[wst[2]], writes=[wd], out=wd[:, 0:2, :].rearrange("p a b -> p (a b)"), in_=wst[2][:, 0:4096])
                P.V('tensor_copy', reads=[wst[2]], writes=[wd], out=wd[:, 2:4, :].rearrange("p a b -> p (a b)"), in_=wst[2][:, 4096:8192])
                for half in range(2):
                    pb = psb[half]
                    for c8 in range(8):
                        c = half * 8 + c8
                        P.TR(reads=[x_, identb], writes=[pb], out=pb[:, c8 * 128:(c8 + 1) * 128], in_=x_[:, c:D:16],
                             identity=identb[:])
                    evac(xT_[:, half * 1024:(half + 1) * 1024], pb[:, 0:1024], [pb], [xT_])
                pg = next_ps()
                for kc in range(DC):
                    P.MM(reads=[xT_, wg], writes=[pg], out=pg[:, 0:512], lhsT=xT_[:, kc * 128:(kc + 1) * 128], rhs=wg[:, kc, :],
                         start=(kc == 0), stop=(kc == DC - 1))
                pu = next_ps()
                for kc in range(DC):
                    P.MM(reads=[xT_, wu], writes=[pu], out=pu[:, 0:512], lhsT=xT_[:, kc * 128:(kc + 1) * 128], rhs=wu[:, kc, :],
                         start=(kc == 0), stop=(kc == DC - 1))
                P.A('activation', reads=[pg], writes=[sg_], out=sg_[:], in_=pg[:, 0:512], func=AF.Silu)
                P.V('tensor_tensor', reads=[sg_, pu], writes=[hd_], out=hd_[:], in0=sg_[:], in1=pu[:, 0:512], op=ALU.mult)
                pb = psb[b % 2]
                for kc in range(4):
                    P.TR(reads=[hd_, identb], writes=[pb], out=pb[:, kc * 128:(kc + 1) * 128], in_=hd_[:, kc:512:4],
                         identity=identb[:])
                evac(hT_[:], pb[:, 0:512], [pb], [hT_])
                for fg in range(4):
                    py = next_ps()
                    for kc in range(4):
                        P.MM(reads=[hT_, wd], writes=[py], out=py[:, 0:512], lhsT=hT_[:, kc * 128:(kc + 1) * 128],
                             rhs=wd[:, kc, fg * 512:(fg + 1) * 512], start=(kc == 0), stop=(kc == 3))
                    evac(y_[:, fg * 512:(fg + 1) * 512], py[:, 0:512], [py], [y_])
                P.dma('sync', yb[b * 128:(b + 1) * 128, :], y_[:], reads=[y_], key=y_)
            P.barrier()
            P.phase_begin()
            G1 = [P.sbuf("G1", [128, D], F32) for _ in range(2)]
            G2 = [P.sbuf("G2", [128, D], F32) for _ in range(2)]
            h1t = [P.sbuf("h1t", [128, D], F32) for _ in range(2)]
            stt = P.sbuf("stt", [128, 4, 6], F32)
            mv = P.sbuf("mv", [128, 2], F32)
            rs = P.sbuf("rs", [128, 1], F32)
            if last:
                gbc = P.sbuf("gbc", [128, D], F32)
                bbc = P.sbuf("bbc", [128, D], F32)
                P.dma('sync', gbc[:], ln2g_bc[l], writes=[gbc], key=gbc)
                P.dma('sync', bbc[:], ln2b_bc[l], writes=[bbc], key=bbc)
            else:
                hs = [P.sbuf("hs", [128, DC, 128], F32) for _ in range(2)]
                hsb = [P.sbuf("hsb", [128, DC, 128], BF16) for _ in range(2)]
            for g_ in G1 + G2:
                P.V('memset', writes=[g_], ap=g_[:], constant=0.0)
            for blk in range(NBLK):
                z_, g2_, h_ = G1[blk % 2], G2[blk % 2], h1t[blk % 2]
                for k, gt_ in enumerate((z_, g2_)):
                    P.dma_fn('gpsimd', lambda e, gt_=gt_, blk=blk, k=k: e.indirect_dma_start(
                        out=gt_[:, :], out_offset=None, in_=yb[:, :], in_offset=bass.IndirectOffsetOnAxis(ap=dest_i[:, blk, k:k + 1], axis=0),
                        bounds_check=_breg(e, CAP - 1), oob_is_err=False), reads=[dest_i], writes=[gt_], key=gt_)
                P.dma('sync', h_[:], h1tm_f[blk * 128:(blk + 1) * 128, :], writes=[h_], key=h_)
                P.V('tensor_scalar', reads=[z_, cw], writes=[z_], out=z_[:], in0=z_[:], scalar1=cw[:, blk, 0:1], scalar2=None, op0=ALU.mult)
                P.V('scalar_tensor_tensor', reads=[g2_, cw, z_], writes=[z_], out=z_[:], in0=g2_[:], scalar=cw[:, blk, 1:2], in1=z_[:],
                    op0=ALU.mult, op1=ALU.add)
                P.V('scalar_tensor_tensor', reads=[h_, z_], writes=[z_], out=z_[:], in0=h_[:], scalar=ALPHA, in1=z_[:], op0=ALU.mult,
                    op1=ALU.add)
                for j in range(4):
                    P.V('bn_stats', reads=[z_], writes=[stt], out=stt[:, j, :], in_=z_[:, j * 512:(j + 1) * 512])
                P.V('bn_aggr', reads=[stt], writes=[mv], out=mv[:], in_=stt[:].rearrange("p a b -> p (a b)"))
                P.V('tensor_scalar', reads=[mv], writes=[rs], out=rs[:], in0=mv[:, 1:2], scalar1=EPS, scalar2=None, op0=ALU.add)
                P.A('activation', reads=[rs], writes=[rs], out=rs[:], in_=rs[:], func=AF.Sqrt)
                P.V('reciprocal', reads=[rs], writes=[rs], out=rs[:], in_=rs[:])
                P.V('tensor_scalar', reads=[z_, mv, rs], writes=[z_], out=z_[:], in0=z_[:], scalar1=mv[:, 0:1], scalar2=rs[:, 0:1],
                    op0=ALU.subtract, op1=ALU.mult)
                if last:
                    if blk == 0:
                        continue
                    P.V('tensor_tensor', reads=[z_, gbc], writes=[z_], out=z_[:], in0=z_[:], in1=gbc[:], op=ALU.mult)
                    P.V('tensor_tensor', reads=[z_, bbc], writes=[z_], out=z_[:], in0=z_[:], in1=bbc[:], op=ALU.add)
                    P.dma('sync', out[(blk - 1) * 128:blk * 128, :], z_[:], reads=[z_], key=z_)
                else:
                    ho, hb_ = hs[blk % 2], hsb[blk % 2]
                    for c4 in range(4):
                        ps = next_ps()
                        for cc in range(4):
                            c = c4 * 4 + cc
                            P.TR(reads=[z_, identf], writes=[ps], out=ps[:, cc * 128:(cc + 1) * 128], in_=z_[:, c * 128:(c + 1) * 128],
                                 identity=identf[:])
                        for cc in range(4):
                            c = c4 * 4 + cc
                            P.A('activation', reads=[ps, sp], writes=[ho], out=ho[:, c, :], in_=ps[:, cc * 128:(cc + 1) * 128],
                                func=AF.Identity, scale=sp[:, SP_LN2G + c:SP_LN2G + c + 1], bias=sp[:, SP_LN2B + c:SP_LN2B + c + 1])
                    if blk == 0:
                        P.V('memset', writes=[ho], ap=ho[:, :, 0:PAD], constant=0.0)
                    P.V('tensor_copy', reads=[ho], writes=[hb_], out=hb_[:], in_=ho[:])
                    tl, off = blk // 3, (blk % 3) * 128
                    P.dma('sync', hres[tl, :, :, off:off + 128], ho[:], reads=[ho], key=ho)
                    P.dma('sync', hbf[tl, :, :, off:off + 128], hb_[:], reads=[hb_], key=hb_)
            P.barrier()

        class _View:
            def __init__(self, tile, off):
                self.tile = tile
                self.off = off
                self.buf = tile.buf

            def __getitem__(self, idx):
                p, c, n = idx
                assert isinstance(n, slice)
                n0 = (n.start or 0) + self.off
                n1 = (n.stop if n.stop is not None else NT) + self.off
                return self.tile.t[p, c, n0:n1]

        phases = []
        phase_embed()
        done = (stop_after == 'embed')
        for l in range(depth):
            if done:
                break
            P.dma('sync', sp[:], smallp[l], writes=[sp], key=sp)
            for name, fn in (('win', phase_win), ('pool', phase_pool), ('lru', phase_lru), ('attn', phase_attn), ('merge', phase_merge),
                             ('wout', phase_wout), ('router', phase_router)):
                fn(l)
                if stop_after == "%s%d" % (name, l):
                    done = True
                    break
            if done:
                break
            phase_moe(l, last=(l == depth - 1))
        P.emit()
    return nc


def _is_tile_like(x):
    return hasattr(x, 'buf')


def _tab(v, n):
    return np.ascontiguousarray(np.asarray(v, np.float32).reshape(n, 128).T)


def make_consts():
    q = np.arange(128)[:, None]
    s = np.arange(256)[None, :]
    dist = (128 + q - s).astype(np.float32)
    inwin = (dist >= 0) & (dist < 128)
    slopes = (2.0 ** (-8.0 * np.arange(1, 17, dtype=np.float32) / 16)).astype(np.float32)
    ab = np.where(inwin[:, None, :], -slopes[None, :, None] * dist[:, None, :], np.float32(NEG)).astype(np.float32)
    pm = np.where(s < 128 + PAD, np.float32(NEG), np.float32(0.0)).astype(np.float32) * np.ones((128, 1), np.float32)
    invc = np.ones((4, 128, T), np.float32)
    tt = np.arange(T) - PAD
    for g, w in enumerate((2, 4, 8, 16)):
        cnt = np.where(tt >= 0, np.minimum(tt + 1, w), 1).astype(np.float32)
        invc[g] = (1.0 / cnt)[None, :]
    tri = (np.arange(128)[:, None] < np.arange(128)[None, :]).astype(np.float32).astype(ml_dtypes.bfloat16)
    thr = np.ascontiguousarray(np.broadcast_to((128.0 * np.arange(NSB, dtype=np.float32))[None, :], (128, NSB)))
    piota = np.arange(128, dtype=np.float32)[:, None].copy()
    return {
        "c_tri": tri, "c_thr": thr, "c_piota": piota,
        "c_ab": np.ascontiguousarray(ab), "c_pm": np.ascontiguousarray(pm), "c_invcnt": invc,
        "c_identf": np.eye(128, dtype=np.float32), "c_identb": np.eye(128, dtype=np.float32).astype(ml_dtypes.bfloat16),
    }


def make_inputs(inp, b):
    f = lambda k: np.ascontiguousarray(np.asarray(inp[k], np.float32))
    xin = np.zeros((T, D), np.float32)
    xin[PAD:PAD + NMETA] = np.asarray(inp['meta'], np.float32)
    xin[PAD + NMETA:] = np.asarray(inp['x'][b], np.float32)
    smallp = np.zeros((DEPTH, 128, SP_N), np.float32)
    for l in range(DEPTH):
        smallp[l, :, SP_LN1G:SP_LN1G + 16] = _tab(inp['ln1_g'][l], 16)
        smallp[l, :, SP_LN1B:SP_LN1B + 16] = _tab(inp['ln1_b'][l], 16)
        smallp[l, :, SP_LN2G:SP_LN2G + 16] = _tab(inp['ln2_g'][l], 16)
        smallp[l, :, SP_LN2B:SP_LN2B + 16] = _tab(inp['ln2_b'][l], 16)
        smallp[l, :, SP_PSC:SP_PSC + 8] = _tab(inp['pool_scale'][l], 8)
        for j in range(4):
            smallp[l, :, SP_CW + j * 8:SP_CW + j * 8 + 8] = _tab(inp['conv_w'][l][j], 8)
        smallp[l, :, SP_CB:SP_CB + 8] = _tab(inp['conv_b'][l], 8)
        smallp[l, :, SP_BA:SP_BA + 8] = _tab(inp['lru_ba'][l], 8)
        smallp[l, :, SP_BX:SP_BX + 8] = _tab(inp['lru_bx'][l], 8)
        smallp[l, :, SP_LAM:SP_LAM + 8] = _tab(inp['lru_lambda'][l], 8)
        smallp[l, :, SP_SINK:SP_SINK + 16] = np.asarray(inp['attn_sink'][l], np.float32)[None, :]
    embp = np.concatenate([_tab(inp['ln_emb_g'], 16), _tab(inp['ln_emb_b'], 16)], axis=1)
    rbias = np.concatenate([np.asarray(inp['router_grp_b'], np.float32), np.asarray(inp['router_exp_b'], np.float32)], axis=1)[:, None, :]
    rcat = np.concatenate([np.asarray(inp['router_grp_w'], np.float32), np.asarray(inp['router_exp_w'], np.float32)], axis=2)
    rw_tab = np.ascontiguousarray(rcat.reshape(DEPTH, DC, 128, 36).transpose(0, 2, 1, 3).reshape(DEPTH, 128, DC * 36))
    m = {
        "xin": xin, "w_in": f('w_in'), "pool_w": f('pool_w'), "lru_wa": f('lru_wa'), "lru_wx": f('lru_wx'),
        "proj_pool": f('proj_pool'), "proj_attn": f('proj_attn'), "proj_lru": f('proj_lru'), "w_out": f('w_out'),
        "rw_tab": rw_tab, "rbias": np.ascontiguousarray(rbias),
        "exp_w_gate": f('exp_w_gate'), "exp_w_up": f('exp_w_up'), "exp_w_down": f('exp_w_down'),
        "smallp": smallp, "embp": np.ascontiguousarray(embp),
        "ln2g_bc": np.ascontiguousarray(np.broadcast_to(np.asarray(inp['ln2_g'], np.float32)[:, None, :], (DEPTH, 128, D))),
        "ln2b_bc": np.ascontiguousarray(np.broadcast_to(np.asarray(inp['ln2_b'], np.float32)[:, None, :], (DEPTH, 128, D))),
    }
    m.update(make_consts())
    return m


_NC_CACHE = {}


def kernel(**inputs):
    B = inputs['x'].shape[0]
    if 'nc' not in _NC_CACHE:
        _NC_CACHE['nc'] = build_nc()
    nc = _NC_CACHE['nc']
    shared = None
    in_maps = []
    for b in range(B):
        m = make_inputs(inputs, b) if shared is None else dict(shared, xin=None)
        if shared is None:
            shared = m
        else:
            xin = np.zeros((T, D), np.float32)
            xin[PAD:PAD + NMETA] = np.asarray(inputs['meta'], np.float32)
            xin[PAD + NMETA:] = np.asarray(inputs['x'][b], np.float32)
            m['xin'] = xin
        in_maps.append(m)
    res = run_bass_kernel_spmd(nc, in_maps, core_ids=list(range(B)))
    return np.stack([np.asarray(r["out"], np.float32) for r in res.results], axis=0)
```

```python
import contextlib
import numpy as np
import ml_dtypes
import concourse.bass as bass
import concourse.mybir as mybir
from concourse.bass_utils import run_bass_kernel_spmd

F32 = mybir.dt.float32
BF16 = mybir.dt.bfloat16
AF = mybir.ActivationFunctionType
ALU = mybir.AluOpType
AX = mybir.AxisListType

COMPUTE = ('scalar', 'vector', 'tensor', 'gpsimd')
QUEUES = ('sync', 'scalar', 'vector', 'tensor', 'gpsimd')
DT_SIZE = {F32: 4, BF16: 2, mybir.dt.int32: 4}

D = 2048
DC = 16
SEQ = 4096
NMETA = 16
PAD = 112
T = 4224
NT = 384
NTILES = 11
NBLK = 33
DEPTH = 2
NCH_COLS = 84
C_POOL, C_Q, C_K, C_V, C_LX, C_LY, C_G = 0, 8, 16, 18, 20, 28, 36
NEXP = 32
ALPHA = (2.0 * DEPTH) ** 0.25
EPS = 1e-5
NEG = -1e30
NSB = 97
CAP = NSB * 128
BIGI = 1048576.0
I32 = mybir.dt.int32
SP_LN1G, SP_LN1B, SP_LN2G, SP_LN2B = 0, 16, 32, 48
SP_PSC, SP_CW, SP_CB, SP_BA, SP_BX, SP_LAM, SP_SINK = 64, 72, 104, 112, 120, 128, 136
SP_N = 152


class Buf:
    __slots__ = ('name', 'w', 'r', 'sem')

    def __init__(self, name):
        self.name = name
        self.w = None
        self.r = []
        self.sem = None


class Op:
    __slots__ = ('q', 'fn', 'deps', 'is_dma', 'sem', 'val', 'needed')

    def __init__(self, q, fn, is_dma):
        self.q = q
        self.fn = fn
        self.deps = []
        self.is_dma = is_dma
        self.sem = None
        self.val = 0
        self.needed = False


class Tile:
    __slots__ = ('t', 'buf')

    def __init__(self, t, name):
        self.t = t
        self.buf = Buf(name)

    def __getitem__(self, idx):
        return self.t[idx]


def _b(x):
    return getattr(x, 'buf', x)


_BREG = {}


def _breg(e, val):
    k = (id(e), int(val))
    if k not in _BREG:
        _BREG[k] = e.to_reg(int(val))
    return _BREG[k]


class Prog:
    SB_LO = 20480
    SB_HI = 222 * 1024

    def __init__(self, nc, n_dma_sems=56):
        self.nc = nc
        self.ops = {q: [] for q in QUEUES}
        self.esem = {}
        self.dma_pool = []
        self.n_dma_sems = n_dma_sems
        self.pool_idx = 0
        self.sb_off = self.SB_LO
        self.sb_base = self.SB_LO
        self.sb_max = 0
        self.uid = 0
        self.live = []

    def setup(self, stack):
        for e in COMPUTE:
            self.esem[e] = stack.enter_context(self.nc.semaphore("es_" + e))
        for i in range(self.n_dma_sems):
            self.dma_pool.append([stack.enter_context(self.nc.semaphore("ds%d" % i)), 0])

    def sbuf(self, name, shape, dtype):
        nbytes = int(np.prod(shape[1:])) * DT_SIZE[dtype]
        off = (self.sb_off + 63) // 64 * 64
        self.uid += 1
        t = self.nc.alloc_sbuf_tensor_at("%s_%d" % (name, self.uid), list(shape), dtype, offset=off)
        self.sb_off = off + nbytes
        self.sb_max = max(self.sb_max, self.sb_off)
        assert self.sb_off <= self.SB_HI, ("SBUF overflow", name, self.sb_off)
        return Tile(t, name)

    def phase_begin(self):
        self.sb_off = self.sb_base

    def persist_mark(self):
        self.sb_base = self.sb_off

    def _hazards(self, op, reads, writes):
        deps = []
        strong = set()
        for b in reads:
            b = _b(b)
            if b.w is not None:
                deps.append(b.w)
                strong.add(id(b.w))
        for b in writes:
            b = _b(b)
            if b.w is not None:
                deps.append(b.w)
                strong.add(id(b.w))
            deps.extend(b.r)
        for b in reads:
            _b(b).r.append(op)
        for b in writes:
            b = _b(b)
            b.w = op
            b.r = []
        seen = set()
        for d in deps:
            if d is op or id(d) in seen:
                continue
            seen.add(id(d))
            if (not d.is_dma) and (not op.is_dma) and d.q == op.q:
                if d.q == 'tensor' or id(d) not in strong:
                    continue
            if not d.is_dma:
                d.needed = True
            op.deps.append(d)

    def op(self, eng, fn, reads=(), writes=()):
        o = Op(eng, fn, False)
        self._hazards(o, reads, writes)
        self.ops[eng].append(o)
        return o

    def dma(self, q, out, in_, reads=(), writes=(), key=None):
        o = Op(q, (lambda e, out=out, in_=in_: e.dma_start(out=out, in_=in_)), True)
        b = _b(key)
        if b.sem is None:
            assert self.pool_idx < len(self.dma_pool), "out of dma sems"
            b.sem = self.dma_pool[self.pool_idx]
            self.pool_idx += 1
            self.live.append(b)
        b.sem[1] += 16
        o.sem = b.sem[0]
        o.val = b.sem[1]
        self._hazards(o, reads, writes)
        self.ops[q].append(o)
        return o

    def dma_fn(self, q, fn, reads=(), writes=(), key=None):
        o = Op(q, fn, True)
        b = _b(key)
        if b.sem is None:
            assert self.pool_idx < len(self.dma_pool), "out of dma sems"
            b.sem = self.dma_pool[self.pool_idx]
            self.pool_idx += 1
            self.live.append(b)
        b.sem[1] += 16
        o.sem = b.sem[0]
        o.val = b.sem[1]
        self._hazards(o, reads, writes)
        self.ops[q].append(o)
        return o

    def barrier(self):
        last = []
        for e in COMPUTE:
            for o in reversed(self.ops[e]):
                if o.fn is not None and not o.is_dma:
                    o.needed = True
                    last.append(o)
                    break
        dmas = []
        for i in range(self.pool_idx):
            s, c = self.dma_pool[i]
            if c > 0:
                d = Op('sync', None, True)
                d.sem = s
                d.val = c
                dmas.append(d)
        for q in QUEUES:
            o = Op(q, None, False)
            o.deps = [d for d in last if d.q != q] + dmas
            self.ops[q].append(o)
        for b in self.live:
            b.sem = None
        self.live = []
        self.pool_idx = 0

    def emit(self):
        nc = self.nc
        for e in COMPUTE:
            c = 0
            for o in self.ops[e]:
                if o.needed and not o.is_dma:
                    c += 1
                    o.sem = self.esem[e]
                    o.val = c
        with nc.Block() as block:
            for q in QUEUES:
                lst = self.ops[q]
                if not lst:
                    continue

                def body(eng, lst=lst):
                    waited = {}
                    for o in lst:
                        for d in o.deps:
                            k = id(d.sem)
                            if waited.get(k, 0) >= d.val:
                                continue
                            waited[k] = d.val
                            eng.wait_ge(d.sem, d.val)
                        if o.fn is None:
                            continue
                        ins = o.fn(eng)
                        if o.is_dma:
                            ins.then_inc(o.sem, 16)
                        elif o.needed:
                            ins.then_inc(o.sem, 1)

                getattr(block, q)(body)

    def V(self, name, reads=(), writes=(), **kw):
        return self.op('vector', lambda e: getattr(e, name)(**kw), reads, writes)

    def A(self, name, reads=(), writes=(), **kw):
        return self.op('scalar', lambda e: getattr(e, name)(**kw), reads, writes)

    def G(self, name, reads=(), writes=(), **kw):
        return self.op('gpsimd', lambda e: getattr(e, name)(**kw), reads, writes)

    def MM(self, reads=(), writes=(), **kw):
        return self.op('tensor', lambda e: e.matmul(**kw), reads, writes)

    def TR(self, reads=(), writes=(), **kw):
        return self.op('tensor', lambda e: e.transpose(**kw), reads, writes)


def build_nc(depth=DEPTH, debug=False, stop_after=None):
    _BREG.clear()
    nc = bass.Bass("TRN2", target_bir_lowering=False)
    dbg_set = set(debug) if debug else set()

    def scr(name, shape, dt):
        return nc.dram_tensor(name, list(shape), dt, kind=("ExternalOutput" if name in dbg_set else "Internal")).ap()

    def din(name, shape, dt=F32):
        return nc.dram_tensor(name, list(shape), dt, kind="ExternalInput").ap()

    xin = din("xin", [T, D])
    w_in = din("w_in", [DEPTH, D, 10752])
    pool_w = din("pool_w", [DEPTH, 4, 256, 256])
    lru_wa = din("lru_wa", [DEPTH, 4, 256, 256])
    lru_wx = din("lru_wx", [DEPTH, 4, 256, 256])
    proj = [din("proj_pool", [DEPTH, 1024, D]), din("proj_attn", [DEPTH, 1024, D]), din("proj_lru", [DEPTH, 1024, D])]
    w_out = din("w_out", [DEPTH, D, D])
    rw_tab = din("rw_tab", [DEPTH, 128, DC * 36])
    rbias = din("rbias", [DEPTH, 1, 36])
    ewg = din("exp_w_gate", [DEPTH, NEXP, D, 512])
    ewu = din("exp_w_up", [DEPTH, NEXP, D, 512])
    ewd = din("exp_w_down", [DEPTH, NEXP, 512, D])
    smallp = din("smallp", [DEPTH, 128, SP_N])
    embp = din("embp", [128, 32])
    c_ab = din("c_ab", [128, 16, 256])
    c_pm = din("c_pm", [128, 256])
    c_invcnt = din("c_invcnt", [4, 128, T])
    c_identf = din("c_identf", [128, 128])
    c_identb = din("c_identb", [128, 128], BF16)
    c_tri = din("c_tri", [128, 128], BF16)
    c_thr = din("c_thr", [128, NSB])
    c_piota = din("c_piota", [128, 1])
    ln2g_bc = din("ln2g_bc", [DEPTH, 128, D])
    ln2b_bc = din("ln2b_bc", [DEPTH, 128, D])

    out = nc.dram_tensor("out", [SEQ, D], F32, kind="ExternalOutput").ap()
    hres = scr("hres", [NTILES, 128, DC, NT], F32)
    hbf = scr("hbf", [NTILES, 128, DC, NT], BF16)
    cols = scr("cols", [NTILES, 128, NCH_COLS, NT], F32)
    mix = scr("mix", [NTILES, 128, 24, NT], BF16)
    merged = scr("merged", [NTILES, 128, DC, NT], BF16)
    h1tm_f = scr("h1tm_f", [T, D], F32)
    h1tm_b = scr("h1tm_b", [T, D], BF16)
    xs = scr("xs", [CAP, D], BF16)
    yb = scr("yb", [CAP, D], F32)

    def rowap(x, c, p0=0, p1=128):
        return x.rearrange("t p c n -> p t c n")[p0:p1, :, c, :]

    def rowview(ap2d):
        return ap2d.rearrange("p (t n) -> p t n", n=NT)

    with contextlib.ExitStack() as st:
        P = Prog(nc)
        P.setup(st)
        psf = [Tile(st.enter_context(nc.psum_tensor("psf%d" % i, [128, 512], F32)), "psf%d" % i) for i in range(6)]
        psb = [Tile(st.enter_context(nc.psum_tensor("psb%d" % i, [128, 1024], BF16)), "psb%d" % i) for i in range(2)]
        psi = [0]

        def next_ps():
            psi[0] += 1
            return psf[psi[0] % 6]

        identf = P.sbuf("identf", [128, 128], F32)
        identb = P.sbuf("identb", [128, 128], BF16)
        onesf = P.sbuf("onesf", [128, 128], F32)
        embt = P.sbuf("embt", [128, 32], F32)
        sp = P.sbuf("sp", [128, SP_N], F32)
        lamc = P.sbuf("lamc", [128, 16], F32)
        Lall = P.sbuf("Lall", [128, NBLK, 36], F32)
        dest_i = P.sbuf("dest_i", [128, NBLK, 2], I32)
        cw = P.sbuf("cw", [128, NBLK, 2], F32)
        widx = P.sbuf("widx", [128, NSB, 16], I32)
        didx = P.sbuf("didx", [128, NSB, 4], I32)
        P.persist_mark()
        P.dma('sync', identf[:], c_identf, writes=[identf], key=identf)
        P.dma('sync', identb[:], c_identb, writes=[identb], key=identb)
        P.dma('sync', embt[:], embp, writes=[embt], key=embt)
        P.V('memset', writes=[onesf], ap=onesf[:], constant=1.0)

        evac_i = [0]

        def evac(out_ap, in_ap, reads, writes):
            evac_i[0] += 1
            if evac_i[0] % 2 == 0:
                P.A('copy', reads=reads, writes=writes, out=out_ap, in_=in_ap)
            else:
                P.V('tensor_copy', reads=reads, writes=writes, out=out_ap, in_=in_ap)

        def emit_ln(hz, n, gcol, bcol, tmp, zero_cols=0, hb=None):
            ps1 = next_ps()
            ps2 = next_ps()
            for mc in range(DC):
                P.MM(reads=[onesf, hz], writes=[ps1], out=ps1[:, 0:n], lhsT=onesf[:], rhs=hz[:, mc, 0:n],
                     start=(mc == 0), stop=(mc == DC - 1))
            for mc in range(DC):
                zs = tmp['zsq'][mc % 2]
                P.A('activation', reads=[hz], writes=[zs], out=zs[:, 0:n], in_=hz[:, mc, 0:n], func=AF.Square)
                P.MM(reads=[onesf, zs], writes=[ps2], out=ps2[:, 0:n], lhsT=onesf[:], rhs=zs[:, 0:n],
                     start=(mc == 0), stop=(mc == DC - 1))
            mean, rstd = tmp['mean'], tmp['rstd']
            P.A('mul', reads=[ps1], writes=[mean], out=mean[:, 0:n], in_=ps1[:, 0:n], mul=1.0 / D)
            P.V('tensor_tensor', reads=[mean], writes=[rstd], out=rstd[:, 0:n], in0=mean[:, 0:n], in1=mean[:, 0:n], op=ALU.mult)
            P.V('scalar_tensor_tensor', reads=[ps2, rstd], writes=[rstd], out=rstd[:, 0:n], in0=ps2[:, 0:n], scalar=1.0 / D,
                in1=rstd[:, 0:n], op0=ALU.mult, op1=ALU.subtract)
            P.V('tensor_scalar', reads=[rstd], writes=[rstd], out=rstd[:, 0:n], in0=rstd[:, 0:n], scalar1=0.0, scalar2=EPS,
                op0=ALU.max, op1=ALU.add)
            P.A('activation', reads=[rstd], writes=[rstd], out=rstd[:, 0:n], in_=rstd[:, 0:n], func=AF.Sqrt)
            P.V('reciprocal', reads=[rstd], writes=[rstd], out=rstd[:, 0:n], in_=rstd[:, 0:n])
            for mc in range(DC):
                P.V('tensor_tensor', reads=[hz, mean], writes=[hz], out=hz[:, mc, 0:n], in0=hz[:, mc, 0:n], in1=mean[:, 0:n], op=ALU.subtract)
                P.V('tensor_tensor', reads=[hz, rstd], writes=[hz], out=hz[:, mc, 0:n], in0=hz[:, mc, 0:n], in1=rstd[:, 0:n], op=ALU.mult)
                P.A('activation', reads=[hz, sp, embt], writes=[hz], out=hz[:, mc, 0:n], in_=hz[:, mc, 0:n], func=AF.Identity,
                    scale=gcol(mc), bias=bcol(mc))
            if zero_cols:
                P.V('memset', writes=[hz], ap=hz[:, :, 0:zero_cols], constant=0.0)
            if hb is not None:
                P.G('tensor_copy', reads=[hz], writes=[hb], out=hb[:, :, 0:n], in_=hz[:, :, 0:n])

        def phase_embed():
            P.phase_begin()
            xt = [P.sbuf("xt", [128, D], F32) for _ in range(2)]
            xn = [P.sbuf("xn", [128, D], F32) for _ in range(2)]
            stt = P.sbuf("stt", [128, 4, 6], F32)
            mv = P.sbuf("mv", [128, 2], F32)
            rs = P.sbuf("rs", [128, 1], F32)
            hs = [P.sbuf("hs", [128, DC, NT], F32) for _ in range(2)]
            hsb = [P.sbuf("hsb", [128, DC, NT], BF16) for _ in range(2)]
            for blk in range(NBLK):
                x_ = xt[blk % 2]
                n_ = xn[blk % 2]
                tl, off = blk // 3, (blk % 3) * 128
                h_ = hs[tl % 2]
                hb_ = hsb[tl % 2]
                P.dma('sync', x_[:], xin[blk * 128:(blk + 1) * 128, :], writes=[x_], key=x_)
                for j in range(4):
                    P.V('bn_stats', reads=[x_], writes=[stt], out=stt[:, j, :], in_=x_[:, j * 512:(j + 1) * 512])
                P.V('bn_aggr', reads=[stt], writes=[mv], out=mv[:], in_=stt[:].rearrange("p a b -> p (a b)"))
                P.V('tensor_scalar', reads=[mv], writes=[rs], out=rs[:], in0=mv[:, 1:2], scalar1=EPS, scalar2=None, op0=ALU.add)
                P.A('activation', reads=[rs], writes=[rs], out=rs[:], in_=rs[:], func=AF.Sqrt)
                P.V('reciprocal', reads=[rs], writes=[rs], out=rs[:], in_=rs[:])
                P.V('tensor_scalar', reads=[x_, mv, rs], writes=[n_], out=n_[:], in0=x_[:], scalar1=mv[:, 0:1], scalar2=rs[:, 0:1],
                    op0=ALU.subtract, op1=ALU.mult)
                import os
                CUT = int(os.environ.get('EMBED_CUT', '9'))
                for c4 in range(4):
                    ps = next_ps()
                    for cc in range(4):
                        c = c4 * 4 + cc
                        if CUT >= 1:
                            P.TR(reads=[n_, identf], writes=[ps], out=ps[:, cc * 128:(cc + 1) * 128], in_=n_[:, c * 128:(c + 1) * 128],
                                 identity=identf[:])
                    for cc in range(4):
                        c = c4 * 4 + cc
                        if CUT >= 2:
                            P.A('activation', reads=[ps, embt], writes=[h_], out=h_[:, c, off:off + 128], in_=ps[:, cc * 128:(cc + 1) * 128],
                                func=AF.Identity, scale=embt[:, c:c + 1], bias=embt[:, 16 + c:17 + c])
                        else:
                            P.V('tensor_copy', reads=[n_], writes=[h_], out=h_[:, c, off:off + 128], in_=n_[:, c * 128:(c + 1) * 128])
                if blk == 0:
                    P.V('memset', writes=[h_], ap=h_[:, :, 0:PAD], constant=0.0)
                if blk % 3 == 2:
                    P.V('tensor_copy', reads=[h_], writes=[hb_], out=hb_[:], in_=h_[:])
                    P.dma('gpsimd', hres[tl], h_[:], reads=[h_], key=h_)
                    P.dma('gpsimd', hbf[tl], hb_[:], reads=[hb_], key=hb_)
            P.barrier()

        def phase_win(l):
            P.phase_begin()
            wb = [P.sbuf("wb", [128, DC, 512], BF16) for _ in range(2)]
            hb = [P.sbuf("hb", [128, DC, NT], BF16) for _ in range(3)]
            ot = [P.sbuf("ot", [128, 4, NT], F32) for _ in range(2)]
            colsdep = [Buf("colsdep%d" % mg) for mg in range(21)]
            wsrc = w_in[l].rearrange("(kc p) m -> p kc m", p=128)

            U = [P.sbuf("U", [128, 16 + T], F32) for _ in range(2)]
            Wk = [P.sbuf("Wk", [128, 16 + T], F32) for _ in range(2)]
            icns = P.sbuf("icns", [128, 4, 16], F32)
            dlt = [P.sbuf("dlt", [128, T], BF16) for _ in range(2)]
            po = [P.sbuf("po", [128, T], BF16) for _ in range(2)]
            pw = P.sbuf("pw", [128, 2, 256], BF16)

            def pool_gen():
                for u_ in U + Wk:
                    P.V('memset', writes=[u_], ap=u_[:, 0:16], constant=0.0)
                for g in range(4):
                    P.dma('sync', icns[:, g, :], c_invcnt[g][:, PAD:PAD + 16], writes=[icns], key=icns)
                yield
                for g in range(4):
                    wwin = float(2 << g)
                    P.dma('gpsimd', pw[:], pool_w[l, g].rearrange("(kc p) m -> p kc m", p=128), writes=[pw], key=pw)
                    for half in range(2):
                        c = 2 * g + half
                        u_ = U[c % 2]
                        P.dma('sync', rowview(u_[:, 16:16 + T]), rowap(cols, C_POOL + c), writes=[u_, colsdep[c // 4]], key=u_)
                        yield
                        src = u_
                        for j in range(g + 1):
                            sh = 1 << j
                            dst = Wk[j % 2]
                            P.V('tensor_tensor', reads=[src], writes=[dst], out=dst[:, 16:16 + T], in0=src[:, 16:16 + T],
                                in1=src[:, 16 - sh:16 - sh + T], op=ALU.add)
                            src = dst
                            yield
                        other = Wk[(g + 1) % 2]
                        P.V('tensor_scalar', reads=[src], writes=[other], out=other[:, 16:16 + T], in0=src[:, 16:16 + T], scalar1=1.0 / wwin,
                            scalar2=None, op0=ALU.mult)
                        P.V('tensor_tensor', reads=[src, icns], writes=[other], out=other[:, 16 + PAD:16 + PAD + 16],
                            in0=src[:, 16 + PAD:16 + PAD + 16], in1=icns[:, g, :], op=ALU.mult)
                        yield
                        P.V('tensor_tensor', reads=[other, u_], writes=[dlt[half]], out=dlt[half][:], in0=other[:, 16:16 + T],
                            in1=u_[:, 16:16 + T], op=ALU.subtract)
                        yield
                    for mo in range(2):
                        co = 2 * g + mo
                        po_ = po[co % 2]
                        for tl in range(NTILES):
                            ps = next_ps()
                            for kc in range(2):
                                P.MM(reads=[pw, dlt[kc]], writes=[ps], out=ps[:, 0:NT], lhsT=pw[:, kc, mo * 128:(mo + 1) * 128],
                                     rhs=dlt[kc][:, tl * NT:(tl + 1) * NT], start=(kc == 0), stop=(kc == 1))
                            P.A('activation', reads=[ps, sp], writes=[po_], out=po_[:, tl * NT:(tl + 1) * NT], in_=ps[:, 0:NT],
                                func=AF.Identity, scale=sp[:, SP_PSC + co:SP_PSC + co + 1])
                            yield
                        P.dma('gpsimd', rowap(mix, co), rowview(po_[:]), reads=[po_], key=po_)
                        yield

            gen = pool_gen()
            it = 0

            def load_w(mg):
                w_ = wb[mg % 2]
                for half in range(2):
                    P.dma('gpsimd', w_[:, half * 8:(half + 1) * 8, :], wsrc[:, half * 8:(half + 1) * 8, mg * 512:(mg + 1) * 512],
                          writes=[w_], key=w_)
            load_w(0)
            for mg in range(21):
                w_ = wb[mg % 2]
                if mg + 1 < 21:
                    load_w(mg + 1)
                for tl in range(NTILES):
                    h_ = hb[it % 3]
                    o_ = ot[it % 2]
                    it += 1
                    P.dma('sync', h_[:], hbf[tl], writes=[h_], key=h_)
                    for mc in range(4):
                        ps = next_ps()
                        for kc in range(DC):
                            P.MM(reads=[w_, h_], writes=[ps], out=ps[:, 0:NT], lhsT=w_[:, kc, mc * 128:(mc + 1) * 128], rhs=h_[:, kc, :],
                                 start=(kc == 0), stop=(kc == DC - 1))
                        evac(o_[:, mc, :], ps[:, 0:NT], [ps], [o_])
                    P.dma('gpsimd', cols[tl, :, mg * 4:(mg + 1) * 4, :], o_[:], reads=[o_, colsdep[mg]], key=o_)
                    if mg >= 2:
                        next(gen, None)
            for _ in gen:
                pass
            P.barrier()

        def phase_pool(l):
            return

        def phase_lru(l):
            P.phase_begin()
            X = P.sbuf("X", [128, 3 + T], F32)
            XC = [P.sbuf("XC", [128, T], F32) for _ in range(2)]
            xcb = [P.sbuf("xcb", [128, T], BF16) for _ in range(2)]
            GA = P.sbuf("GA", [128, T], F32)
            GX = P.sbuf("GX", [128, T], F32)
            AR = P.sbuf("AR", [128, T], F32)
            TB = P.sbuf("TB", [128, T], F32)
            lo = P.sbuf("lo", [128, T], BF16)
            wa = P.sbuf("wa", [128, 2, 256], BF16)
            wx = P.sbuf("wx", [128, 2, 256], BF16)
            P.A('activation', reads=[sp], writes=[lamc], out=lamc[:, 0:8], in_=sp[:, SP_LAM:SP_LAM + 8], func=AF.Exp, scale=-1.0)
            P.A('activation', reads=[lamc], writes=[lamc], out=lamc[:, 0:8], in_=lamc[:, 0:8], func=AF.Ln, bias=1.0)
            P.A('mul', reads=[lamc], writes=[lamc], out=lamc[:, 8:16], in_=lamc[:, 0:8], mul=-16.0)
            P.A('mul', reads=[lamc], writes=[lamc], out=lamc[:, 0:8], in_=lamc[:, 0:8], mul=-8.0)
            P.V('memset', writes=[X], ap=X[:, 0:3], constant=0.0)
            for blk in range(4):
                P.dma('gpsimd', wa[:], lru_wa[l, blk].rearrange("(kc p) m -> p kc m", p=128), writes=[wa], key=wa)
                P.dma('gpsimd', wx[:], lru_wx[l, blk].rearrange("(kc p) m -> p kc m", p=128), writes=[wx], key=wx)
                for half in range(2):
                    c = 2 * blk + half
                    xc = XC[half]
                    P.dma('sync', rowview(X[:, 3:3 + T]), rowap(cols, C_LX + c), writes=[X], key=X)
                    P.V('tensor_scalar', reads=[X, sp], writes=[xc], out=xc[:], in0=X[:, 0:T], scalar1=sp[:, SP_CW + c:SP_CW + c + 1],
                        scalar2=sp[:, SP_CB + c:SP_CB + c + 1], op0=ALU.mult, op1=ALU.add)
                    for j in range(1, 4):
                        P.V('scalar_tensor_tensor', reads=[X, sp, xc], writes=[xc], out=xc[:], in0=X[:, j:j + T],
                            scalar=sp[:, SP_CW + j * 8 + c:SP_CW + j * 8 + c + 1], in1=xc[:], op0=ALU.mult, op1=ALU.add)
                    P.G('tensor_copy', reads=[xc], writes=[xcb[half]], out=xcb[half][:], in_=xc[:])
                for mo in range(2):
                    co = 2 * blk + mo
                    for tl in range(NTILES):
                        sl = slice(tl * NT, (tl + 1) * NT)
                        psa = next_ps()
                        for kc in range(2):
                            P.MM(reads=[wa, xcb[kc]], writes=[psa], out=psa[:, 0:NT], lhsT=wa[:, kc, mo * 128:(mo + 1) * 128],
                                 rhs=xcb[kc][:, sl], start=(kc == 0), stop=(kc == 1))
                        P.A('activation', reads=[psa, sp], writes=[GA], out=GA[:, sl], in_=psa[:, 0:NT], func=AF.Sigmoid,
                            bias=sp[:, SP_BA + co:SP_BA + co + 1])
                        psx = next_ps()
                        for kc in range(2):
                            P.MM(reads=[wx, xcb[kc]], writes=[psx], out=psx[:, 0:NT], lhsT=wx[:, kc, mo * 128:(mo + 1) * 128],
                                 rhs=xcb[kc][:, sl], start=(kc == 0), stop=(kc == 1))
                        P.A('activation', reads=[psx, sp], writes=[GX], out=GX[:, sl], in_=psx[:, 0:NT], func=AF.Sigmoid,
                            bias=sp[:, SP_BX + co:SP_BX + co + 1])
                    P.A('activation', reads=[GA, lamc], writes=[AR], out=AR[:], in_=GA[:], func=AF.Exp, scale=lamc[:, co:co + 1])
                    P.A('activation', reads=[GA, lamc], writes=[TB], out=TB[:], in_=GA[:], func=AF.Exp, scale=lamc[:, 8 + co:9 + co])
                    P.V('tensor_scalar', reads=[TB], writes=[TB], out=TB[:], in0=TB[:], scalar1=-1.0, scalar2=1.0, op0=ALU.mult, op1=ALU.add)
                    P.V('tensor_scalar', reads=[TB], writes=[TB], out=TB[:], in0=TB[:], scalar1=0.0, scalar2=None, op0=ALU.max)
                    P.A('activation', reads=[TB], writes=[TB], out=TB[:], in_=TB[:], func=AF.Sqrt)
                    P.V('tensor_tensor', reads=[TB, GX], writes=[TB], out=TB[:], in0=TB[:], in1=GX[:], op=ALU.mult)
                    P.V('tensor_tensor', reads=[TB, XC[mo]], writes=[TB], out=TB[:], in0=TB[:], in1=XC[mo][:], op=ALU.mult)
                    P.V('memset', writes=[TB], ap=TB[:, 0:PAD], constant=0.0)
                    P.V('tensor_tensor_scan', reads=[AR, TB], writes=[GA], out=GA[:], data0=AR[:], data1=TB[:], initial=0.0,
                        op0=ALU.mult, op1=ALU.add)
                    P.dma('sync', rowview(GX[:]), rowap(cols, C_LY + co), writes=[GX], key=GX)
                    P.A('activation', reads=[GX], writes=[GX], out=GX[:], in_=GX[:], func=AF.Gelu)
                    P.V('tensor_tensor', reads=[GA, GX], writes=[lo], out=lo[:], in0=GA[:], in1=GX[:], op=ALU.mult)
                    P.dma('gpsimd', rowap(mix, 16 + co), rowview(lo[:]), reads=[lo], key=lo)
            P.barrier()

        def phase_attn(l):
            P.phase_begin()
            AB = P.sbuf("AB", [128, 16, 256], F32)
            PM = P.sbuf("PM", [128, 256], F32)
            VT = P.sbuf("VT", [128, NBLK + 1, 256], BF16)
            kT = P.sbuf("kT", [64, 4, 128 + T], BF16)
            vrow = P.sbuf("vrow", [128, T], BF16)
            qT = [P.sbuf("qT", [64, T], BF16) for _ in range(2)]
            AO = [P.sbuf("AO", [128, T], BF16) for _ in range(2)]
            ssb = [P.sbuf("ssb", [128, 256], F32) for _ in range(4)]
            pn = [P.sbuf("pn", [128, 256], BF16) for _ in range(4)]
            pT = [P.sbuf("pT", [128, 256], BF16) for _ in range(4)]
            sm = [P.sbuf("sm", [128, 8], F32) for _ in range(4)]
            psS = [psf[0], psf[1]]
            psO = [psf[2], psf[3]]
            P.dma('sync', AB[:], c_ab, writes=[AB], key=AB)
            P.dma('sync', PM[:], c_pm, writes=[PM], key=PM)
            P.V('memset', writes=[VT], ap=VT[:, 0, :], constant=0.0)
            P.V('memset', writes=[kT], ap=kT[:, :, 0:128], constant=0.0)
            for j in range(4):
                P.dma('gpsimd', rowview(kT[0:64, j, 128:128 + T]), rowap(cols, C_K + j // 2, (j % 2) * 64, (j % 2) * 64 + 64),
                      writes=[kT], key=kT)
            for vc in range(2):
                P.dma('gpsimd', rowview(vrow[:]), rowap(cols, C_V + vc), writes=[vrow], key=vrow)
                for blk in range(NBLK):
                    pb = psb[blk % 2]
                    P.TR(reads=[vrow, identb], writes=[pb], out=pb[:, 0:128], in_=vrow[:, blk * 128:(blk + 1) * 128], identity=identb[:])
                    evac(VT[:, blk + 1, vc * 128:(vc + 1) * 128], pb[:, 0:128], [pb], [VT])
            P.barrier()

            class Reg:
                def __init__(self, ap):
                    self.ap = ap
                    self.buf = Buf("reg")

            DEP = 4
            rS = [Reg(psf[k // 2][:, (k % 2) * 256:(k % 2) * 256 + 256]) for k in range(4)]
            rO = [psf[2][:, k * 128:(k + 1) * 128] for k in range(4)]
            rOb = [Reg(None) for _ in range(4)]
            rB = [Reg(psb[k // 4][:, (k % 4) * 256:(k % 4) * 256 + 256]) for k in range(8)]
            iters = [(h, blk) for h in range(16) for blk in range(NBLK)]
            NI = len(iters)

            def stageA(i):
                h, blk = iters[i]
                j = h // 4
                q_ = qT[h % 2]
                p0 = (h % 2) * 64
                if blk == 0:
                    P.dma('gpsimd', rowview(q_[0:64, :]), rowap(cols, C_Q + h // 2, p0, p0 + 64), writes=[q_], key=q_)
                s_, sm_, pS = ssb[i % DEP], sm[i % DEP], rS[i % 4]
                P.MM(reads=[q_, kT], writes=[pS], out=pS.ap, lhsT=q_[0:64, blk * 128:(blk + 1) * 128],
                     rhs=kT[0:64, j, blk * 128:blk * 128 + 256], start=True, stop=True)
                P.V('scalar_tensor_tensor', reads=[pS, AB], writes=[s_], out=s_[:], in0=pS.ap, scalar=0.125, in1=AB[:, h, :],
                    op0=ALU.mult, op1=ALU.add)
                if blk == 0:
                    P.V('tensor_tensor', reads=[s_, PM], writes=[s_], out=s_[:], in0=s_[:], in1=PM[:], op=ALU.add)
                if blk == 1:
                    P.V('tensor_tensor', reads=[s_, PM], writes=[s_], out=s_[:, 0:PAD], in0=s_[:, 0:PAD], in1=PM[:, 0:PAD], op=ALU.add)
                P.V('reduce_max', reads=[s_], writes=[sm_], out=sm_[:, 0:1], in_=s_[:], axis=AX.X)
                P.V('tensor_scalar', reads=[sm_, sp], writes=[sm_], out=sm_[:, 1:2], in0=sm_[:, 0:1],
                    scalar1=sp[:, SP_SINK + h:SP_SINK + h + 1], scalar2=-1.0, op0=ALU.max, op1=ALU.mult)

            def stageB_act(i):
                h, blk = iters[i]
                s_, sm_ = ssb[i % DEP], sm[i % DEP]
                P.A('activation', reads=[s_, sm_], writes=[s_, sm_], out=s_[:], in_=s_[:], func=AF.Exp, bias=sm_[:, 1:2],
                    accum_out=sm_[:, 2:3])
                P.A('activation', reads=[sp, sm_], writes=[sm_], out=sm_[:, 3:4], in_=sp[:, SP_SINK + h:SP_SINK + h + 1], func=AF.Exp,
                    bias=sm_[:, 1:2])

            def stageB_dve(i):
                s_, sm_, pn_ = ssb[i % DEP], sm[i % DEP], pn[i % DEP]
                P.V('tensor_tensor', reads=[sm_], writes=[sm_], out=sm_[:, 4:5], in0=sm_[:, 2:3], in1=sm_[:, 3:4], op=ALU.add)
                P.V('reciprocal', reads=[sm_], writes=[sm_], out=sm_[:, 5:6], in_=sm_[:, 4:5])
                P.V('tensor_scalar', reads=[s_, sm_], writes=[pn_], out=pn_[:], in0=s_[:], scalar1=sm_[:, 5:6], scalar2=None, op0=ALU.mult)

            def stageC1(i):
                pn_, pT_, pb = pn[i % DEP], pT[i % DEP], rB[i % 8]
                for kb in range(2):
                    P.TR(reads=[pn_, identb], writes=[pb], out=pb.ap[:, kb * 128:(kb + 1) * 128], in_=pn_[:, kb * 128:(kb + 1) * 128],
                         identity=identb[:])
                P.A('copy', reads=[pb], writes=[pT_], out=pT_[:], in_=pb.ap)

            def stageC2(i):
                h, blk = iters[i]
                j = h // 4
                p0 = (h % 2) * 64
                ao = AO[(h // 2) % 2]
                pT_, pO, pOb = pT[i % DEP], rO[i % 4], rOb[i % 4]
                for kb in range(2):
                    P.MM(reads=[VT, pT_], writes=[pOb], out=pO[p0:p0 + 64, :], lhsT=VT[:, blk + kb, j * 64:(j + 1) * 64],
                         rhs=pT_[:, kb * 128:(kb + 1) * 128], start=(kb == 0), stop=(kb == 1))
                P.A('copy', reads=[pOb], writes=[ao], out=ao[p0:p0 + 64, blk * 128:(blk + 1) * 128], in_=pO[p0:p0 + 64, :])
                if h % 2 == 1 and blk == NBLK - 1:
                    P.dma('gpsimd', rowap(mix, 8 + h // 2), rowview(ao[:]), reads=[ao], key=ao)

            for step in range(NI + 3):
                if step - 1 >= 0 and step - 1 < NI:
                    stageB_act(step - 1)
                if step - 3 >= 0 and step - 3 < NI:
                    stageC2(step - 3)
                if step < NI:
                    stageA(step)
                if step - 2 >= 0 and step - 2 < NI:
                    stageC1(step - 2)
                if step - 1 >= 0 and step - 1 < NI:
                    stageB_dve(step - 1)
            P.barrier()

        def phase_merge(l):
            P.phase_begin()
            pw = [[P.sbuf("pjw", [128, 8, 1024], BF16) for _ in range(3)]]
            mx = [P.sbuf("mx", [128, 24, NT], BF16) for _ in range(2)]
            gt = [P.sbuf("gt", [128, 3, 8, NT], F32) for _ in range(2)]
            acc = [P.sbuf("acc", [128, NT], F32) for _ in range(2)]
            tm = [P.sbuf("tm", [128, NT], F32) for _ in range(2)]
            mt = [P.sbuf("mt", [128, 8, NT], BF16) for _ in range(2)]
            it = 0
            def load_pw(mg):
                for i in range(3):
                    P.dma('gpsimd', pw[0][i][:], proj[i][l].rearrange("(kc p) m -> p kc m", p=128)[:, :, mg * 1024:(mg + 1) * 1024],
                          writes=[pw[0][i]], key=pw[0][i])
            for mg in range(2):
                pws = pw[0]
                load_pw(mg)
                for tl in range(NTILES):
                    mx_, gt_, mt_ = mx[it % 2], gt[it % 2], mt[it % 2]
                    it += 1
                    P.dma('sync', mx_[:], mix[tl], writes=[mx_], key=mx_)
                    for i in range(3):
                        c0 = C_G + i * 16 + mg * 8
                        P.dma('sync', gt_[:, i, :, :], cols[tl, :, c0:c0 + 8, :], writes=[gt_], key=gt_)
                    P.A('activation', reads=[gt_], writes=[gt_], out=gt_[:].rearrange("p a b n -> p (a b n)"), in_=gt_[:].rearrange("p a b n -> p (a b n)"), func=AF.Sigmoid)
                    for mc in range(8):
                        ac, t_ = acc[mc % 2], tm[mc % 2]
                        for i in range(3):
                            ps = next_ps()
                            for kc in range(8):
                                P.MM(reads=[pws[i], mx_], writes=[ps], out=ps[:, 0:NT], lhsT=pws[i][:, kc, mc * 128:(mc + 1) * 128],
                                     rhs=mx_[:, i * 8 + kc, :], start=(kc == 0), stop=(kc == 7))
                            if i == 0:
                                P.V('tensor_tensor', reads=[ps, gt_], writes=[ac], out=ac[:], in0=ps[:, 0:NT], in1=gt_[:, i, mc, :], op=ALU.mult)
                            else:
                                P.V('tensor_tensor', reads=[ps, gt_], writes=[t_], out=t_[:], in0=ps[:, 0:NT], in1=gt_[:, i, mc, :], op=ALU.mult)
                                if i == 1:
                                    P.V('tensor_tensor', reads=[ac, t_], writes=[ac], out=ac[:], in0=ac[:], in1=t_[:], op=ALU.add)
                                else:
                                    P.V('tensor_tensor', reads=[ac, t_], writes=[mt_], out=mt_[:, mc, :], in0=ac[:], in1=t_[:], op=ALU.add)
                    P.dma('gpsimd', merged[tl, :, mg * 8:(mg + 1) * 8, :], mt_[:], reads=[mt_], key=mt_)
            P.barrier()

        def phase_wout(l):
            P.phase_begin()
            wo = P.sbuf("wo", [128, DC, D], BF16)
            mg_ = [P.sbuf("mgd", [128, DC, NT], BF16) for _ in range(2)]
            hz = [P.sbuf("hz", [128, DC, NT], F32) for _ in range(2)]
            tmp = {'zsq': [P.sbuf("zsq", [128, NT], F32) for _ in range(2)], 'mean': P.sbuf("mean", [128, NT], F32),
                   'rstd': P.sbuf("rstd", [128, NT], F32)}
            tmf = [P.sbuf("tmf", [128, D], F32) for _ in range(2)]
            tmb = [P.sbuf("tmb", [128, D], BF16) for _ in range(2)]
            wr = P.sbuf("wr", [128, DC, 36], F32)
            rb = P.sbuf("rb", [1, 36], F32)
            P.dma('sync', wr[:].rearrange("p a b -> p (a b)"), rw_tab[l], writes=[wr], key=wr)
            P.dma('sync', rb[:], rbias[l], writes=[rb], key=rb)
            wsrc = w_out[l].rearrange("(kc p) m -> p kc m", p=128)
            for q4 in range(4):
                for half in range(2):
                    P.dma('gpsimd', wo[:, half * 8:(half + 1) * 8, q4 * 512:(q4 + 1) * 512],
                          wsrc[:, half * 8:(half + 1) * 8, q4 * 512:(q4 + 1) * 512], writes=[wo], key=wo)
            for tl in range(NTILES):
                m_, z_ = mg_[tl % 2], hz[tl % 2]
                P.dma('sync', m_[:], merged[tl], writes=[m_], key=m_)
                P.dma('sync', z_[:], hres[tl], writes=[z_], key=z_)
                for mc in range(DC):
                    ps = next_ps()
                    for kc in range(DC):
                        P.MM(reads=[wo, m_], writes=[ps], out=ps[:, 0:NT], lhsT=wo[:, kc, mc * 128:(mc + 1) * 128], rhs=m_[:, kc, :],
                             start=(kc == 0), stop=(kc == DC - 1))
                    P.V('scalar_tensor_tensor', reads=[z_, ps], writes=[z_], out=z_[:, mc, :], in0=z_[:, mc, :], scalar=ALPHA, in1=ps[:, 0:NT],
                        op0=ALU.mult, op1=ALU.add)
                emit_ln(z_, NT, lambda mc: sp[:, SP_LN1G + mc:SP_LN1G + mc + 1], lambda mc: sp[:, SP_LN1B + mc:SP_LN1B + mc + 1], tmp,
                        zero_cols=(PAD if tl == 0 else 0), hb=None)
                for sb3 in range(3):
                    blk = tl * 3 + sb3
                    tf, tb = tmf[blk % 2], tmb[blk % 2]
                    pl = next_ps()
                    for kc in range(DC):
                        P.MM(reads=[z_, wr], writes=[pl], out=pl[:, 0:36], lhsT=z_[:, kc, sb3 * 128:(sb3 + 1) * 128], rhs=wr[:, kc, :],
                             start=(kc == 0), stop=False)
                    P.MM(reads=[onesf, rb], writes=[pl], out=pl[:, 0:36], lhsT=onesf[0:1, :], rhs=rb[0:1, :], start=False, stop=True)
                    P.V('tensor_copy', reads=[pl], writes=[Lall], out=Lall[:, blk, :], in_=pl[:, 0:36])
                    for c4 in range(4):
                        ps = next_ps()
                        for cc in range(4):
                            c = c4 * 4 + cc
                            P.TR(reads=[z_, identf], writes=[ps], out=ps[:, cc * 128:(cc + 1) * 128],
                                 in_=z_[:, c, sb3 * 128:(sb3 + 1) * 128], identity=identf[:])
                        evac(tf[:, c4 * 512:(c4 + 1) * 512], ps[:, 0:512], [ps], [tf])
                    P.G('tensor_copy', reads=[tf], writes=[tb], out=tb[:], in_=tf[:])
                    P.dma('gpsimd', h1tm_f[blk * 128:(blk + 1) * 128, :], tf[:], reads=[tf], key=tf)
                    P.dma('gpsimd', h1tm_b[blk * 128:(blk + 1) * 128, :], tb[:], reads=[tb], key=tb)
            P.barrier()

        def phase_router(l):
            P.phase_begin()
            ELm = [P.sbuf("ELm", [128, 32], F32) for _ in range(2)]
            sc = [P.sbuf("sc", [128, 32], F32) for _ in range(2)]
            M1a = P.sbuf("M1a", [128, NBLK, 32], F32)
            M2a = P.sbuf("M2a", [128, NBLK, 32], F32)
            M12b = P.sbuf("M12b", [128, NBLK, 32], BF16)
            onesb = P.sbuf("onesb", [128, 128], BF16)
            trib = P.sbuf("trib", [128, 128], BF16)
            thr = P.sbuf("thr", [128, NSB], F32)
            pio = P.sbuf("pio", [128, 1], F32)
            cnt = P.sbuf("cnt", [128, 32], F32)
            nbk = P.sbuf("nbk", [128, 32], F32)
            pend = P.sbuf("pend", [128, 32], F32)
            pstart = P.sbuf("pstart", [128, 32], F32)
            Dm = [P.sbuf("Dm", [128, 32], F32) for _ in range(2)]
            tt = [P.sbuf("tt", [128, 32], F32) for _ in range(2)]
            destf = P.sbuf("destf", [128, NBLK, 2], F32)
            be = P.sbuf("be", [128, NSB], F32)
            chg = P.sbuf("chg", [128, NSB], F32)
            gb = P.sbuf("gb", [128, NSB], F32)
            db = P.sbuf("db", [128, NSB], F32)
            widf = P.sbuf("widf", [128, NSB, 16], F32)
            didf = P.sbuf("didf", [128, NSB, 4], F32)
            padfix = P.sbuf("padfix", [128, 2], F32)
            P.dma('sync', trib[:], c_tri, writes=[trib], key=trib)
            P.dma('sync', thr[:], c_thr, writes=[thr], key=thr)
            P.dma('sync', pio[:], c_piota, writes=[pio], key=pio)
            P.V('memset', writes=[onesb], ap=onesb[:], constant=1.0)
            for blk in range(NBLK):
                E_, s_ = ELm[blk % 2], sc[blk % 2]
                L_ = Lall
                P.V('reduce_max', reads=[L_], writes=[s_], out=s_[:, 0:1], in_=Lall[:, blk, 0:4], axis=AX.X)
                P.V('tensor_scalar', reads=[s_], writes=[s_], out=s_[:, 1:2], in0=s_[:, 0:1], scalar1=-1.0, scalar2=None, op0=ALU.mult)
                P.A('activation', reads=[L_, s_], writes=[s_], out=s_[:, 24:28], in_=Lall[:, blk, 0:4], func=AF.Exp, bias=s_[:, 1:2],
                    accum_out=s_[:, 2:3])
                P.V('reciprocal', reads=[s_], writes=[s_], out=s_[:, 3:4], in_=s_[:, 2:3])
                P.V('tensor_scalar', reads=[L_, s_], writes=[s_], out=s_[:, 4:8], in0=Lall[:, blk, 0:4], scalar1=s_[:, 0:1], scalar2=None,
                    op0=ALU.is_equal)
                P.V('tensor_scalar', reads=[s_], writes=[s_], out=s_[:, 4:8], in0=s_[:, 4:8], scalar1=-1.0, scalar2=1e30, op0=ALU.add,
                    op1=ALU.mult)
                for g in range(4):
                    P.V('tensor_scalar', reads=[L_, s_], writes=[E_], out=E_[:, g * 8:(g + 1) * 8], in0=Lall[:, blk, 4 + g * 8:12 + g * 8],
                        scalar1=s_[:, 4 + g:5 + g], scalar2=None, op0=ALU.add)
                P.V('max', reads=[E_], writes=[s_], out=s_[:, 8:16], in_=E_[:])
                P.V('tensor_tensor', reads=[s_], writes=[s_], out=s_[:, 16:17], in0=s_[:, 8:9], in1=s_[:, 9:10], op=ALU.subtract)
                P.A('activation', reads=[s_], writes=[s_], out=s_[:, 17:18], in_=s_[:, 16:17], func=AF.Sigmoid)
                P.V('tensor_scalar', reads=[s_], writes=[s_], out=s_[:, 18:19], in0=s_[:, 17:18], scalar1=-1.0, scalar2=1.0, op0=ALU.mult,
                    op1=ALU.add)
                P.V('tensor_scalar', reads=[s_], writes=[cw], out=cw[:, blk, :], in0=s_[:, 17:19], scalar1=s_[:, 3:4], scalar2=None,
                    op0=ALU.mult)
                P.V('tensor_scalar', reads=[E_, s_], writes=[M1a], out=M1a[:, blk, :], in0=E_[:], scalar1=s_[:, 8:9], scalar2=None,
                    op0=ALU.is_equal)
                P.V('tensor_scalar', reads=[E_, s_], writes=[M2a], out=M2a[:, blk, :], in0=E_[:], scalar1=s_[:, 9:10], scalar2=None,
                    op0=ALU.is_equal)
                if blk == 0:
                    P.V('memset', writes=[M1a], ap=M1a[0:PAD, 0, :], constant=0.0)
                    P.V('memset', writes=[M2a], ap=M2a[0:PAD, 0, :], constant=0.0)
                P.V('tensor_tensor', reads=[M1a, M2a], writes=[M12b], out=M12b[:, blk, :], in0=M1a[:, blk, :], in1=M2a[:, blk, :], op=ALU.add)
            pc = next_ps()
            for blk in range(NBLK):
                P.MM(reads=[onesb, M12b], writes=[pc], out=pc[:, 0:32], lhsT=onesb[:], rhs=M12b[:, blk, :], start=(blk == 0),
                     stop=(blk == NBLK - 1))
            P.V('tensor_copy', reads=[pc], writes=[cnt], out=cnt[:], in_=pc[:, 0:32])
            P.V('memset', writes=[nbk], ap=nbk[:], constant=0.0)
            for k in range(34):
                P.V('scalar_tensor_tensor', reads=[cnt, nbk], writes=[nbk], out=nbk[:], in0=cnt[:], scalar=float(128 * k), in1=nbk[:],
                    op0=ALU.is_gt, op1=ALU.add)
            P.V('tensor_scalar', reads=[nbk], writes=[nbk], out=nbk[:], in0=nbk[:], scalar1=128.0, scalar2=None, op0=ALU.mult)
            P.V('tensor_tensor_scan', reads=[onesf, nbk], writes=[pend], out=pend[:], data0=onesf[:, 0:32], data1=nbk[:], initial=0.0,
                op0=ALU.mult, op1=ALU.add)
            P.V('tensor_tensor', reads=[pend, nbk], writes=[pstart], out=pstart[:], in0=pend[:], in1=nbk[:], op=ALU.subtract)
            for blk in range(NBLK):
                pC = next_ps()
                P.MM(reads=[trib, M12b], writes=[pC], out=pC[:, 0:32], lhsT=trib[:], rhs=M12b[:, blk, :], start=True, stop=(blk == 0))
                for j in range(blk):
                    P.MM(reads=[onesb, M12b], writes=[pC], out=pC[:, 0:32], lhsT=onesb[:], rhs=M12b[:, j, :], start=False, stop=(j == blk - 1))
                D_, t_ = Dm[blk % 2], tt[blk % 2]
                P.V('tensor_tensor', reads=[pC, pstart], writes=[D_], out=D_[:], in0=pC[:, 0:32], in1=pstart[:], op=ALU.add)
                for k, Ma in enumerate((M1a, M2a)):
                    P.V('tensor_tensor', reads=[Ma, D_], writes=[t_], out=t_[:], in0=Ma[:, blk, :], in1=D_[:], op=ALU.mult)
                    P.V('reduce_sum', reads=[t_], writes=[destf], out=destf[:, blk, k:k + 1], in_=t_[:], axis=AX.X)
            P.V('reduce_sum', reads=[M1a], writes=[padfix], out=padfix[:, 0:1], in_=M1a[:, 0, :], axis=AX.X)
            P.V('tensor_scalar', reads=[padfix], writes=[padfix], out=padfix[:, 1:2], in0=padfix[:, 0:1], scalar1=-BIGI, scalar2=BIGI,
                op0=ALU.mult, op1=ALU.add)
            for k in range(2):
                P.V('tensor_tensor', reads=[destf, padfix], writes=[destf], out=destf[:, 0, k:k + 1], in0=destf[:, 0, k:k + 1],
                    in1=padfix[:, 1:2], op=ALU.add)
            P.V('tensor_copy', reads=[destf], writes=[dest_i], out=dest_i[:], in_=destf[:])
            P.V('memset', writes=[be], ap=be[:], constant=0.0)
            for e in range(NEXP):
                P.V('scalar_tensor_tensor', reads=[thr, pend, be], writes=[be], out=be[:], in0=thr[:], scalar=pend[:, e:e + 1], in1=be[:],
                    op0=ALU.is_ge, op1=ALU.add)
            P.V('tensor_scalar', reads=[be], writes=[be], out=be[:], in0=be[:], scalar1=31.0, scalar2=None, op0=ALU.min)
            P.V('memset', writes=[chg], ap=chg[:, 0:1], constant=1.0)
            P.V('tensor_tensor', reads=[be], writes=[chg], out=chg[:, 1:NSB], in0=be[:, 1:NSB], in1=be[:, 0:NSB - 1], op=ALU.not_equal)
            for (dst, mul) in ((gb, 128.0), (db, 128.0)):
                P.V('tensor_scalar', reads=[be], writes=[dst], out=dst[:], in0=be[:], scalar1=mul, scalar2=float(l * NEXP) * mul - BIGI, op0=ALU.mult, op1=ALU.add)
                P.V('tensor_tensor', reads=[dst, chg], writes=[dst], out=dst[:], in0=dst[:], in1=chg[:], op=ALU.mult)
                P.V('tensor_scalar', reads=[dst], writes=[dst], out=dst[:], in0=dst[:], scalar1=BIGI, scalar2=None, op0=ALU.add)
                P.V('tensor_scalar', reads=[dst, pio], writes=[dst], out=dst[:], in0=dst[:], scalar1=pio[:, 0:1], scalar2=None, op0=ALU.add)
            for kc in range(16):
                P.V('tensor_scalar', reads=[gb], writes=[widf], out=widf[:, :, kc], in0=gb[:], scalar1=float(kc * 128), scalar2=None, op0=ALU.add)
            for kc in range(4):
                P.V('tensor_scalar', reads=[db], writes=[didf], out=didf[:, :, kc], in0=db[:], scalar1=float(kc * 128), scalar2=None, op0=ALU.add)
            P.V('tensor_copy', reads=[widf], writes=[widx], out=widx[:], in_=widf[:])
            P.V('tensor_copy', reads=[didf], writes=[didx], out=didx[:], in_=didf[:])
            P.barrier()

        def phase_moe(l, last):
            P.phase_begin()
            xsrc = [P.sbuf("xsrc", [128, D], BF16) for _ in range(2)]
            for blk in range(NBLK):
                x_ = xsrc[blk % 2]
                P.dma('sync', x_[:], h1tm_b[blk * 128:(blk + 1) * 128, :], writes=[x_], key=x_)
                for k in range(2):
                    P.dma_fn('gpsimd', lambda e, x_=x_, blk=blk, k=k: e.indirect_dma_start(
                        out=xs[:, :], out_offset=bass.IndirectOffsetOnAxis(ap=dest_i[:, blk, k:k + 1], axis=0), in_=x_[:, :], in_offset=None,
                        bounds_check=_breg(e, CAP - 1), oob_is_err=False), reads=[x_, dest_i], key=x_)
            P.barrier()
            P.phase_begin()
            xsb = [P.sbuf("xsb", [128, D], BF16) for _ in range(2)]
            xT = [P.sbuf("xT", [128, D], BF16) for _ in range(2)]
            wg = P.sbuf("wg", [128, DC, 512], BF16)
            wu = P.sbuf("wu", [128, DC, 512], BF16)
            wd = P.sbuf("wd", [128, 4, D], BF16)
            sgt = [P.sbuf("sgt", [128, 512], F32) for _ in range(2)]
            hdt = [P.sbuf("hdt", [128, 512], BF16) for _ in range(2)]
            hdT = [P.sbuf("hdT", [128, 512], BF16) for _ in range(2)]
            yblk = [P.sbuf("yblk", [128, D], F32) for _ in range(2)]
            wst = [P.sbuf("wst", [128, 8192], F32) for _ in range(3)]
            gsrc = ewg.rearrange("l e (p kc) m -> (l e p) (kc m)", kc=16)
            usrc = ewu.rearrange("l e (p kc) m -> (l e p) (kc m)", kc=16)
            dsrc = ewd.rearrange("l e (p kc) m -> (l e p) (kc m)", kc=4)
            wbound = (l + 1) * NEXP * 128 - 1
            for b in range(NSB):
                x_, xT_, sg_, hd_, hT_, y_ = xsb[b % 2], xT[b % 2], sgt[b % 2], hdt[b % 2], hdT[b % 2], yblk[b % 2]
                P.dma('sync', x_[:], xs[b * 128:(b + 1) * 128, :], writes=[x_], key=x_)
                for (stg, src) in zip(wst, (gsrc, usrc, dsrc)):
                    P.dma_fn('gpsimd', lambda e, stg=stg, src=src, b=b: e.indirect_dma_start(
                        out=stg[:, :], out_offset=None, in_=src[:, :],
                        in_offset=bass.IndirectOffsetOnAxis(ap=widx[:, b, 0:1], axis=0), bounds_check=_breg(e, wbound), oob_is_err=False),
                        reads=[widx], writes=[stg], key=stg)
                P.A('copy', reads=[wst[0]], writes=[wg], out=wg[:].rearrange("p a b -> p (a b)"), in_=wst[0][:])
                P.V('tensor_copy', reads=[wst[1]], writes=[wu], out=wu[:].rearrange("p a b -> p (a b)"), in_=wst[1][:])
                P.A('copy', reads=[wst[2]], writes=[wd], out=wd[:, 0:2, :].rearrange("p a b -> p (a b)"), in_=wst[2][:, 0:4096])
                P.V('tensor_copy', reads=[wst[2]], writes=[wd], out=wd[:, 2:4, :].rearrange("p a b -> p (a b)"), in_=wst[2][:, 4096:8192])
                for half in range(2):
                    pb = psb[half]
                    for c8 in range(8):
                        c = half * 8 + c8
                        P.TR(reads=[x_, identb], writes=[pb], out=pb[:, c8 * 128:(c8 + 1) * 128], in_=x_[:, c:D:16],
                             identity=identb[:])
                    evac(xT_[:, half * 1024:(half + 1) * 1024], pb[:, 0:1024], [pb], [xT_])
                pg = next_ps()
                for kc in range(DC):
                    P.MM(reads=[xT_, wg], writes=[pg], out=pg[:, 0:512], lhsT=xT_[:, kc * 128:(kc + 1) * 128], rhs=wg[:, kc, :],
                         start=(kc == 0), stop=(kc == DC - 1))
                pu = next_ps()
                for kc in range(DC):
                    P.MM(reads=[xT_, wu], writes=[pu], out=pu[:, 0:512], lhsT=xT_[:, kc * 128:(kc + 1) * 128], rhs=wu[:, kc, :],
                         start=(kc == 0), stop=(kc == DC - 1))
                P.A('activation', reads=[pg], writes=[sg_], out=sg_[:], in_=pg[:, 0:512], func=AF.Silu)
                P.V('tensor_tensor', reads=[sg_, pu], writes=[hd_], out=hd_[:], in0=sg_[:], in1=pu[:, 0:512], op=ALU.mult)
                pb = psb[b % 2]
                for kc in range(4):
                    P.TR(reads=[hd_, identb], writes=[pb], out=pb[:, kc * 128:(kc + 1) * 128], in_=hd_[:, kc:512:4],
                         identity=identb[:])
                evac(hT_[:], pb[:, 0:512], [pb], [hT_])
                for fg in range(4):
                    py = next_ps()
                    for kc in range(4):
                        P.MM(reads=[hT_, wd], writes=[py], out=py[:, 0:512], lhsT=hT_[:, kc * 128:(kc + 1) * 128],
                             rhs=wd[:, kc, fg * 512:(fg + 1) * 512], start=(kc == 0), stop=(kc == 3))
                    evac(y_[:, fg * 512:(fg + 1) * 512], py[:, 0:512], [py], [y_])
                P.dma('sync', yb[b * 128:(b + 1) * 128, :], y_[:], reads=[y_], key=y_)
            P.barrier()
            P.phase_begin()
            G1 = [P.sbuf("G1", [128, D], F32) for _ in range(2)]
            G2 = [P.sbuf("G2", [128, D], F32) for _ in range(2)]
            h1t = [P.sbuf("h1t", [128, D], F32) for _ in range(2)]
            stt = P.sbuf("stt", [128, 4, 6], F32)
            mv = P.sbuf("mv", [128, 2], F32)
            rs = P.sbuf("rs", [128, 1], F32)
            if last:
                gbc = P.sbuf("gbc", [128, D], F32)
                bbc = P.sbuf("bbc", [128, D], F32)
                P.dma('sync', gbc[:], ln2g_bc[l], writes=[gbc], key=gbc)
                P.dma('sync', bbc[:], ln2b_bc[l], writes=[bbc], key=bbc)
            else:
                hs = [P.sbuf("hs", [128, DC, NT], F32) for _ in range(2)]
                hsb = [P.sbuf("hsb", [128, DC, NT], BF16) for _ in range(2)]
            for g_ in G1 + G2:
                P.V('memset', writes=[g_], ap=g_[:], constant=0.0)
            for blk in range(NBLK):
                z_, g2_, h_ = G1[blk % 2], G2[blk % 2], h1t[blk % 2]
                for k, gt_ in enumerate((z_, g2_)):
                    P.dma_fn('gpsimd', lambda e, gt_=gt_, blk=blk, k=k: e.indirect_dma_start(
                        out=gt_[:, :], out_offset=None, in_=yb[:, :], in_offset=bass.IndirectOffsetOnAxis(ap=dest_i[:, blk, k:k + 1], axis=0),
                        bounds_check=_breg(e, CAP - 1), oob_is_err=False), reads=[dest_i], writes=[gt_], key=gt_)
                P.dma('sync', h_[:], h1tm_f[blk * 128:(blk + 1) * 128, :], writes=[h_], key=h_)
                P.V('tensor_scalar', reads=[z_, cw], writes=[z_], out=z_[:], in0=z_[:], scalar1=cw[:, blk, 0:1], scalar2=None, op0=ALU.mult)
                P.V('scalar_tensor_tensor', reads=[g2_, cw, z_], writes=[z_], out=z_[:], in0=g2_[:], scalar=cw[:, blk, 1:2], in1=z_[:],
                    op0=ALU.mult, op1=ALU.add)
                P.V('scalar_tensor_tensor', reads=[h_, z_], writes=[z_], out=z_[:], in0=h_[:], scalar=ALPHA, in1=z_[:], op0=ALU.mult,
                    op1=ALU.add)
                for j in range(4):
                    P.V('bn_stats', reads=[z_], writes=[stt], out=stt[:, j, :], in_=z_[:, j * 512:(j + 1) * 512])
                P.V('bn_aggr', reads=[stt], writes=[mv], out=mv[:], in_=stt[:].rearrange("p a b -> p (a b)"))
                P.V('tensor_scalar', reads=[mv], writes=[rs], out=rs[:], in0=mv[:, 1:2], scalar1=EPS, scalar2=None, op0=ALU.add)
                P.A('activation', reads=[rs], writes=[rs], out=rs[:], in_=rs[:], func=AF.Sqrt)
                P.V('reciprocal', reads=[rs], writes=[rs], out=rs[:], in_=rs[:])
                P.V('tensor_scalar', reads=[z_, mv, rs], writes=[z_], out=z_[:], in0=z_[:], scalar1=mv[:, 0:1], scalar2=rs[:, 0:1],
                    op0=ALU.subtract, op1=ALU.mult)
                if last:
                    if blk == 0:
                        continue
                    P.V('tensor_tensor', reads=[z_, gbc], writes=[z_], out=z_[:], in0=z_[:], in1=gbc[:], op=ALU.mult)
                    P.V('tensor_tensor', reads=[z_, bbc], writes=[z_], out=z_[:], in0=z_[:], in1=bbc[:], op=ALU.add)
                    P.dma('sync', out[(blk - 1) * 128:blk * 128, :], z_[:], reads=[z_], key=z_)
                else:
                    tl, off = blk // 3, (blk % 3) * 128
                    ho, hb_ = hs[tl % 2], hsb[tl % 2]
                    for c4 in range(4):
                        ps = next_ps()
                        for cc in range(4):
                            c = c4 * 4 + cc
                            P.TR(reads=[z_, identf], writes=[ps], out=ps[:, cc * 128:(cc + 1) * 128], in_=z_[:, c * 128:(c + 1) * 128],
                                 identity=identf[:])
                        for cc in range(4):
                            c = c4 * 4 + cc
                            P.A('activation', reads=[ps, sp], writes=[ho], out=ho[:, c, off:off + 128], in_=ps[:, cc * 128:(cc + 1) * 128],
                                func=AF.Identity, scale=sp[:, SP_LN2G + c:SP_LN2G + c + 1], bias=sp[:, SP_LN2B + c:SP_LN2B + c + 1])
                    if blk == 0:
                        P.V('memset', writes=[ho], ap=ho[:, :, 0:PAD], constant=0.0)
                    if blk % 3 == 2:
                        P.V('tensor_copy', reads=[ho], writes=[hb_], out=hb_[:], in_=ho[:])
                        P.dma('sync', hres[tl], ho[:], reads=[ho], key=ho)
                        P.dma('sync', hbf[tl], hb_[:], reads=[hb_], key=hb_)
            P.barrier()

        class _View:
            def __init__(self, tile, off):
                self.tile = tile
                self.off = off
                self.buf = tile.buf

            def __getitem__(self, idx):
                p, c, n = idx
                assert isinstance(n, slice)
                n0 = (n.start or 0) + self.off
                n1 = (n.stop if n.stop is not None else NT) + self.off
                return self.tile.t[p, c, n0:n1]

        phases = []
        phase_embed()
        done = (stop_after == 'embed')
        for l in range(depth):
            if done:
                break
            P.dma('sync', sp[:], smallp[l], writes=[sp], key=sp)
            for name, fn in (('win', phase_win), ('pool', phase_pool), ('lru', phase_lru), ('attn', phase_attn), ('merge', phase_merge),
                             ('wout', phase_wout), ('router', phase_router)):
                fn(l)
                if stop_after == "%s%d" % (name, l):
                    done = True
                    break
            if done:
                break
            phase_moe(l, last=(l == depth - 1))
        P.emit()
    return nc


def _is_tile_like(x):
    return hasattr(x, 'buf')


def _tab(v, n):
    return np.ascontiguousarray(np.asarray(v, np.float32).reshape(n, 128).T)


def make_consts():
    q = np.arange(128)[:, None]
    s = np.arange(256)[None, :]
    dist = (128 + q - s).astype(np.float32)
    inwin = (dist >= 0) & (dist < 128)
    slopes = (2.0 ** (-8.0 * np.arange(1, 17, dtype=np.float32) / 16)).astype(np.float32)
    ab = np.where(inwin[:, None, :], -slopes[None, :, None] * dist[:, None, :], np.float32(NEG)).astype(np.float32)
    pm = np.where(s < 128 + PAD, np.float32(NEG), np.float32(0.0)).astype(np.float32) * np.ones((128, 1), np.float32)
    invc = np.ones((4, 128, T), np.float32)
    tt = np.arange(T) - PAD
    for g, w in enumerate((2, 4, 8, 16)):
        cnt = np.where(tt >= 0, np.minimum(tt + 1, w), 1).astype(np.float32)
        invc[g] = (1.0 / cnt)[None, :]
    tri = (np.arange(128)[:, None] < np.arange(128)[None, :]).astype(np.float32).astype(ml_dtypes.bfloat16)
    thr = np.ascontiguousarray(np.broadcast_to((128.0 * np.arange(NSB, dtype=np.float32))[None, :], (128, NSB)))
    piota = np.arange(128, dtype=np.float32)[:, None].copy()
    return {
        "c_tri": tri, "c_thr": thr, "c_piota": piota,
        "c_ab": np.ascontiguousarray(ab), "c_pm": np.ascontiguousarray(pm), "c_invcnt": invc,
        "c_identf": np.eye(128, dtype=np.float32), "c_identb": np.eye(128, dtype=np.float32).astype(ml_dtypes.bfloat16),
    }


def make_inputs(inp, b):
    f = lambda k: np.ascontiguousarray(np.asarray(inp[k], np.float32))
    xin = np.zeros((T, D), np.float32)
    xin[PAD:PAD + NMETA] = np.asarray(inp['meta'], np.float32)
    xin[PAD + NMETA:] = np.asarray(inp['x'][b], np.float32)
    smallp = np.zeros((DEPTH, 128, SP_N), np.float32)
    for l in range(DEPTH):
        smallp[l, :, SP_LN1G:SP_LN1G + 16] = _tab(inp['ln1_g'][l], 16)
        smallp[l, :, SP_LN1B:SP_LN1B + 16] = _tab(inp['ln1_b'][l], 16)
        smallp[l, :, SP_LN2G:SP_LN2G + 16] = _tab(inp['ln2_g'][l], 16)
        smallp[l, :, SP_LN2B:SP_LN2B + 16] = _tab(inp['ln2_b'][l], 16)
        smallp[l, :, SP_PSC:SP_PSC + 8] = _tab(inp['pool_scale'][l], 8)
        for j in range(4):
            smallp[l, :, SP_CW + j * 8:SP_CW + j * 8 + 8] = _tab(inp['conv_w'][l][j], 8)
        smallp[l, :, SP_CB:SP_CB + 8] = _tab(inp['conv_b'][l], 8)
        smallp[l, :, SP_BA:SP_BA + 8] = _tab(inp['lru_ba'][l], 8)
        smallp[l, :, SP_BX:SP_BX + 8] = _tab(inp['lru_bx'][l], 8)
        smallp[l, :, SP_LAM:SP_LAM + 8] = _tab(inp['lru_lambda'][l], 8)
        smallp[l, :, SP_SINK:SP_SINK + 16] = np.asarray(inp['attn_sink'][l], np.float32)[None, :]
    embp = np.concatenate([_tab(inp['ln_emb_g'], 16), _tab(inp['ln_emb_b'], 16)], axis=1)
    rbias = np.concatenate([np.asarray(inp['router_grp_b'], np.float32), np.asarray(inp['router_exp_b'], np.float32)], axis=1)[:, None, :]
    rcat = np.concatenate([np.asarray(inp['router_grp_w'], np.float32), np.asarray(inp['router_exp_w'], np.float32)], axis=2)
    rw_tab = np.ascontiguousarray(rcat.reshape(DEPTH, DC, 128, 36).transpose(0, 2, 1, 3).reshape(DEPTH, 128, DC * 36))
    m = {
        "xin": xin, "w_in": f('w_in'), "pool_w": f('pool_w'), "lru_wa": f('lru_wa'), "lru_wx": f('lru_wx'),
        "proj_pool": f('proj_pool'), "proj_attn": f('proj_attn'), "proj_lru": f('proj_lru'), "w_out": f('w_out'),
        "rw_tab": rw_tab, "rbias": np.ascontiguousarray(rbias),
        "exp_w_gate": f('exp_w_gate'), "exp_w_up": f('exp_w_up'), "exp_w_down": f('exp_w_down'),
        "smallp": smallp, "embp": np.ascontiguousarray(embp),
        "ln2g_bc": np.ascontiguousarray(np.broadcast_to(np.asarray(inp['ln2_g'], np.float32)[:, None, :], (DEPTH, 128, D))),
        "ln2b_bc": np.ascontiguousarray(np.broadcast_to(np.asarray(inp['ln2_b'], np.float32)[:, None, :], (DEPTH, 128, D))),
    }
    m.update(make_consts())
    return m


_NC_CACHE = {}


def kernel(**inputs):
    B = inputs['x'].shape[0]
    if 'nc' not in _NC_CACHE:
        _NC_CACHE['nc'] = build_nc()
    nc = _NC_CACHE['nc']
    shared = None
    in_maps = []
    for b in range(B):
        m = make_inputs(inputs, b) if shared is None else dict(shared, xin=None)
        if shared is None:
            shared = m
        else:
            xin = np.zeros((T, D), np.float32)
            xin[PAD:PAD + NMETA] = np.asarray(inputs['meta'], np.float32)
            xin[PAD + NMETA:] = np.asarray(inputs['x'][b], np.float32)
            m['xin'] = xin
        in_maps.append(m)
    res = run_bass_kernel_spmd(nc, in_maps, core_ids=list(range(B)))
    return np.stack([np.asarray(r["out"], np.float32) for r in res.results], axis=0)
```
